# Optimizing a Trainium2 kernel written in Bass

```python
import math
import jax
import jax.numpy as jnp
from jax import lax
import numpy as np


D_MODEL = 2048
BATCH = 4
SEQ = 4096
DEPTH = 4

GRID_W = 64
CTX_LEN = 256
N_MIXERS = 3
EPS = 1e-6

D_RNN = D_MODEL
LRU_BLOCK = 128
LRU_HEADS = D_RNN // LRU_BLOCK
CONV_W = 4
CONV_PAD_LEFT = 2
LRU_C = 8.0

DIFF_HEAD_DIM = 64
DIFF_HEADS = D_MODEL // (2 * DIFF_HEAD_DIM)
DIFF_SCALE = DIFF_HEAD_DIM ** -0.5
Q_BLOCK = 128
ROPE_BASE = 10000.0

GM_HALF = D_MODEL
CHUNK = 128
GM_GROUP_CH = 128
GM_GROUPS = GM_HALF // GM_GROUP_CH

N_GROUPS = 4
EXPERTS_PER_GROUP = 8
N_EXPERTS = N_GROUPS * EXPERTS_PER_GROUP
TOP_K = 2
D_EXPERT = 512
MOE_BLOCK = 128

kernel_name = 'hybrid_dit_rglru_diffattn_gmlp_hmoe'


def rmsnorm(x, g):
    xf = x.astype(jnp.float32)
    y = xf * lax.rsqrt(jnp.mean(xf * xf, axis=-1, keepdims=True) + EPS)
    return (y * g.astype(jnp.float32)).astype(x.dtype)


def layernorm(x, g, b):
    xf = x.astype(jnp.float32)
    mu = jnp.mean(xf, axis=-1, keepdims=True)
    var = jnp.mean(jnp.square(xf - mu), axis=-1, keepdims=True)
    y = (xf - mu) * lax.rsqrt(var + EPS)
    return (y * g.astype(jnp.float32) + b.astype(jnp.float32)).astype(x.dtype)


def modulate(h, shift, scale):
    return h * (1 + scale) + shift


def axial_rope(n_tokens):
    rows = n_tokens // GRID_W
    row = jnp.repeat(jnp.arange(rows, dtype=jnp.float32), GRID_W)
    col = jnp.tile(jnp.arange(GRID_W, dtype=jnp.float32), rows)
    n_freq = DIFF_HEAD_DIM // 4
    inv_freq = ROPE_BASE ** (-jnp.arange(n_freq, dtype=jnp.float32) / n_freq)
    ang = jnp.concatenate([row[:, None] * inv_freq, col[:, None] * inv_freq], axis=-1)
    return jnp.cos(ang), jnp.sin(ang)


def apply_rope(x, cos, sin):
    half = x.shape[-1] // 2
    x1, x2 = x[..., :half], x[..., half:]
    cs, sn = cos[:, None, None, :], sin[:, None, None, :]
    return jnp.concatenate([x1 * cs - x2 * sn, x2 * cs + x1 * sn], axis=-1).astype(x.dtype)


def centred_depthwise_conv(x, w, b):
    L = x.shape[1]
    xp = jnp.pad(x, ((0, 0), (CONV_PAD_LEFT, CONV_W - 1 - CONV_PAD_LEFT), (0, 0)))
    return sum(xp[:, k:k + L] * w[k] for k in range(CONV_W)) + b


def block_diag_linear(x, w, b):
    xb = x.reshape(x.shape[:-1] + (LRU_HEADS, LRU_BLOCK))
    y = jnp.einsum('blhi,hij->blhj', xb, w, preferred_element_type=jnp.float32)
    return y.reshape(x.shape) + b.astype(jnp.float32)


def rglru_coeffs(xc, w_gate, b_gate, lam):
    r = jax.nn.sigmoid(block_diag_linear(xc, w_gate[0], b_gate[0]))
    i = jax.nn.sigmoid(block_diag_linear(xc, w_gate[1], b_gate[1]))
    log_a = -LRU_C * r * jax.nn.softplus(-lam.astype(jnp.float32))
    a = jnp.exp(log_a)
    mult = jnp.sqrt(-jnp.expm1(2.0 * log_a))
    return a, mult * i * xc.astype(jnp.float32)


def linear_scan(a, u, reverse, h0=None):
    def combine(l, r):
        return l[0] * r[0], r[0] * l[1] + r[1]
    a_cum, h = lax.associative_scan(combine, (a, u), axis=1, reverse=reverse)
    return h if h0 is None else h + a_cum * h0[:, None, :]


def rglru_mixer(h_ctx, h_lat, w_in, conv_w, conv_b, gate_w, gate_b, lam, w_out, ctx_out):
    rec_ctx, gate_ctx = jnp.split(h_ctx @ w_in, 2, axis=-1)
    rec_lat, gate_lat = jnp.split(h_lat @ w_in, 2, axis=-1)
    xc_ctx = centred_depthwise_conv(rec_ctx, conv_w, conv_b)
    xc_lat = centred_depthwise_conv(rec_lat, conv_w, conv_b)
    sum_ctx, sum_lat = 0.0, 0.0
    for d, reverse in enumerate((False, True)):
        a, u = rglru_coeffs(xc_ctx, gate_w[d], gate_b[d], lam[d])
        hs_ctx = linear_scan(a, u, reverse)
        h_end = hs_ctx[:, 0] if reverse else hs_ctx[:, -1]
        a, u = rglru_coeffs(xc_lat, gate_w[d], gate_b[d], lam[d])
        sum_lat = sum_lat + linear_scan(a, u, reverse, h_end)
        sum_ctx = sum_ctx + hs_ctx
    y_lat = (sum_lat.astype(h_lat.dtype) * jax.nn.gelu(gate_lat)) @ w_out
    y_ctx = (sum_ctx.astype(h_ctx.dtype) * jax.nn.gelu(gate_ctx)) @ w_out if ctx_out else None
    return y_ctx, y_lat


def diff_attention_mixer(h_ctx, h_lat, w_qkv, lam_vecs, subln_g, w_out, lambda_init, ctx_out):
    B, S, D = h_lat.shape

    def project(h):
        L = h.shape[1]
        q, k, v = jnp.split(h @ w_qkv, 3, axis=-1)
        return (q.reshape(B, L, DIFF_HEADS, 2, DIFF_HEAD_DIM),
                k.reshape(B, L, DIFF_HEADS, 2, DIFF_HEAD_DIM),
                v.reshape(B, L, DIFF_HEADS, 2 * DIFF_HEAD_DIM))

    q_c, k_c, v_c = project(h_ctx)
    q_l, k_l, v_l = project(h_lat)
    cos, sin = axial_rope(S)
    q_l, k_l = apply_rope(q_l, cos, sin), apply_rope(k_l, cos, sin)
    lv = lam_vecs.astype(jnp.float32)
    lam = jnp.exp(jnp.sum(lv[0] * lv[1])) - jnp.exp(jnp.sum(lv[2] * lv[3])) + lambda_init

    k_c = k_c.transpose(0, 2, 3, 1, 4)
    v_c = v_c.transpose(0, 2, 1, 3)
    k_all = jnp.concatenate([k_c, k_l.transpose(0, 2, 3, 1, 4)], axis=3)
    v_all = jnp.concatenate([v_c, v_l.transpose(0, 2, 1, 3)], axis=2)

    def attend(qb, k, v):
        s = jnp.einsum('bhcqd,bhckd->bhcqk', qb, k, preferred_element_type=jnp.float32) * DIFF_SCALE
        p = jax.nn.softmax(s, axis=-1)
        attn = p[:, :, 0] - lam * p[:, :, 1]
        return jnp.einsum('bhqk,bhkv->bhqv', attn.astype(v.dtype), v)

    def finish(o):
        L = o.shape[2]
        o = rmsnorm(o, subln_g) * (1.0 - lambda_init)
        return o.transpose(0, 2, 1, 3).reshape(B, L, D) @ w_out

    n_blk = S // Q_BLOCK
    q_blocks = q_l.transpose(0, 2, 3, 1, 4).reshape(B, DIFF_HEADS, 2, n_blk, Q_BLOCK, DIFF_HEAD_DIM)
    q_blocks = q_blocks.transpose(3, 0, 1, 2, 4, 5)
    o_blocks = lax.map(lambda qb: attend(qb, k_all, v_all), q_blocks)
    o_lat = o_blocks.transpose(1, 2, 0, 3, 4).reshape(B, DIFF_HEADS, S, 2 * DIFF_HEAD_DIM)
    y_lat = finish(o_lat)
    y_ctx = finish(attend(q_c.transpose(0, 2, 3, 1, 4), k_c, v_c)) if ctx_out else None
    return y_ctx, y_lat


def gmlp_mixer(h_ctx, h_lat, w_uv, b_uv, ln_g, ln_b, w_s, b_s, w_out, ctx_out):
    def spatial_gating(h):
        B, L, _ = h.shape
        u, v = jnp.split(jax.nn.gelu(h @ w_uv + b_uv), 2, axis=-1)
        v = layernorm(v, ln_g, ln_b).reshape(B, L // CHUNK, CHUNK, GM_GROUPS, GM_GROUP_CH)
        v = jnp.einsum('gpq,bnqgc->bnpgc', w_s, v) + b_s.T[:, :, None]
        return (u * v.reshape(B, L, GM_HALF)) @ w_out
    y_lat = spatial_gating(h_lat)
    y_ctx = spatial_gating(h_ctx) if ctx_out else None
    return y_ctx, y_lat


def hier_moe(h, wg, bg, we, be, w1, w3, w2):
    T, D = h.shape
    logits_g = jnp.dot(h, wg, preferred_element_type=jnp.float32) + bg.astype(jnp.float32)
    p_top, g_idx = lax.top_k(jax.nn.softmax(logits_g, axis=-1), 1)
    logits_e = (jnp.dot(h, we, preferred_element_type=jnp.float32) + be.astype(jnp.float32))
    logits_e = logits_e.reshape(T, N_GROUPS, EXPERTS_PER_GROUP)
    le = jnp.take_along_axis(logits_e, g_idx[:, :, None], axis=1)[:, 0]
    top_v, top_i = lax.top_k(le, TOP_K)
    gate = p_top * jax.nn.softmax(top_v, axis=-1)
    expert = (g_idx * EXPERTS_PER_GROUP + top_i).reshape(-1)
    A = T * TOP_K
    order = jnp.argsort(expert)
    sorted_e = expert[order]
    counts = jnp.bincount(expert, length=N_EXPERTS)
    starts = jnp.cumsum(counts) - counts
    padded = (counts + MOE_BLOCK - 1) // MOE_BLOCK * MOE_BLOCK
    pend = jnp.cumsum(padded)
    pstart = pend - padded
    dest_sorted = (pstart[sorted_e] + jnp.arange(A) - starts[sorted_e]).astype(jnp.int32)
    dest = jnp.zeros((A,), jnp.int32).at[order].set(dest_sorted)
    n_blocks = (A + N_EXPERTS * (MOE_BLOCK - 1) + MOE_BLOCK - 1) // MOE_BLOCK
    xs = jnp.zeros((n_blocks * MOE_BLOCK, D), h.dtype).at[dest].set(jnp.repeat(h, TOP_K, axis=0))
    block_expert = jnp.minimum(
        jnp.searchsorted(pend, jnp.arange(n_blocks) * MOE_BLOCK, side='right'), N_EXPERTS - 1)

    def expert_block(args):
        xb, e = args
        return (jax.nn.silu(xb @ w1[e]) * (xb @ w3[e])) @ w2[e]

    ys = lax.map(expert_block, (xs.reshape(n_blocks, MOE_BLOCK, D), block_expert)).reshape(-1, D)
    y = ys[dest].reshape(T, TOP_K, D).astype(jnp.float32)
    return jnp.einsum('tk,tkd->td', gate, y).astype(h.dtype)


def setup_inputs(seed: int = 0) -> dict:
    key = jax.random.key(seed)
    ks = iter(jax.random.split(key, 48))
    f32 = jnp.float32

    def nrm(shape, scale):
        return jax.random.normal(next(ks), shape, f32) * scale

    n_a = len(range(0, DEPTH, N_MIXERS))
    n_b = len(range(1, DEPTH, N_MIXERS))
    n_c = len(range(2, DEPTH, N_MIXERS))
    sd = D_MODEL ** -0.5
    a8 = jax.random.uniform(next(ks), (n_a, 2, D_RNN), f32, 0.9, 0.999)
    s = a8 ** (1.0 / LRU_C)
    lru_lambda = jnp.log(s) - jnp.log1p(-s)
    return {
        'x': nrm((BATCH, SEQ, D_MODEL), 1.0),
        'c': nrm((BATCH, D_MODEL), 1.0),
        'ctx': nrm((BATCH, CTX_LEN, D_MODEL), 1.0),
        'c_ctx': nrm((D_MODEL,), 1.0),
        'ada_w': nrm((DEPTH, D_MODEL, 6 * D_MODEL), 0.5 * sd),
        'ada_b': nrm((DEPTH, 6 * D_MODEL), 0.02),
        'norm_mix_g': 1.0 + nrm((DEPTH, D_MODEL), 0.05),
        'norm_ffn_g': 1.0 + nrm((DEPTH, D_MODEL), 0.05),
        'norm_final_g': 1.0 + nrm((D_MODEL,), 0.05),
        'router_group_w': nrm((DEPTH, D_MODEL, N_GROUPS), sd),
        'router_group_b': nrm((DEPTH, N_GROUPS), 0.01),
        'router_expert_w': nrm((DEPTH, D_MODEL, N_EXPERTS), sd),
        'router_expert_b': nrm((DEPTH, N_EXPERTS), 0.01),
        'expert_w1': nrm((DEPTH, N_EXPERTS, D_MODEL, D_EXPERT), sd),
        'expert_w3': nrm((DEPTH, N_EXPERTS, D_MODEL, D_EXPERT), sd),
        'expert_w2': nrm((DEPTH, N_EXPERTS, D_EXPERT, D_MODEL), D_EXPERT ** -0.5),
        'lru_w_in': nrm((n_a, D_MODEL, 2 * D_RNN), sd),
        'lru_conv_w': nrm((n_a, CONV_W, D_RNN), 0.5),
        'lru_conv_b': nrm((n_a, D_RNN), 0.02),
        'lru_gate_w': nrm((n_a, 2, 2, LRU_HEADS, LRU_BLOCK, LRU_BLOCK), LRU_BLOCK ** -0.5),
        'lru_gate_b': nrm((n_a, 2, 2, D_RNN), 0.1),
        'lru_lambda': lru_lambda,
        'lru_w_out': nrm((n_a, D_RNN, D_MODEL), D_RNN ** -0.5),
        'diff_w_qkv': nrm((n_b, D_MODEL, 3 * D_MODEL), sd),
        'diff_lambda': nrm((n_b, 4, DIFF_HEAD_DIM), 0.1),
        'diff_subln_g': 1.0 + nrm((n_b, 2 * DIFF_HEAD_DIM), 0.05),
        'diff_w_out': nrm((n_b, D_MODEL, D_MODEL), sd),
        'gmlp_w_uv': nrm((n_c, D_MODEL, 2 * GM_HALF), sd),
        'gmlp_b_uv': nrm((n_c, 2 * GM_HALF), 0.02),
        'gmlp_ln_g': 1.0 + nrm((n_c, GM_HALF), 0.05),
        'gmlp_ln_b': nrm((n_c, GM_HALF), 0.02),
        'gmlp_w_s': nrm((n_c, GM_GROUPS, CHUNK, CHUNK), CHUNK ** -0.5),
        'gmlp_b_s': 1.0 + nrm((n_c, GM_GROUPS, CHUNK), 0.1),
        'gmlp_w_out': nrm((n_c, GM_HALF, D_MODEL), GM_HALF ** -0.5),
    }


def reference(x, c, ctx, c_ctx, ada_w, ada_b, norm_mix_g, norm_ffn_g, norm_final_g,
              router_group_w, router_group_b, router_expert_w, router_expert_b,
              expert_w1, expert_w3, expert_w2,
              lru_w_in, lru_conv_w, lru_conv_b, lru_gate_w, lru_gate_b, lru_lambda, lru_w_out,
              diff_w_qkv, diff_lambda, diff_subln_g, diff_w_out,
              gmlp_w_uv, gmlp_b_uv, gmlp_ln_g, gmlp_ln_b, gmlp_w_s, gmlp_b_s, gmlp_w_out):
    B, S, D = x.shape
    silu_c = jax.nn.silu(c)
    silu_cc = jax.nn.silu(c_ctx)
    for i in range(DEPTH):
        last = i == DEPTH - 1
        kind, slot = i % N_MIXERS, i // N_MIXERS
        mod_lat = jnp.split((silu_c @ ada_w[i] + ada_b[i])[:, None, :], 6, axis=-1)
        mod_ctx = jnp.split(silu_cc @ ada_w[i] + ada_b[i], 6, axis=-1)
        h_lat = modulate(rmsnorm(x, norm_mix_g[i]), mod_lat[0], mod_lat[1])
        h_ctx = modulate(rmsnorm(ctx, norm_mix_g[i]), mod_ctx[0], mod_ctx[1])
        if kind == 0:
            y_ctx, y_lat = rglru_mixer(h_ctx, h_lat, lru_w_in[slot], lru_conv_w[slot], lru_conv_b[slot],
                                       lru_gate_w[slot], lru_gate_b[slot], lru_lambda[slot],
                                       lru_w_out[slot], not last)
        elif kind == 1:
            lambda_init = 0.8 - 0.6 * math.exp(-0.3 * i)
            y_ctx, y_lat = diff_attention_mixer(h_ctx, h_lat, diff_w_qkv[slot], diff_lambda[slot],
                                                diff_subln_g[slot], diff_w_out[slot], lambda_init, not last)
        else:
            y_ctx, y_lat = gmlp_mixer(h_ctx, h_lat, gmlp_w_uv[slot], gmlp_b_uv[slot], gmlp_ln_g[slot],
                                      gmlp_ln_b[slot], gmlp_w_s[slot], gmlp_b_s[slot], gmlp_w_out[slot],
                                      not last)
        x = x + mod_lat[2] * y_lat
        moe = (router_group_w[i], router_group_b[i], router_expert_w[i], router_expert_b[i],
               expert_w1[i], expert_w3[i], expert_w2[i])
        f_lat = modulate(rmsnorm(x, norm_ffn_g[i]), mod_lat[3], mod_lat[4]).reshape(B * S, D)
        if last:
            x = x + mod_lat[5] * hier_moe(f_lat, *moe).reshape(B, S, D)
        else:
            ctx = ctx + mod_ctx[2] * y_ctx
            f_ctx = modulate(rmsnorm(ctx, norm_ffn_g[i]), mod_ctx[3], mod_ctx[4]).reshape(-1, D)
            n_ctx = f_ctx.shape[0]
            y2 = hier_moe(jnp.concatenate([f_ctx, f_lat], axis=0), *moe)
            ctx = ctx + mod_ctx[5] * y2[:n_ctx].reshape(ctx.shape)
            x = x + mod_lat[5] * y2[n_ctx:].reshape(B, S, D)
    return rmsnorm(x, norm_final_g)
```

```python
import contextlib
import math
import numpy as np
import concourse.bass as bass
import concourse.mybir as mybir
from concourse.bass_utils import run_bass_kernel_spmd

F32 = mybir.dt.float32
BF16 = mybir.dt.bfloat16
I32 = mybir.dt.int32
AF = mybir.ActivationFunctionType
ALU = mybir.AluOpType
AX = mybir.AxisListType

D = 2048
NCH = 16
B = 4
S = 4096
CTX = 256
DEPTH = 4
EPS = 1e-6


class Res:
    __slots__ = ("name", "ws", "rs", "excl", "multi")

    def __init__(self, name="", excl=False, multi=False):
        self.name = name
        self.ws = {}
        self.rs = {}
        self.excl = excl
        self.multi = multi


class Ins:
    __slots__ = ("eng", "fn", "deps", "needs_inc", "semval", "dkey", "idx", "group")

    def __init__(self, eng, fn, dkey=None):
        self.idx = 0
        self.group = None
        self.eng = eng
        self.fn = fn
        self.deps = []
        self.needs_inc = False
        self.semval = None
        self.dkey = dkey


class Buf:
    __slots__ = ("ap", "r")

    def __init__(self, ap, name=""):
        self.ap = ap
        self.r = Res(name)

    def __getitem__(self, k):
        return self.ap[k]


_DSZ = {F32: 4, BF16: 2, I32: 4}
ARENA_BYTES = 204 * 1024


class Prog:
    ENGS = ("pe", "act", "dve", "pool", "sp")

    def __init__(self, nc):
        self.nc = nc
        self.lists = {e: [] for e in self.ENGS}
        self.last = {}
        self.ngroup = 0
        self.stack = contextlib.ExitStack()
        self.arena = self.stack.enter_context(nc.sbuf_tensor("arena", [128, ARENA_BYTES // 4], F32))
        self.off = 0
        self.banks = []
        for i in range(8):
            t = self.stack.enter_context(nc.psum_tensor(f"bank{i}", [128, 512], F32))
            bk = Buf(t[:], f"bank{i}")
            bk.r.excl = True
            self.banks.append(bk)

    def sb(self, shape, dtype, name=""):
        n = 1
        for v in shape[1:]:
            n *= v
        nbytes = (n * _DSZ[dtype] + 63) // 64 * 64
        w = nbytes // 4
        assert self.off + w <= ARENA_BYTES // 4, f"SBUF arena overflow allocating {name} {shape}"
        v = self.arena[0:shape[0], self.off:self.off + w]
        self.off += w
        if dtype != F32:
            v = v.bitcast(dtype)
        v = v[:, 0:n]
        if len(shape) > 2:
            names = " ".join(f"d{i}" for i in range(len(shape) - 1))
            kw = {f"d{i}": shape[i + 1] for i in range(len(shape) - 2)}
            v = v.rearrange(f"p ({names}) -> p {names}", **kw)
        return Buf(v, name)

    def newgroup(self):
        self.ngroup += 1
        return self.ngroup

    def mark(self):
        return self.off

    def release(self, m):
        self.off = m

    def dram(self, name, shape, dtype, kind="Internal"):
        return self.nc.dram_tensor(name, list(shape), dtype, kind=kind).ap()

    def op(self, eng, fn, reads=(), writes=(), dkey=None, group=None):
        ins = Ins(eng, fn, dkey)
        ins.idx = len(self.lists[eng])
        ins.group = group
        deps = {}

        def add(d):
            if d is None or d is ins:
                return
            if d.eng == "pe" and eng == "pe" and d.dkey is None and dkey is None:
                return
            if group is not None and d.group == group:
                return
            key = (d.eng, d.dkey)
            o = deps.get(key)
            if o is None or o.idx < d.idx:
                deps[key] = d

        me = (eng, dkey)
        for r in reads:
            for d in r.ws.values():
                add(d)
            if r.excl:
                for k_, d in r.rs.items():
                    if k_ != me:
                        add(d)
        for w in writes:
            for d in w.rs.values():
                add(d)
            if not w.multi:
                for d in w.ws.values():
                    add(d)
        ins.deps = list(deps.values())
        for d in ins.deps:
            d.needs_inc = True
        for r in reads:
            r.rs[me] = ins
        for w in writes:
            if w.multi:
                w.ws[me] = ins
            else:
                w.ws = {me: ins}
            w.rs = {}
        self.lists[eng].append(ins)
        self.last[me] = ins
        return ins

    def barrier(self):
        lasts = list(self.last.values())
        for e in self.ENGS:
            ins = Ins(e, lambda eng: eng.nop(), None)
            ins.idx = len(self.lists[e])
            ins.deps = [l for l in lasts if not (l.eng == e and l.dkey is None)]
            for d in ins.deps:
                d.needs_inc = True
            self.lists[e].append(ins)
            self.last[(e, None)] = ins

    def dma(self, q, out, in_, r=(), w=(), key=None, group=None, **kw):
        side = None
        for x in list(w) + list(r):
            if not x.multi:
                side = x
                break
        dk = (q, id(side) if side is not None else key)
        return self.op(q, lambda e: e.dma_start(out=out, in_=in_, **kw), r, w, dkey=dk, group=group)

    def matmul(self, out, lhsT, rhs, start, stop, r=(), w=()):
        return self.op("pe", lambda e: e.matmul(out, lhsT, rhs, start=start, stop=stop), r, w)

    def transpose(self, out, in_, ident, r=(), w=()):
        return self.op("pe", lambda e: e.transpose(out, in_, ident), r, w)

    def act(self, out, in_, func, r=(), w=(), bias=None, scale=None, eng="act"):
        kw = {}
        if bias is not None:
            kw["bias"] = bias
        if scale is not None:
            kw["scale"] = scale
        return self.op(eng, lambda e: e.activation(out=out, in_=in_, func=func, **kw), r, w)

    def tt(self, eng, out, in0, in1, op, r=(), w=()):
        return self.op(eng, lambda e: e.tensor_tensor(out=out, in0=in0, in1=in1, op=op), r, w)

    def ts(self, eng, out, in0, s1, op0, s2=None, op1=None, r=(), w=()):
        if op1 is None:
            return self.op(eng, lambda e: e.tensor_scalar(out=out, in0=in0, scalar1=s1, scalar2=None, op0=op0), r, w)
        return self.op(eng, lambda e: e.tensor_scalar(out=out, in0=in0, scalar1=s1, scalar2=s2, op0=op0, op1=op1), r, w)

    def stt(self, eng, out, in0, scalar, in1, op0, op1, r=(), w=()):
        return self.op(eng, lambda e: e.scalar_tensor_tensor(out=out, in0=in0, scalar=scalar, in1=in1, op0=op0, op1=op1), r, w)

    def copy(self, eng, out, in_, r=(), w=()):
        if eng == "act":
            return self.op(eng, lambda e: e.copy(out=out, in_=in_), r, w)
        return self.op(eng, lambda e: e.tensor_copy(out=out, in_=in_), r, w)

    def memset(self, eng, out, val, w=()):
        return self.op(eng, lambda e: e.memset(out, val), (), w)

    def emit(self, final_waits=()):
        nc = self.nc
        final_waits = [ins for k, ins in self.last.items() if k[1] is not None]
        for ins in final_waits:
            ins.needs_inc = True
        esem = {}
        dsem = {}
        for e in self.ENGS:
            cnt = 0
            dcnt = {}
            for ins in self.lists[e]:
                if ins.dkey is not None:
                    dcnt[ins.dkey] = dcnt.get(ins.dkey, 0) + 16
                    ins.semval = dcnt[ins.dkey]
                    dsem.setdefault(ins.dkey, None)
                elif ins.needs_inc:
                    cnt += 1
                    ins.semval = cnt
            esem[e] = None
        for e in self.ENGS:
            esem[e] = self.stack.enter_context(nc.semaphore(f"s_{e}"))
        for i, k in enumerate(dsem):
            dsem[k] = self.stack.enter_context(nc.semaphore(f"d_{i}"))

        def sem_of(ins):
            return dsem[ins.dkey] if ins.dkey is not None else esem[ins.eng]

        def run(e, eng):
            waited = {}
            for ins in self.lists[e]:
                for d in ins.deps:
                    s = sem_of(d)
                    k = id(s)
                    if waited.get(k, 0) < d.semval:
                        eng.wait_ge(s, d.semval)
                        waited[k] = d.semval
                bi = ins.fn(eng)
                if ins.dkey is not None:
                    bi.then_inc(dsem[ins.dkey], 16)
                elif ins.needs_inc:
                    bi.then_inc(esem[e], 1)
            if e == "sp":
                for d in final_waits:
                    s = sem_of(d)
                    if waited.get(id(s), 0) < d.semval:
                        eng.wait_ge(s, d.semval)
                        waited[id(s)] = d.semval

        with nc.Block() as block:
            @block.tensor
            def _(eng):
                run("pe", eng)

            @block.scalar
            def _(eng):
                run("act", eng)

            @block.vector
            def _(eng):
                run("dve", eng)

            @block.gpsimd
            def _(eng):
                run("pool", eng)

            @block.sync
            def _(eng):
                run("sp", eng)
        self.stack.close()


def new_nc():
    return bass.Bass("TRN2", target_bir_lowering=False)


def make_consts(p):
    c = {}
    c["ones_bf"] = p.sb([128, 128], BF16, "ones_bf")
    p.memset("pool", c["ones_bf"].ap, 1.0, w=[c["ones_bf"].r])
    c["eps"] = p.sb([128, 1], F32, "eps")
    p.memset("pool", c["eps"].ap, EPS, w=[c["eps"].r])
    idf = p.sb([128, 128], F32, "ident_f")
    p.memset("pool", idf.ap, 0.0, w=[idf.r])
    p.op("pool", lambda e: e.affine_select(out=idf.ap, in_=idf.ap, pattern=[[-1, 128]], compare_op=ALU.not_equal,
                                           fill=1.0, base=0, channel_multiplier=1), [idf.r], [idf.r])
    c["ident_f"] = idf
    c["ident_bf"] = p.sb([128, 128], BF16, "ident_bf")
    p.copy("pool", c["ident_bf"].ap, idf.ap, r=[idf.r], w=[c["ident_bf"].r])
    return c


def emit_rstd(p, c, x, N, sq, bank, rstd):
    p.act(sq.ap[:, :, 0:N], x.ap[:, :, 0:N], AF.Square, r=[x.r], w=[sq.r])
    for kc in range(NCH):
        p.matmul(bank.ap[:, 0:N], c["ones_bf"].ap, sq.ap[:, kc, 0:N], kc == 0, kc == NCH - 1,
                 r=[c["ones_bf"].r, sq.r], w=[bank.r])
    p.act(rstd.ap[:, 0:N], bank.ap[:, 0:N], AF.Sqrt, r=[bank.r, c["eps"].r], w=[rstd.r], bias=c["eps"].ap, scale=1.0 / D)
    p.op("dve", lambda e: e.reciprocal(out=rstd.ap[:, 0:N], in_=rstd.ap[:, 0:N]), [rstd.r], [rstd.r])


def emit_mod(p, x, rstd, gs, sh, N, out_bf, tmps=None, out_f32=None):
    gs_ap, gs_r = gs
    sh_ap, sh_r = sh
    for cch in range(NCH):
        if out_f32 is not None:
            dst = out_f32.ap[:, cch, 0:N]
            dr = out_f32.r
        else:
            t = tmps[cch % len(tmps)]
            dst = t.ap[:, 0:N]
            dr = t.r
        p.stt("dve", dst, x.ap[:, cch, 0:N], gs_ap[:, cch:cch + 1], rstd.ap[:, 0:N], ALU.mult, ALU.mult,
              r=[x.r, gs_r, rstd.r], w=[dr])
        if out_f32 is not None:
            p.act(dst, dst, AF.Identity, r=[dr, sh_r], w=[dr], bias=sh_ap[:, cch:cch + 1])
        else:
            p.act(out_bf.ap[:, cch, 0:N], dst, AF.Identity, r=[dr, sh_r], w=[out_bf.r], bias=sh_ap[:, cch:cch + 1])
    if out_f32 is not None and out_bf is not None:
        p.copy("pool", out_bf.ap[:, :, 0:N], out_f32.ap[:, :, 0:N], r=[out_f32.r], w=[out_bf.r])


def load_w(p, dst, w2d, key, nsplit=4, q="pool"):
    K = w2d.shape[0]
    kc = K // 128
    src = w2d.rearrange("(kc p) n -> p kc n", p=128)
    step = max(1, kc // nsplit)
    grp = p.newgroup()
    for k0 in range(0, kc, step):
        p.dma(q, dst.ap[:, k0:k0 + step, :], src[:, k0:k0 + step, :], w=[dst.r], key=key, group=grp)


ADA_COLS = 6 * D // 8


def build_ada():
    nc = new_nc()
    p = Prog(nc)
    cT = nc.dram_tensor("cT", [128, NCH, 5], F32, kind="ExternalInput").ap()
    w = nc.dram_tensor("w", [DEPTH, D, ADA_COLS], F32, kind="ExternalInput").ap()
    bia = nc.dram_tensor("b", [DEPTH, 1, ADA_COLS], F32, kind="ExternalInput").ap()
    out = nc.dram_tensor("mod", [DEPTH, 5, ADA_COLS], F32, kind="ExternalOutput").ap()
    s = p.sb([128, NCH, 5], F32, "s")
    p.dma("sp", s.ap, cT, w=[s.r], key="in")
    p.act(s.ap, s.ap, AF.Silu, r=[s.r], w=[s.r])
    wb = [p.sb([128, NCH, 512], F32, f"wb{i}") for i in range(2)]
    bb = [p.sb([5, 512], F32, f"bb{i}") for i in range(2)]
    ob = [p.sb([5, 512], F32, f"ob{i}") for i in range(2)]
    outs = []
    it = 0
    for l in range(DEPTH):
        for nb in range(ADA_COLS // 512):
            W = wb[it % 2]
            bt = bb[it % 2]
            o = ob[it % 2]
            bank = p.banks[it % 2]
            src = w[l, :, nb * 512:(nb + 1) * 512].rearrange("(kc p) n -> p kc n", p=128)
            grp = p.newgroup()
            for k0 in range(0, NCH, 4):
                p.dma("sp", W.ap[:, k0:k0 + 4, :], src[:, k0:k0 + 4, :], w=[W.r], key="w", group=grp)
            p.dma("sp", bt.ap, bia[l, :, nb * 512:(nb + 1) * 512].partition_broadcast(5), w=[bt.r], key="b")
            for kc in range(NCH):
                p.matmul(bank.ap[0:5, :], s.ap[:, kc, :], W.ap[:, kc, :], kc == 0, kc == NCH - 1, r=[s.r, W.r], w=[bank.r])
            p.tt("dve", o.ap, bank.ap[0:5, :], bt.ap, ALU.add, r=[bank.r, bt.r], w=[o.r])
            outs.append(p.dma("sp", out[l, :, nb * 512:(nb + 1) * 512], o.ap, r=[o.r], key="out"))
            it += 1
    p.emit(final_waits=outs[-1:])
    return nc


def pp(v):
    v = np.asarray(v)
    return np.ascontiguousarray(v.reshape(-1, 128).T)


def fm_src(x2d, c0, n):
    return x2d[:, c0:c0 + n].rearrange("(kc p) n -> p kc n", p=128)


def load_fm(p, q, dst, x2d, c0, n, key, nsplit=2):
    src = fm_src(x2d, c0, n)
    step = NCH // nsplit
    grp = p.newgroup()
    for k0 in range(0, NCH, step):
        p.dma(q, dst.ap[:, k0:k0 + step, 0:n], src[:, k0:k0 + step, :], w=[dst.r], key=key, group=grp)


def store_fm(p, q, x2d, c0, n, src, key, nsplit=2):
    dst = fm_src(x2d, c0, n)
    step = NCH // nsplit
    out = None
    grp = p.newgroup()
    for k0 in range(0, NCH, step):
        out = p.dma(q, dst[:, k0:k0 + step, :], src.ap[:, k0:k0 + step, 0:n], r=[src.r], key=key, group=grp)
    return out


TB = CTX + S
LRU_NV = 328
LV_GPREV, LV_MOD, LV_GMIX, LV_CW, LV_CB, LV_GB, LV_LAM = 0, 32, 224, 240, 272, 280, 312


def build_lru():
    nc = new_nc()
    p = Prog(nc)
    xa = nc.dram_tensor("xa", [D, TB], F32, kind="ExternalInput").ap()
    yb = nc.dram_tensor("yb", [D, TB], F32, kind="ExternalInput").ap()
    vec = nc.dram_tensor("vec", [128, LRU_NV], F32, kind="ExternalInput").ap()
    w_in = nc.dram_tensor("w_in", [D, 2048], F32, kind="ExternalInput").ap()
    gw = nc.dram_tensor("gw", [2, 2, 8, 128, 128], F32, kind="ExternalInput").ap()
    gout = nc.dram_tensor("g", [1024, TB], BF16, kind="ExternalOutput").ap()
    scr = p.dram("scr", [16, 128, TB], F32)
    scr_r = Res("scr", multi=True)
    c = make_consts(p)
    V = p.sb([128, LRU_NV], F32, "V")
    p.dma("sp", V.ap, vec, w=[V.r], key="in")
    one = p.sb([128, 1], F32, "one")
    p.memset("pool", one.ap, 1.0, w=[one.r])
    gs = p.sb([128, 2, NCH], F32, "gs")
    for cls in range(2):
        sc = V.ap[:, LV_MOD + cls * 96 + 16: LV_MOD + cls * 96 + 32]
        p.stt("dve", gs.ap[:, cls, :], sc, 1.0, V.ap[:, LV_GMIX:LV_GMIX + 16], ALU.add, ALU.mult, r=[V.r], w=[gs.r])
    ca = p.sb([128, 16], F32, "ca")
    c2 = p.sb([128, 16], F32, "c2")
    p.act(ca.ap, V.ap[:, LV_LAM:LV_LAM + 16], AF.Exp, r=[V.r], w=[ca.r], scale=-1.0)
    p.act(ca.ap, ca.ap, AF.Ln, r=[ca.r, one.r], w=[ca.r], bias=one.ap)
    p.ts("dve", c2.ap, ca.ap, -16.0, ALU.mult, r=[ca.r], w=[c2.r])
    p.ts("dve", ca.ap, ca.ap, -8.0, ALU.mult, r=[ca.r], w=[ca.r])
    gw_sb = p.sb([128, 32, 128], BF16, "gw")
    p.dma("pool", gw_sb.ap, gw.rearrange("d r h i o -> i (d r h) o"), w=[gw_sb.r], key="gw")
    m0 = p.mark()

    NB = 256
    w_sb = p.sb([128, NCH, 2048], BF16, "w_in")
    load_w(p, w_sb, w_in, key="w", nsplit=8)
    xab = [p.sb([128, NCH, NB], F32, f"xa{i}") for i in range(2)]
    ybb = [p.sb([128, NCH, NB], F32, f"yb{i}") for i in range(2)]
    h = p.sb([128, NCH, NB], BF16, "h")
    sq = p.sb([128, NCH, NB], BF16, "sq")
    rstd = p.sb([128, NB], F32, "rstd")
    tmps = [p.sb([128, NB], F32, f"tmp{i}") for i in range(2)]
    stage = p.sb([128, NCH, NB], F32, "stage")
    nblk = TB // NB
    for bi in range(nblk):
        c0 = bi * NB
        cls = 0 if bi == 0 else 1
        X = xab[bi % 2]
        Y = ybb[bi % 2]
        load_fm(p, "sp", X, xa, c0, NB, "xa")
        load_fm(p, "sp", Y, yb, c0, NB, "yb")
        for kc in range(NCH):
            p.stt("dve", X.ap[:, kc, :], Y.ap[:, kc, :], V.ap[:, LV_GPREV + cls * 16 + kc: LV_GPREV + cls * 16 + kc + 1],
                  X.ap[:, kc, :], ALU.mult, ALU.add, r=[X.r, Y.r, V.r], w=[X.r])
        emit_rstd(p, c, X, NB, sq, p.banks[0], rstd)
        sh = (V.ap[:, LV_MOD + cls * 96: LV_MOD + cls * 96 + 16], V.r)
        emit_mod(p, X, rstd, (gs.ap[:, cls, :], gs.r), sh, NB, h, tmps=tmps)
        for oc in range(16):
            bank = p.banks[1 + oc % 4]
            for kc in range(NCH):
                p.matmul(bank.ap[:, 0:NB], w_sb.ap[:, kc, oc * 128:(oc + 1) * 128], h.ap[:, kc, :], kc == 0, kc == NCH - 1,
                         r=[w_sb.r, h.r], w=[bank.r])
            p.copy("act" if oc % 2 == 0 else "dve", stage.ap[:, oc, :], bank.ap[:, 0:NB], r=[bank.r], w=[stage.r])
        dst = scr[:, :, c0:c0 + NB].rearrange("oc p t -> p oc t")
        grp = p.newgroup()
        for k0 in range(0, 16, 8):
            p.dma("sp", dst[:, k0:k0 + 8, :], stage.ap[:, k0:k0 + 8, :], r=[stage.r], w=[scr_r], key="scr", group=grp)

    p.barrier()
    p.release(m0)
    rec = p.sb([128, TB], F32, "rec")
    gat = p.sb([128, TB], F32, "gat")
    xc = p.sb([128, TB], F32, "xc")
    xcb = p.sb([128, TB], BF16, "xcb")
    Rb = p.sb([128, TB], F32, "Rb")
    Ib = p.sb([128, TB], F32, "Ib")
    Eb = p.sb([128, TB], F32, "Eb")
    H = [p.sb([128, TB], F32, f"H{i}") for i in range(2)]
    ob = p.sb([128, TB], BF16, "ob")
    segs = [(0, CTX), (CTX, TB)]
    outs = []
    for j in range(8):
        p.dma("sp", rec.ap, scr[j], r=[scr_r], w=[rec.r], key="rec")
        p.dma("sp", gat.ap, scr[8 + j], r=[scr_r], w=[gat.r], key="gat")
        cw = lambda k: V.ap[:, LV_CW + j * 4 + k: LV_CW + j * 4 + k + 1]
        p.ts("dve", xc.ap, rec.ap, cw(2), ALU.mult, V.ap[:, LV_CB + j:LV_CB + j + 1], ALU.add, r=[rec.r, V.r], w=[xc.r])
        for (s0, e0) in segs:
            for k, off in ((0, -2), (1, -1), (3, 1)):
                if off < 0:
                    o_sl = slice(s0 - off, e0)
                    i_sl = slice(s0, e0 + off)
                else:
                    o_sl = slice(s0, e0 - off)
                    i_sl = slice(s0 + off, e0)
                p.stt("dve", xc.ap[:, o_sl], rec.ap[:, i_sl], cw(k), xc.ap[:, o_sl], ALU.mult, ALU.add,
                      r=[rec.r, xc.r, V.r], w=[xc.r])
        p.copy("act", xcb.ap, xc.ap, r=[xc.r], w=[xcb.r])
        for d in range(2):
            for which, dstb in ((0, Rb), (1, Ib)):
                gi = (d * 2 + which) * 8 + j
                bcol = V.ap[:, LV_GB + gi: LV_GB + gi + 1]
                for bi, t0 in enumerate(range(0, TB, 512)):
                    n = min(512, TB - t0)
                    bank = p.banks[bi % 4]
                    p.matmul(bank.ap[:, 0:n], gw_sb.ap[:, gi, :], xcb.ap[:, t0:t0 + n], True, True, r=[gw_sb.r, xcb.r], w=[bank.r])
                    p.act(dstb.ap[:, t0:t0 + n], bank.ap[:, 0:n], AF.Sigmoid, r=[bank.r, V.r], w=[dstb.r], bias=bcol)
            li = d * 8 + j
            p.act(Eb.ap, Rb.ap, AF.Exp, r=[Rb.r, c2.r], w=[Eb.r], scale=c2.ap[:, li:li + 1])
            p.act(Rb.ap, Rb.ap, AF.Exp, r=[Rb.r, ca.r], w=[Rb.r], scale=ca.ap[:, li:li + 1])
            p.act(Eb.ap, Eb.ap, AF.Sqrt, r=[Eb.r, one.r], w=[Eb.r], bias=one.ap, scale=-1.0)
            p.tt("dve", Ib.ap, Ib.ap, Eb.ap, ALU.mult, r=[Ib.r, Eb.r], w=[Ib.r])
            p.tt("dve", Ib.ap, Ib.ap, xc.ap, ALU.mult, r=[Ib.r, xc.r], w=[Ib.r])
            Hd = H[d]
            if d == 0:
                p.op("dve", lambda e, Hd=Hd: e.tensor_tensor_scan(out=Hd.ap, data0=Rb.ap, data1=Ib.ap, initial=0.0,
                                                                   op0=ALU.mult, op1=ALU.add), [Rb.r, Ib.r], [Hd.r])
            else:
                p.op("dve", lambda e, Hd=Hd: e.tensor_tensor_scan(out=Hd.ap[:, 0:CTX][:, ::-1], data0=Rb.ap[:, 0:CTX][:, ::-1],
                                                                   data1=Ib.ap[:, 0:CTX][:, ::-1], initial=0.0,
                                                                   op0=ALU.mult, op1=ALU.add), [Rb.r, Ib.r], [Hd.r])
                p.op("dve", lambda e, Hd=Hd: e.tensor_tensor_scan(out=Hd.ap[:, CTX:TB][:, ::-1], data0=Rb.ap[:, CTX:TB][:, ::-1],
                                                                   data1=Ib.ap[:, CTX:TB][:, ::-1], initial=Hd.ap[:, 0:1],
                                                                   op0=ALU.mult, op1=ALU.add), [Rb.r, Ib.r, Hd.r], [Hd.r])
        p.tt("dve", H[0].ap, H[0].ap, H[1].ap, ALU.add, r=[H[0].r, H[1].r], w=[H[0].r])
        p.act(gat.ap, gat.ap, AF.Gelu_apprx_tanh, r=[gat.r], w=[gat.r])
        p.tt("dve", ob.ap, H[0].ap, gat.ap, ALU.mult, r=[H[0].r, gat.r], w=[ob.r])
        outs.append(p.dma("sp", gout[j * 128:(j + 1) * 128, :], ob.ap, r=[ob.r], key="out"))
    p.emit(final_waits=outs[-1:])
    return nc


def lru_vec(gprev, mod, layer, slot, hf, inp):
    ch = slice(hf * 1024, (hf + 1) * 1024)
    cols = [pp(gprev[0]), pp(gprev[1])]
    for cls in range(2):
        for k in range(6):
            cols.append(pp(mod[cls, k]))
    cols.append(pp(inp["norm_mix_g"][layer]))
    cw = inp["lru_conv_w"][slot][:, ch]
    cols.append(np.ascontiguousarray(cw.reshape(4, 8, 128).transpose(2, 1, 0).reshape(128, 32)))
    cols.append(pp(inp["lru_conv_b"][slot][ch]))
    gb = inp["lru_gate_b"][slot][:, :, ch]
    cols.append(np.ascontiguousarray(gb.reshape(2, 2, 8, 128).transpose(3, 0, 1, 2).reshape(128, 32)))
    lam = inp["lru_lambda"][slot][:, ch]
    cols.append(np.ascontiguousarray(lam.reshape(2, 8, 128).transpose(2, 0, 1).reshape(128, 16)))
    v = np.concatenate(cols, axis=1).astype(np.float32)
    assert v.shape == (128, LRU_NV)
    return v


TC = 128 + S // 2
NT = TC // 128
PCORES = 4
TP = TB
NTP = TP // 128
NBP = (2 * TP + 32 * 127 + 127) // 128
NSP = NBP * 128
PV_GPREV, PV_MOD, PV_GFFN, POST_NV = 0, 32, 224, 240
BIG = 1.0e30
RC_N = 36 + NBP


def build_post():
    nc = new_nc()
    p = Prog(nc)
    xa = nc.dram_tensor("xa", [D, TP], F32, kind="ExternalInput").ap()
    yb = nc.dram_tensor("yb", [D, TP], F32, kind="ExternalInput").ap()
    G = nc.dram_tensor("G", [D, TP], BF16, kind="ExternalInput").ap()
    vec = nc.dram_tensor("vec", [128, POST_NV], F32, kind="ExternalInput").ap()
    w_out = nc.dram_tensor("w_out", [D, D], F32, kind="ExternalInput").ap()
    wr = nc.dram_tensor("wr", [D, 36], F32, kind="ExternalInput").ap()
    rc = nc.dram_tensor("rc", [1, RC_N], F32, kind="ExternalInput").ap()
    pcol = nc.dram_tensor("pcol", [128, 1], F32, kind="ExternalInput").ap()
    w1 = nc.dram_tensor("w1", [32 * 128, NCH * 512], F32, kind="ExternalInput").ap()
    w3 = nc.dram_tensor("w3", [32 * 128, NCH * 512], F32, kind="ExternalInput").ap()
    w2 = nc.dram_tensor("w2", [32 * 128, 4 * D], F32, kind="ExternalInput").ap()
    xmid = nc.dram_tensor("xmid", [D, TP], F32, kind="ExternalOutput").ap()
    ymoe = nc.dram_tensor("ymoe", [TP, D], F32, kind="ExternalOutput").ap()
    fd = p.dram("fd", [TP, D], BF16)
    xs = p.dram("xs", [NSP, D], BF16)
    ys = p.dram("ys", [NSP, D], F32)
    fd_r = Res("fd", multi=True)
    xs_r = Res("xs", multi=True)
    ys_r = Res("ys", multi=True)
    c = make_consts(p)
    V = p.sb([128, POST_NV], F32, "V")
    p.dma("sp", V.ap, vec, w=[V.r], key="in")
    RC = p.sb([128, RC_N], F32, "RC")
    p.dma("sp", RC.ap, rc.partition_broadcast(128), w=[RC.r], key="in")
    PC = p.sb([128, 1], F32, "PC")
    p.dma("sp", PC.ap, pcol, w=[PC.r], key="in")
    gs = p.sb([128, 2, NCH], F32, "gs")
    for cls in range(2):
        sc = V.ap[:, PV_MOD + cls * 96 + 64: PV_MOD + cls * 96 + 80]
        p.stt("dve", gs.ap[:, cls, :], sc, 1.0, V.ap[:, PV_GFFN:PV_GFFN + 16], ALU.add, ALU.mult, r=[V.r], w=[gs.r])
    U = p.sb([128, 128], BF16, "U")
    uf = p.sb([128, 128], F32, "uf")
    p.memset("pool", uf.ap, 0.0, w=[uf.r])
    p.op("pool", lambda e: e.affine_select(out=uf.ap, in_=uf.ap, pattern=[[-1, 128]], compare_op=ALU.is_ge,
                                           fill=1.0, base=0, channel_multiplier=1), [uf.r], [uf.r])
    p.copy("pool", U.ap, uf.ap, r=[uf.r], w=[U.r])
    desti = p.sb([128, NTP, 2], I32, "desti")
    gall = p.sb([128, NTP, 2], F32, "gall")
    idxw = p.sb([128, NBP], I32, "idxw")
    m0 = p.mark()

    NB = 256
    cum = p.sb([128, 32], F32, "cum")
    p.memset("pool", cum.ap, 0.0, w=[cum.r])
    posall = p.sb([128, NTP, 32], F32, "posall")
    mkall = p.sb([128, NTP, 2, 32], F32, "mkall")
    wo = p.sb([128, NCH, D], BF16, "wo")
    load_w(p, wo, w_out, key="w", nsplit=8)
    wr_sb = p.sb([128, NCH, 36], F32, "wr")
    p.dma("sp", wr_sb.ap, wr.rearrange("(kc p) n -> p kc n", p=128), w=[wr_sb.r], key="in")
    Xb = [p.sb([128, NCH, NB], F32, f"X{i}") for i in range(2)]
    Y = p.sb([128, NCH, NB], F32, "Y")
    Gb = [p.sb([128, NCH, NB], BF16, f"G{i}") for i in range(2)]
    sq = p.sb([128, NCH, NB], BF16, "sq")
    fbf = p.sb([128, NCH, NB], BF16, "fbf")
    rstd = p.sb([128, NB], F32, "rstd")
    ftok = [p.sb([128, D], BF16, f"ftok{i}") for i in range(2)]
    Mt = [p.sb([128, 32], BF16, f"Mt{i}") for i in range(2)]

    def rt(name, shape, dt=F32):
        return [p.sb(shape, dt, f"{name}{i}") for i in range(2)]
    lg = rt("lg", [128, 36]); m4 = rt("m4", [128, 1]); d4 = rt("d4", [128, 4]); s4 = rt("s4", [128, 1])
    oh4 = rt("oh4", [128, 4]); lem = rt("lem", [128, 32]); lem2 = rt("lem2", [128, 32])
    top1 = rt("top1", [128, 1]); top2 = rt("top2", [128, 1]); d12 = rt("d12", [128, 1])
    blocks = [(CTX * 0, CTX)] + [(CTX + NB * i, NB) for i in range((TP - CTX) // NB)]
    xm_outs = []
    ti = 0
    for bi, (c0, N) in enumerate(blocks):
        cls = 0 if bi == 0 else 1
        X = Xb[bi % 2]
        Gt = Gb[bi % 2]
        load_fm(p, "sp", X, xa, c0, N, "xa")
        load_fm(p, "sp", Y, yb, c0, N, "yb")
        load_fm(p, "sp", Gt, G, c0, N, "G")
        for kc in range(NCH):
            p.stt("dve", X.ap[:, kc, 0:N], Y.ap[:, kc, 0:N], V.ap[:, PV_GPREV + cls * 16 + kc: PV_GPREV + cls * 16 + kc + 1],
                  X.ap[:, kc, 0:N], ALU.mult, ALU.add, r=[X.r, Y.r, V.r], w=[X.r])
        g1c = PV_MOD + cls * 96 + 32
        for oc in range(NCH):
            bank = p.banks[oc % 2]
            for kc in range(NCH):
                p.matmul(bank.ap[:, 0:N], wo.ap[:, kc, oc * 128:(oc + 1) * 128], Gt.ap[:, kc, 0:N], kc == 0, kc == NCH - 1,
                         r=[wo.r, Gt.r], w=[bank.r])
            p.stt("dve", X.ap[:, oc, 0:N], bank.ap[:, 0:N], V.ap[:, g1c + oc: g1c + oc + 1], X.ap[:, oc, 0:N], ALU.mult, ALU.add,
                  r=[bank.r, X.r, V.r], w=[X.r])
        xm_outs.append(store_fm(p, "sp", xmid, c0, N, X, "xmid"))
        emit_rstd(p, c, X, N, sq, p.banks[2], rstd)
        sh = (V.ap[:, PV_MOD + cls * 96 + 48: PV_MOD + cls * 96 + 64], V.r)
        emit_mod(p, X, rstd, (gs.ap[:, cls, :], gs.r), sh, N, fbf, out_f32=Y)
        for tt_ in range(N // 128):
            q = ti % 2
            tsl = slice(tt_ * 128, (tt_ + 1) * 128)
            rb = p.banks[3]
            for kc in range(NCH):
                p.matmul(rb.ap[:, 0:36], Y.ap[:, kc, tsl], wr_sb.ap[:, kc, :], kc == 0, kc == NCH - 1, r=[Y.r, wr_sb.r], w=[rb.r])
            L = lg[q]
            mk1 = mkall.ap[:, ti, 0, :]
            mk2 = mkall.ap[:, ti, 1, :]
            p.tt("dve", L.ap, rb.ap[:, 0:36], RC.ap[:, 0:36], ALU.add, r=[rb.r, RC.r], w=[L.r])
            p.op("dve", lambda e, o=m4[q], L=L: e.tensor_reduce(out=o.ap, in_=L.ap[:, 0:4], axis=AX.X, op=ALU.max), [L.r], [m4[q].r])
            p.ts("dve", d4[q].ap, L.ap[:, 0:4], m4[q].ap, ALU.subtract, r=[L.r, m4[q].r], w=[d4[q].r])
            p.act(d4[q].ap, d4[q].ap, AF.Exp, r=[d4[q].r], w=[d4[q].r])
            p.op("dve", lambda e, o=s4[q], i_=d4[q]: e.tensor_reduce(out=o.ap, in_=i_.ap, axis=AX.X, op=ALU.add), [d4[q].r], [s4[q].r])
            p.op("dve", lambda e, o=s4[q]: e.reciprocal(out=o.ap, in_=o.ap), [s4[q].r], [s4[q].r])
            p.ts("dve", oh4[q].ap, L.ap[:, 0:4], m4[q].ap, ALU.is_equal, r=[L.r, m4[q].r], w=[oh4[q].r])
            p.ts("dve", oh4[q].ap, oh4[q].ap, -1.0, ALU.add, BIG, ALU.mult, r=[oh4[q].r], w=[oh4[q].r])
            for g in range(4):
                p.ts("dve", lem[q].ap[:, 8 * g:8 * g + 8], L.ap[:, 4 + 8 * g:12 + 8 * g], oh4[q].ap[:, g:g + 1], ALU.add,
                     r=[L.r, oh4[q].r], w=[lem[q].r])
            p.op("dve", lambda e, o=top1[q], i_=lem[q]: e.tensor_reduce(out=o.ap, in_=i_.ap, axis=AX.X, op=ALU.max), [lem[q].r], [top1[q].r])
            p.ts("dve", mk1, lem[q].ap, top1[q].ap, ALU.is_equal, r=[lem[q].r, top1[q].r], w=[mkall.r])
            p.stt("dve", lem2[q].ap, mk1, -BIG, lem[q].ap, ALU.mult, ALU.add, r=[mkall.r, lem[q].r], w=[lem2[q].r])
            p.op("dve", lambda e, o=top2[q], i_=lem2[q]: e.tensor_reduce(out=o.ap, in_=i_.ap, axis=AX.X, op=ALU.max), [lem2[q].r], [top2[q].r])
            p.ts("dve", mk2, lem2[q].ap, top2[q].ap, ALU.is_equal, r=[lem2[q].r, top2[q].r], w=[mkall.r])
            p.tt("dve", d12[q].ap, top1[q].ap, top2[q].ap, ALU.subtract, r=[top1[q].r, top2[q].r], w=[d12[q].r])
            p.act(d12[q].ap, d12[q].ap, AF.Sigmoid, r=[d12[q].r], w=[d12[q].r])
            p.tt("dve", gall.ap[:, ti, 0:1], d12[q].ap, s4[q].ap, ALU.mult, r=[d12[q].r, s4[q].r], w=[gall.r])
            p.tt("dve", gall.ap[:, ti, 1:2], s4[q].ap, gall.ap[:, ti, 0:1], ALU.subtract, r=[s4[q].r, gall.r], w=[gall.r])
            p.tt("dve", Mt[q].ap, mk1, mk2, ALU.add, r=[mkall.r], w=[Mt[q].r])
            pb = p.banks[4]
            cb = p.banks[5]
            p.matmul(pb.ap[:, 0:32], U.ap, Mt[q].ap, True, True, r=[U.r, Mt[q].r], w=[pb.r])
            p.matmul(cb.ap[:, 0:32], c["ones_bf"].ap, Mt[q].ap, True, True, r=[c["ones_bf"].r, Mt[q].r], w=[cb.r])
            p.tt("dve", posall.ap[:, ti, :], pb.ap[:, 0:32], cum.ap, ALU.add, r=[pb.r, cum.r], w=[posall.r])
            p.tt("dve", cum.ap, cb.ap[:, 0:32], cum.ap, ALU.add, r=[cb.r, cum.r], w=[cum.r])
            F = ftok[q]
            for half in range(2):
                tb = p.banks[6 + half]
                tbv = tb.ap.bitcast(BF16)
                for k8 in range(8):
                    kc = half * 8 + k8
                    p.transpose(tbv[:, k8 * 128:(k8 + 1) * 128], fbf.ap[:, kc, tsl], c["ident_bf"].ap, r=[fbf.r, c["ident_bf"].r], w=[tb.r])
                p.copy("act", F.ap[:, half * 1024:(half + 1) * 1024], tbv[:, 0:1024], r=[tb.r], w=[F.r])
            p.dma("sp", fd[ti * 128:(ti + 1) * 128, :], F.ap, r=[F.r], w=[fd_r], key="fd")
            ti += 1
    assert ti == NTP

    padded = p.sb([128, 32], F32, "padded")
    pend = p.sb([128, 32], F32, "pend")
    pstart = p.sb([128, 32], F32, "pstart")
    onesr = p.sb([128, 32], F32, "onesr")
    be_f = p.sb([128, NBP], F32, "be_f")
    p.memset("pool", onesr.ap, 1.0, w=[onesr.r])
    p.memset("pool", padded.ap, 0.0, w=[padded.r])
    for m_ in range((2 * TP) // 128):
        p.stt("dve", padded.ap, cum.ap, float(128 * m_), padded.ap, ALU.is_gt, ALU.add, r=[cum.r, padded.r], w=[padded.r])
    p.ts("dve", padded.ap, padded.ap, 128.0, ALU.mult, r=[padded.r], w=[padded.r])
    p.op("dve", lambda e: e.tensor_tensor_scan(out=pend.ap, data0=onesr.ap, data1=padded.ap, initial=0.0, op0=ALU.mult, op1=ALU.add),
         [onesr.r, padded.r], [pend.r])
    p.tt("dve", pstart.ap, pend.ap, padded.ap, ALU.subtract, r=[pend.r, padded.r], w=[pstart.r])
    p.memset("pool", be_f.ap, 0.0, w=[be_f.r])
    for e_ in range(32):
        p.stt("dve", be_f.ap, RC.ap[:, 36:36 + NBP], pend.ap[:, e_:e_ + 1], be_f.ap, ALU.is_ge, ALU.add, r=[RC.r, pend.r, be_f.r], w=[be_f.r])
    p.ts("dve", be_f.ap, be_f.ap, 31.0, ALU.min, r=[be_f.r], w=[be_f.r])
    p.ts("dve", be_f.ap, be_f.ap, 128.0, ALU.mult, PC.ap[:, 0:1], ALU.add, r=[be_f.r, PC.r], w=[be_f.r])
    p.copy("dve", idxw.ap, be_f.ap, r=[be_f.r], w=[idxw.r])
    pe_ = rt("pe", [128, 32]); t32 = rt("t32", [128, 32]); dstk = rt("dstk", [128, 1])
    for ti in range(NTP):
        q = ti % 2
        p.tt("dve", pe_[q].ap, posall.ap[:, ti, :], pstart.ap, ALU.add, r=[posall.r, pstart.r], w=[pe_[q].r])
        for k in range(2):
            p.tt("dve", t32[q].ap, mkall.ap[:, ti, k, :], pe_[q].ap, ALU.mult, r=[mkall.r, pe_[q].r], w=[t32[q].r])
            p.op("dve", lambda e, o=dstk[q], i_=t32[q]: e.tensor_reduce(out=o.ap, in_=i_.ap, axis=AX.X, op=ALU.add), [t32[q].r], [dstk[q].r])
            p.copy("dve", desti.ap[:, ti, k:k + 1], dstk[q].ap, r=[dstk[q].r], w=[desti.r])
        F = ftok[q]
        p.dma("sp", F.ap, fd[ti * 128:(ti + 1) * 128, :], r=[fd_r], w=[F.r], key="fdr")
        for k in range(2):
            p.op("pool", lambda e, F=F, ti=ti, k=k: e.indirect_dma_start(
                out=xs[:, :], out_offset=bass.IndirectOffsetOnAxis(ap=desti.ap[:, ti, k:k + 1], axis=0),
                in_=F.ap[:, :], in_offset=None), [F.r, desti.r], [xs_r], dkey=("pool", id(F.r)))

    p.barrier()
    p.release(m0)
    W1 = [p.sb([128, NCH * 512], BF16, f"W1{i}") for i in range(2)]
    W3 = [p.sb([128, NCH * 512], BF16, f"W3{i}") for i in range(2)]
    W2 = [p.sb([128, 4 * D], BF16, f"W2{i}") for i in range(2)]
    XS = [p.sb([128, D], BF16, f"XS{i}") for i in range(2)]
    xsT = p.sb([128, NCH, 128], BF16, "xsT")
    hh = p.sb([128, 512], BF16, "hh")
    hT = p.sb([128, 4, 128], BF16, "hT")
    sil = p.sb([128, 512], F32, "sil")
    ysb = [p.sb([128, D], F32, f"ysb{i}") for i in range(2)]
    for b in range(NBP):
        q = b % 2
        for Wt, wsrc, key in ((W1[q], w1, "w1"), (W3[q], w3, "w3"), (W2[q], w2, "w2")):
            p.op("pool", lambda e, Wt=Wt, wsrc=wsrc, b=b: e.indirect_dma_start(
                out=Wt.ap[:, :], out_offset=None, in_=wsrc[:, :],
                in_offset=bass.IndirectOffsetOnAxis(ap=idxw.ap[:, b:b + 1], axis=0)), [idxw.r], [Wt.r], dkey=("pool", id(Wt.r)))
        p.dma("sp", XS[q].ap, xs[b * 128:(b + 1) * 128, :], r=[xs_r], w=[XS[q].r], key="xs")
        for half in range(2):
            tb = p.banks[half]
            tbv = tb.ap.bitcast(BF16)
            for k8 in range(8):
                kc = half * 8 + k8
                p.transpose(tbv[:, k8 * 128:(k8 + 1) * 128], XS[q].ap[:, kc * 128:(kc + 1) * 128], c["ident_bf"].ap,
                            r=[XS[q].r, c["ident_bf"].r], w=[tb.r])
            p.copy("act" if half == 0 else "dve", xsT.ap[:, half * 8:(half + 1) * 8, :],
                   tbv[:, 0:1024].rearrange("p (a b) -> p a b", a=8), r=[tb.r], w=[xsT.r])
        b1 = p.banks[2]
        b3 = p.banks[3]
        for kc in range(NCH):
            p.matmul(b1.ap[:, 0:512], xsT.ap[:, kc, :], W1[q].ap[:, kc * 512:(kc + 1) * 512], kc == 0, kc == NCH - 1,
                     r=[W1[q].r, xsT.r], w=[b1.r])
        for kc in range(NCH):
            p.matmul(b3.ap[:, 0:512], xsT.ap[:, kc, :], W3[q].ap[:, kc * 512:(kc + 1) * 512], kc == 0, kc == NCH - 1,
                     r=[W3[q].r, xsT.r], w=[b3.r])
        p.act(sil.ap, b1.ap[:, 0:512], AF.Silu, r=[b1.r], w=[sil.r])
        p.tt("dve", hh.ap, sil.ap, b3.ap[:, 0:512], ALU.mult, r=[sil.r, b3.r], w=[hh.r])
        tb = p.banks[4]
        tbv = tb.ap.bitcast(BF16)
        for hc in range(4):
            p.transpose(tbv[:, hc * 128:(hc + 1) * 128], hh.ap[:, hc * 128:(hc + 1) * 128], c["ident_bf"].ap,
                        r=[hh.r, c["ident_bf"].r], w=[tb.r])
        p.copy("act", hT.ap, tbv[:, 0:512].rearrange("p (a b) -> p a b", a=4), r=[tb.r], w=[hT.r])
        yb_ = ysb[q]
        for db in range(4):
            bank = p.banks[5 + db % 3]
            for hc in range(4):
                p.matmul(bank.ap[:, 0:512], hT.ap[:, hc, :], W2[q].ap[:, hc * D + db * 512: hc * D + (db + 1) * 512],
                         hc == 0, hc == 3, r=[hT.r, W2[q].r], w=[bank.r])
            p.copy("act" if db % 2 == 0 else "dve", yb_.ap[:, db * 512:(db + 1) * 512], bank.ap[:, 0:512], r=[bank.r], w=[yb_.r])
        p.dma("sp", ys[b * 128:(b + 1) * 128, :], yb_.ap, r=[yb_.r], w=[ys_r], key="ys")

    p.barrier()
    p.release(m0)
    R1 = [p.sb([128, D], F32, f"R1{i}") for i in range(2)]
    R2 = [p.sb([128, D], F32, f"R2{i}") for i in range(2)]
    YO = [p.sb([128, D], F32, f"YO{i}") for i in range(2)]
    outs = []
    for ti in range(NTP):
        q = ti % 2
        for k, Rk in ((0, R1[q]), (1, R2[q])):
            p.op("pool", lambda e, Rk=Rk, ti=ti, k=k: e.indirect_dma_start(
                out=Rk.ap[:, :], out_offset=None, in_=ys[:, :],
                in_offset=bass.IndirectOffsetOnAxis(ap=desti.ap[:, ti, k:k + 1], axis=0)),
                [ys_r, desti.r], [Rk.r], dkey=("pool", id(Rk.r)))
        p.ts("dve", YO[q].ap, R1[q].ap, gall.ap[:, ti, 0:1], ALU.mult, r=[R1[q].r, gall.r], w=[YO[q].r])
        p.stt("dve", YO[q].ap, R2[q].ap, gall.ap[:, ti, 1:2], YO[q].ap, ALU.mult, ALU.add, r=[R2[q].r, gall.r, YO[q].r], w=[YO[q].r])
        outs.append(p.dma("sp", ymoe[ti * 128:(ti + 1) * 128, :], YO[q].ap, r=[YO[q].r], key="out"))
    p.emit(final_waits=[xm_outs[-1], outs[-1]])
    return nc


def post_vec(gprev, mod, layer, inp):
    cols = [pp(gprev[0]), pp(gprev[1])]
    for cls in range(2):
        for k in range(6):
            cols.append(pp(mod[cls, k]))
    cols.append(pp(inp["norm_ffn_g"][layer]))
    v = np.concatenate(cols, axis=1).astype(np.float32)
    assert v.shape == (128, POST_NV)
    return v


def post_weights(inp, layer):
    w1 = np.ascontiguousarray(inp["expert_w1"][layer].reshape(32, NCH, 128, 512).transpose(0, 2, 1, 3)).reshape(32 * 128, NCH * 512)
    w3 = np.ascontiguousarray(inp["expert_w3"][layer].reshape(32, NCH, 128, 512).transpose(0, 2, 1, 3)).reshape(32 * 128, NCH * 512)
    w2 = np.ascontiguousarray(inp["expert_w2"][layer].reshape(32, 4, 128, D).transpose(0, 2, 1, 3)).reshape(32 * 128, 4 * D)
    wr = np.ascontiguousarray(np.concatenate([inp["router_group_w"][layer], inp["router_expert_w"][layer]], 1))
    rc = np.concatenate([inp["router_group_b"][layer], inp["router_expert_b"][layer],
                         np.arange(NBP, dtype=np.float32) * 128.0])[None].astype(np.float32)
    pcol = np.arange(128, dtype=np.float32).reshape(128, 1)
    return {"w1": w1, "w3": w3, "w2": w2, "wr": wr, "rc": rc, "pcol": pcol}


AT_NV = 242
AV_GPREV, AV_MOD, AV_GMIX, AV_SUBG, AV_PAD = 0, 32, 224, 240, 241
HD = 64
DIFF_SCALE = HD ** -0.5


def rope_tables():
    t = np.arange(S)
    row = (t // 64).astype(np.float32)
    col = (t % 64).astype(np.float32)
    nf = HD // 4
    inv = (10000.0 ** (-np.arange(nf, dtype=np.float32) / nf)).astype(np.float32)
    ang = np.concatenate([row[:, None] * inv, col[:, None] * inv], axis=-1).astype(np.float32)
    cos = np.cos(ang).astype(np.float32)
    sin = np.sin(ang).astype(np.float32)
    jj = np.arange(128) % 32
    cosT = np.ascontiguousarray(cos[:, jj].T)
    sinT = np.ascontiguousarray(sin[:, jj].T)
    R = np.zeros((128, 128), np.float32)
    for p_ in range(128):
        j = p_ % 64
        if j < 32:
            R[p_ + 32, p_] = -1.0
        else:
            R[p_ - 32, p_] = 1.0
    return cosT, sinT, R


def build_attn(lambda_init, dbg_heads=8, dbg_blocks=None, dbg_skip=()):
    nc = new_nc()
    p = Prog(nc)
    xa = nc.dram_tensor("xa", [D, TB], F32, kind="ExternalInput").ap()
    yb = nc.dram_tensor("yb", [D, TB], F32, kind="ExternalInput").ap()
    vec = nc.dram_tensor("vec", [128, AT_NV], F32, kind="ExternalInput").ap()
    lamv = nc.dram_tensor("lamv", [1, 256], F32, kind="ExternalInput").ap()
    wq = nc.dram_tensor("wq", [D, 1024], F32, kind="ExternalInput").ap()
    wk = nc.dram_tensor("wk", [D, 1024], F32, kind="ExternalInput").ap()
    wv = nc.dram_tensor("wv", [D, 1024], F32, kind="ExternalInput").ap()
    cosd = nc.dram_tensor("cosT", [128, S], F32, kind="ExternalInput").ap()
    sind = nc.dram_tensor("sinT", [128, S], F32, kind="ExternalInput").ap()
    Rd = nc.dram_tensor("R", [128, 128], F32, kind="ExternalInput").ap()
    gout = nc.dram_tensor("g", [1024, TB], BF16, kind="ExternalOutput").ap()
    qs = p.dram("qs", [8, 128, TB], BF16)
    ks = p.dram("ks", [8, 128, TB], BF16)
    vs = p.dram("vs", [TB, 1024], BF16)
    qs_r, ks_r, vs_r = Res("qs", multi=True), Res("ks", multi=True), Res("vs", multi=True)
    c = make_consts(p)
    V = p.sb([128, AT_NV], F32, "V")
    p.dma("sp", V.ap, vec, w=[V.r], key="in")
    gs = p.sb([128, 2, NCH], F32, "gs")
    for cls in range(2):
        sc = V.ap[:, AV_MOD + cls * 96 + 16: AV_MOD + cls * 96 + 32]
        p.stt("dve", gs.ap[:, cls, :], sc, 1.0, V.ap[:, AV_GMIX:AV_GMIX + 16], ALU.add, ALU.mult, r=[V.r], w=[gs.r])
    LV = p.sb([128, 256], F32, "LV")
    p.dma("sp", LV.ap, lamv.partition_broadcast(128), w=[LV.r], key="in")
    lt = p.sb([128, 2, 64], F32, "lt")
    le = p.sb([128, 2], F32, "le")
    nlam = p.sb([128, 1], F32, "nlam")
    gsub = p.sb([128, 1], F32, "gsub")
    for i in range(2):
        p.tt("dve", lt.ap[:, i, :], LV.ap[:, 128 * i:128 * i + 64], LV.ap[:, 128 * i + 64:128 * i + 128], ALU.mult, r=[LV.r], w=[lt.r])
        p.op("dve", lambda e, i=i: e.tensor_reduce(out=le.ap[:, i:i + 1], in_=lt.ap[:, i, :], axis=AX.X, op=ALU.add), [lt.r], [le.r])
    p.act(le.ap, le.ap, AF.Exp, r=[le.r], w=[le.r])
    p.tt("dve", nlam.ap, le.ap[:, 1:2], le.ap[:, 0:1], ALU.subtract, r=[le.r], w=[nlam.r])
    p.ts("dve", nlam.ap, nlam.ap, -float(lambda_init), ALU.add, r=[nlam.r], w=[nlam.r])
    p.ts("dve", gsub.ap, V.ap[:, AV_SUBG:AV_SUBG + 1], float(1.0 - lambda_init), ALU.mult, r=[V.r], w=[gsub.r])
    Rb = p.sb([128, 128], BF16, "Rb")
    p.dma("pool", Rb.ap, Rd, w=[Rb.r], key="R")
    m0 = p.mark()

    NB = 256
    Wq = p.sb([128, NCH, 1024], BF16, "Wq")
    Wk = p.sb([128, NCH, 1024], BF16, "Wk")
    Wv = p.sb([128, NCH, 1024], BF16, "Wv")
    load_w(p, Wq, wq, key="wq", nsplit=8)
    load_w(p, Wk, wk, key="wk", nsplit=8)
    load_w(p, Wv, wv, key="wv", nsplit=8)
    X = p.sb([128, NCH, NB], F32, "X")
    Y = p.sb([128, NCH, NB], F32, "Y")
    h = p.sb([128, NCH, NB], BF16, "h")
    sq = p.sb([128, NCH, NB], BF16, "sq")
    rstd = p.sb([128, NB], F32, "rstd")
    tmps = [p.sb([128, NB], F32, f"tmp{i}") for i in range(2)]
    qst = p.sb([128, 8, NB], BF16, "qst")
    kst = p.sb([128, 8, NB], BF16, "kst")
    vst = p.sb([128, NB // 128, 1024], BF16, "vst")
    cosb = p.sb([128, NB], F32, "cosb")
    sinb = p.sb([128, NB], F32, "sinb")
    qb16 = [p.sb([128, NB], BF16, f"qb{i}") for i in range(2)]
    t1 = [p.sb([128, NB], F32, f"t1{i}") for i in range(2)]
    t2 = [p.sb([128, NB], F32, f"t2{i}") for i in range(2)]
    it = 0
    for bi in (range(TB // NB) if dbg_blocks is None else dbg_blocks):
        c0 = bi * NB
        cls = 0 if bi == 0 else 1
        load_fm(p, "sp", X, xa, c0, NB, "xa")
        load_fm(p, "sp", Y, yb, c0, NB, "yb")
        if cls == 1:
            p.dma("sp", cosb.ap, cosd[:, c0 - CTX:c0 - CTX + NB], w=[cosb.r], key="cs")
            p.dma("sp", sinb.ap, sind[:, c0 - CTX:c0 - CTX + NB], w=[sinb.r], key="cs")
        for kc in range(NCH):
            p.stt("dve", X.ap[:, kc, :], Y.ap[:, kc, :], V.ap[:, AV_GPREV + cls * 16 + kc: AV_GPREV + cls * 16 + kc + 1],
                  X.ap[:, kc, :], ALU.mult, ALU.add, r=[X.r, Y.r, V.r], w=[X.r])
        emit_rstd(p, c, X, NB, sq, p.banks[0], rstd)
        sh = (V.ap[:, AV_MOD + cls * 96: AV_MOD + cls * 96 + 16], V.r)
        emit_mod(p, X, rstd, (gs.ap[:, cls, :], gs.r), sh, NB, h, tmps=tmps)
        for W, st in ((Wq, qst), (Wk, kst)):
            for hd in range(8):
                bank = p.banks[1 + it % 3]
                q_ = it % 2
                it += 1
                for kc in range(NCH):
                    p.matmul(bank.ap[:, 0:NB], W.ap[:, kc, hd * 128:(hd + 1) * 128], h.ap[:, kc, :], kc == 0, kc == NCH - 1,
                             r=[W.r, h.r], w=[bank.r])
                if cls == 0:
                    p.copy("act", st.ap[:, hd, :], bank.ap[:, 0:NB], r=[bank.r], w=[st.r])
                else:
                    rbk = p.banks[4 + q_]
                    p.copy("dve", qb16[q_].ap, bank.ap[:, 0:NB], r=[bank.r], w=[qb16[q_].r])
                    p.matmul(rbk.ap[:, 0:NB], Rb.ap, qb16[q_].ap, True, True, r=[Rb.r, qb16[q_].r], w=[rbk.r])
                    p.tt("dve", t1[q_].ap, bank.ap[:, 0:NB], cosb.ap, ALU.mult, r=[bank.r, cosb.r], w=[t1[q_].r])
                    p.tt("dve", t2[q_].ap, rbk.ap[:, 0:NB], sinb.ap, ALU.mult, r=[rbk.r, sinb.r], w=[t2[q_].r])
                    p.tt("pool", st.ap[:, hd, :], t1[q_].ap, t2[q_].ap, ALU.add, r=[t1[q_].r, t2[q_].r], w=[st.r])
        for tt_ in range(NB // 128):
            for nb in range(2):
                bank = p.banks[6 + nb]
                for kc in range(NCH):
                    p.matmul(bank.ap[:, 0:512], h.ap[:, kc, tt_ * 128:(tt_ + 1) * 128], Wv.ap[:, kc, nb * 512:(nb + 1) * 512],
                             kc == 0, kc == NCH - 1, r=[Wv.r, h.r], w=[bank.r])
                p.copy("act" if nb == 0 else "dve", vst.ap[:, tt_, nb * 512:(nb + 1) * 512], bank.ap[:, 0:512], r=[bank.r], w=[vst.r])
        p.dma("sp", qs[:, :, c0:c0 + NB].rearrange("h p t -> p h t"), qst.ap, r=[qst.r], w=[qs_r], key="qs")
        p.dma("sp", ks[:, :, c0:c0 + NB].rearrange("h p t -> p h t"), kst.ap, r=[kst.r], w=[ks_r], key="ks")
        p.dma("sp", vs[c0:c0 + NB, :].rearrange("(t p) v -> p t v", p=128), vst.ap, r=[vst.r], w=[vs_r], key="vs")

    p.barrier()
    p.release(m0)
    NKT = TB // 128
    qT = [p.sb([128, TB], BF16, f"qT{i}") for i in range(2)]
    kT = [p.sb([128, TB], BF16, f"kT{i}") for i in range(2)]
    Vt = [p.sb([128, NKT, 128], BF16, f"Vt{i}") for i in range(2)]
    Pt = [p.sb([128, 512], BF16, f"Pt{i}") for i in range(4)]
    ost = [p.sb([128, TB], BF16, f"ost{i}") for i in range(2)]
    r0 = p.sb([128, 512], F32, "r0")
    o0 = p.sb([128, 512], F32, "o0")
    o1 = p.sb([128, 512], F32, "o1")
    sqo = p.sb([128, 512], BF16, "sqo")
    rs = p.sb([128, 512], F32, "rs")
    qblocks = [(0, CTX, CTX // 128)] + [(CTX + 512 * i, 512, NKT) for i in range(S // 512)]
    outs = []
    pi = 0
    for hd in range(dbg_heads):
        q_ = hd % 2
        p.dma("sp", qT[q_].ap, qs[hd], r=[qs_r], w=[qT[q_].r], key="lq")
        p.dma("sp", kT[q_].ap, ks[hd], r=[ks_r], w=[kT[q_].r], key="lk")
        vsrc = vs[:, hd * 128:(hd + 1) * 128].rearrange("(kt p) v -> p kt v", p=128)
        half = NKT // 2
        grp = p.newgroup()
        p.dma("sp", Vt[q_].ap[:, 0:half, :], vsrc[:, 0:half, :], r=[vs_r], w=[Vt[q_].r], key="lv", group=grp)
        p.dma("sp", Vt[q_].ap[:, half:NKT, :], vsrc[:, half:NKT, :], r=[vs_r], w=[Vt[q_].r], key="lv", group=grp)
        O = ost[q_]
        for (q0, N, nkt) in qblocks:
            Ob = [p.banks[0], p.banks[1]]
            Db = [p.banks[2], p.banks[3]]
            for kt in range(nkt):
                for cc in range(2):
                    sbk = p.banks[4 + pi % 3]
                    P_ = Pt[pi % 4]
                    pi += 1
                    p.matmul(sbk.ap[:, 0:N], kT[q_].ap[cc * 64:(cc + 1) * 64, kt * 128:(kt + 1) * 128],
                             qT[q_].ap[cc * 64:(cc + 1) * 64, q0:q0 + N], True, True, r=[kT[q_].r, qT[q_].r], w=[sbk.r])
                    p.act(P_.ap[:, 0:N], sbk.ap[:, 0:N], AF.Exp, r=[sbk.r], w=[P_.r], scale=float(DIFF_SCALE))
                    p.matmul(Ob[cc].ap[:, 0:N], Vt[q_].ap[:, kt, :], P_.ap[:, 0:N], kt == 0, kt == nkt - 1, r=[Vt[q_].r, P_.r], w=[Ob[cc].r])
                    p.matmul(Db[cc].ap[:, 0:N], c["ones_bf"].ap, P_.ap[:, 0:N], kt == 0, kt == nkt - 1, r=[c["ones_bf"].r, P_.r], w=[Db[cc].r])
            p.op("dve", lambda e, N=N, Db=Db: e.reciprocal(out=r0.ap[:, 0:N], in_=Db[0].ap[:, 0:N]), [Db[0].r], [r0.r])
            p.tt("dve", o0.ap[:, 0:N], Ob[0].ap[:, 0:N], r0.ap[:, 0:N], ALU.mult, r=[Ob[0].r, r0.r], w=[o0.r])
            p.op("dve", lambda e, N=N, Db=Db: e.reciprocal(out=r0.ap[:, 0:N], in_=Db[1].ap[:, 0:N]), [Db[1].r], [r0.r])
            p.tt("dve", o1.ap[:, 0:N], Ob[1].ap[:, 0:N], r0.ap[:, 0:N], ALU.mult, r=[Ob[1].r, r0.r], w=[o1.r])
            p.stt("dve", o0.ap[:, 0:N], o1.ap[:, 0:N], nlam.ap[:, 0:1], o0.ap[:, 0:N], ALU.mult, ALU.add, r=[o1.r, nlam.r, o0.r], w=[o0.r])
            p.act(sqo.ap[:, 0:N], o0.ap[:, 0:N], AF.Square, r=[o0.r], w=[sqo.r])
            nbk = p.banks[7]
            p.matmul(nbk.ap[:, 0:N], c["ones_bf"].ap, sqo.ap[:, 0:N], True, True, r=[c["ones_bf"].r, sqo.r], w=[nbk.r])
            p.act(rs.ap[:, 0:N], nbk.ap[:, 0:N], AF.Sqrt, r=[nbk.r, c["eps"].r], w=[rs.r], bias=c["eps"].ap, scale=1.0 / 128.0)
            p.op("dve", lambda e, N=N: e.reciprocal(out=rs.ap[:, 0:N], in_=rs.ap[:, 0:N]), [rs.r], [rs.r])
            p.stt("dve", O.ap[:, q0:q0 + N], o0.ap[:, 0:N], gsub.ap[:, 0:1], rs.ap[:, 0:N], ALU.mult, ALU.mult, r=[o0.r, gsub.r, rs.r], w=[O.r])
        outs.append(p.dma("sp", gout[hd * 128:(hd + 1) * 128, :], O.ap, r=[O.r], key="out"))
    p.emit(final_waits=outs[-1:] if outs else [])
    return nc


def attn_vec(gprev, mod, layer, slot, inp):
    cols = [pp(gprev[0]), pp(gprev[1])]
    for cls in range(2):
        for k in range(6):
            cols.append(pp(mod[cls, k]))
    cols.append(pp(inp["norm_mix_g"][layer]))
    cols.append(np.asarray(inp["diff_subln_g"][slot]).reshape(128, 1))
    cols.append(np.zeros((128, 1), np.float32))
    v = np.concatenate(cols, axis=1).astype(np.float32)
    assert v.shape == (128, AT_NV)
    return v


GM_NV = 240 + 16
GV_GPREV, GV_MOD, GV_GMIX, GV_BS = 0, 32, 224, 240


def build_gmlp():
    nc = new_nc()
    p = Prog(nc)
    xa = nc.dram_tensor("xa", [D, TC], F32, kind="ExternalInput").ap()
    yb = nc.dram_tensor("yb", [D, TC], F32, kind="ExternalInput").ap()
    vec = nc.dram_tensor("vec", [128, GM_NV], F32, kind="ExternalInput").ap()
    w_uv = nc.dram_tensor("w_uv", [D, 2 * D], F32, kind="ExternalInput").ap()
    rows = nc.dram_tensor("rows", [3, 2 * D], F32, kind="ExternalInput").ap()
    wsT = nc.dram_tensor("wsT", [16, 128, 128], F32, kind="ExternalInput").ap()
    zout = nc.dram_tensor("g", [D, TC], BF16, kind="ExternalOutput").ap()
    c = make_consts(p)
    V = p.sb([128, GM_NV], F32, "V")
    p.dma("sp", V.ap, vec, w=[V.r], key="in")
    gs = p.sb([128, 2, NCH], F32, "gs")
    for cls in range(2):
        sc = V.ap[:, GV_MOD + cls * 96 + 16: GV_MOD + cls * 96 + 32]
        p.stt("dve", gs.ap[:, cls, :], sc, 1.0, V.ap[:, GV_GMIX:GV_GMIX + 16], ALU.add, ALU.mult, r=[V.r], w=[gs.r])
    buvb = p.sb([1, 2 * D], BF16, "buvb")
    p.dma("pool", buvb.ap, rows[0:1, :], w=[buvb.r], key="rows")
    lng = p.sb([128, D], F32, "lng")
    lnb = p.sb([128, D], F32, "lnb")
    p.dma("sp", lng.ap, rows[1:2, 0:D].partition_broadcast(128), w=[lng.r], key="in")
    p.dma("sp", lnb.ap, rows[2:3, 0:D].partition_broadcast(128), w=[lnb.r], key="in")
    ws = p.sb([128, 16, 128], BF16, "ws")
    p.dma("pool", ws.ap, wsT.rearrange("g q p -> q g p"), w=[ws.r], key="ws")
    Wuv = p.sb([128, NCH, 2 * D], BF16, "Wuv")
    load_w(p, Wuv, w_uv, key="w", nsplit=16)
    X = p.sb([128, NCH, 128], F32, "X")
    Y = p.sb([128, NCH, 128], F32, "Y")
    h = p.sb([128, NCH, 128], BF16, "h")
    sq = p.sb([128, NCH, 128], BF16, "sq")
    rstd = p.sb([128, 128], F32, "rstd")
    tmps = [p.sb([128, 128], F32, f"tmp{i}") for i in range(2)]
    u = p.sb([128, D], BF16, "u")
    v = p.sb([128, D], F32, "v")
    vln = p.sb([128, D], BF16, "vln")
    z = p.sb([128, D], BF16, "z")
    zst = sq
    st1 = p.sb([128, 1], F32, "st1")
    st2 = p.sb([128, 1], F32, "st2")
    outs = []
    for ti in range(NT):
        c0 = ti * 128
        cls = 0 if ti == 0 else 1
        load_fm(p, "sp", X, xa, c0, 128, "xa")
        load_fm(p, "sp", Y, yb, c0, 128, "yb")
        for kc in range(NCH):
            p.stt("dve", X.ap[:, kc, :], Y.ap[:, kc, :], V.ap[:, GV_GPREV + cls * 16 + kc: GV_GPREV + cls * 16 + kc + 1],
                  X.ap[:, kc, :], ALU.mult, ALU.add, r=[X.r, Y.r, V.r], w=[X.r])
        emit_rstd(p, c, X, 128, sq, p.banks[0], rstd)
        sh = (V.ap[:, GV_MOD + cls * 96: GV_MOD + cls * 96 + 16], V.r)
        emit_mod(p, X, rstd, (gs.ap[:, cls, :], gs.r), sh, 128, h, tmps=tmps)
        for cb in range(8):
            bank = p.banks[1 + cb % 3]
            for kc in range(NCH):
                p.matmul(bank.ap[:, 0:512], h.ap[:, kc, :], Wuv.ap[:, kc, cb * 512:(cb + 1) * 512], kc == 0, False,
                         r=[h.r, Wuv.r], w=[bank.r])
            p.matmul(bank.ap[:, 0:512], c["ones_bf"].ap[0:1, :], buvb.ap[0:1, cb * 512:(cb + 1) * 512], False, True,
                     r=[c["ones_bf"].r, buvb.r], w=[bank.r])
            if cb < 4:
                p.act(u.ap[:, cb * 512:(cb + 1) * 512], bank.ap[:, 0:512], AF.Gelu_apprx_tanh, r=[bank.r], w=[u.r])
            else:
                p.act(v.ap[:, (cb - 4) * 512:(cb - 3) * 512], bank.ap[:, 0:512], AF.Gelu_apprx_tanh, r=[bank.r], w=[v.r])
        p.op("dve", lambda e: e.tensor_reduce(out=st1.ap, in_=v.ap, axis=AX.X, op=ALU.add), [v.r], [st1.r])
        p.ts("dve", st1.ap, st1.ap, 1.0 / D, ALU.mult, r=[st1.r], w=[st1.r])
        p.ts("dve", v.ap, v.ap, st1.ap[:, 0:1], ALU.subtract, r=[v.r, st1.r], w=[v.r])
        p.tt("dve", z.ap, v.ap, v.ap, ALU.mult, r=[v.r], w=[z.r])
        p.op("dve", lambda e: e.tensor_reduce(out=st2.ap, in_=z.ap, axis=AX.X, op=ALU.add), [z.r], [st2.r])
        p.act(st2.ap, st2.ap, AF.Sqrt, r=[st2.r, c["eps"].r], w=[st2.r], bias=c["eps"].ap, scale=1.0 / D)
        p.op("dve", lambda e: e.reciprocal(out=st2.ap, in_=st2.ap), [st2.r], [st2.r])
        p.stt("dve", v.ap, v.ap, st2.ap[:, 0:1], lng.ap, ALU.mult, ALU.mult, r=[v.r, st2.r, lng.r], w=[v.r])
        p.tt("dve", vln.ap, v.ap, lnb.ap, ALU.add, r=[v.r, lnb.r], w=[vln.r])
        for g in range(16):
            bank = p.banks[4 + (g // 4) % 2]
            j = g % 4
            p.matmul(bank.ap[:, j * 128:(j + 1) * 128], ws.ap[:, g, :], vln.ap[:, g * 128:(g + 1) * 128], True, True,
                     r=[ws.r, vln.r], w=[bank.r])
            p.stt("dve", z.ap[:, g * 128:(g + 1) * 128], bank.ap[:, j * 128:(j + 1) * 128], V.ap[:, GV_BS + g: GV_BS + g + 1],
                  u.ap[:, g * 128:(g + 1) * 128], ALU.add, ALU.mult, r=[bank.r, V.r, u.r], w=[z.r])
        for half in range(2):
            tb = p.banks[6 + half]
            tbv = tb.ap.bitcast(BF16)
            for k8 in range(8):
                kc = half * 8 + k8
                p.transpose(tbv[:, k8 * 128:(k8 + 1) * 128], z.ap[:, kc * 128:(kc + 1) * 128], c["ident_bf"].ap,
                            r=[z.r, c["ident_bf"].r], w=[tb.r])
            p.copy("act", zst.ap[:, half * 8:(half + 1) * 8, :], tbv[:, 0:1024].rearrange("p (a b) -> p a b", a=8), r=[tb.r], w=[zst.r])
        outs.append(store_fm(p, "sp", zout, c0, 128, zst, "out"))
    p.emit(final_waits=outs[-1:])
    return nc


def gmlp_vec(gprev, mod, layer, slot, inp):
    cols = [pp(gprev[0]), pp(gprev[1])]
    for cls in range(2):
        for k in range(6):
            cols.append(pp(mod[cls, k]))
    cols.append(pp(inp["norm_mix_g"][layer]))
    cols.append(np.ascontiguousarray(np.asarray(inp["gmlp_b_s"][slot]).T))
    v = np.concatenate(cols, axis=1).astype(np.float32)
    assert v.shape == (128, GM_NV)
    return v


def gmlp_rows(slot, inp):
    r = np.zeros((3, 2 * D), np.float32)
    r[0] = inp["gmlp_b_uv"][slot]
    r[1, :D] = inp["gmlp_ln_g"][slot]
    r[2, :D] = inp["gmlp_ln_b"][slot]
    return r


TF = S // 2


def build_final():
    nc = new_nc()
    p = Prog(nc)
    xa = nc.dram_tensor("xa", [D, TF], F32, kind="ExternalInput").ap()
    yb = nc.dram_tensor("yb", [D, TF], F32, kind="ExternalInput").ap()
    vec = nc.dram_tensor("vec", [128, 48], F32, kind="ExternalInput").ap()
    out = nc.dram_tensor("out", [D, TF], F32, kind="ExternalOutput").ap()
    c = make_consts(p)
    V = p.sb([128, 48], F32, "V")
    p.dma("sp", V.ap, vec, w=[V.r], key="in")
    NB = 256
    Xb = [p.sb([128, NCH, NB], F32, f"X{i}") for i in range(2)]
    Yb = [p.sb([128, NCH, NB], F32, f"Y{i}") for i in range(2)]
    sq = p.sb([128, NCH, NB], BF16, "sq")
    rstd = p.sb([128, NB], F32, "rstd")
    outs = []
    for bi in range(TF // NB):
        c0 = bi * NB
        X = Xb[bi % 2]
        Y = Yb[bi % 2]
        load_fm(p, "sp", X, xa, c0, NB, "xa")
        load_fm(p, "sp", Y, yb, c0, NB, "yb")
        for kc in range(NCH):
            p.stt("dve", X.ap[:, kc, :], Y.ap[:, kc, :], V.ap[:, kc:kc + 1], X.ap[:, kc, :], ALU.mult, ALU.add,
                  r=[X.r, Y.r, V.r], w=[X.r])
        emit_rstd(p, c, X, NB, sq, p.banks[bi % 2], rstd)
        emit_mod(p, X, rstd, (V.ap[:, 16:32], V.r), (V.ap[:, 32:48], V.r), NB, None, out_f32=Y)
        outs.append(store_fm(p, "sp", out, c0, NB, Y, "out"))
    p.emit(final_waits=outs[-1:])
    return nc


_PROGS = {}


def _prog(name, fn):
    if name not in _PROGS:
        _PROGS[name] = fn()
    return _PROGS[name]


def _run(nc, maps):
    res = run_bass_kernel_spmd(nc, maps, core_ids=list(range(len(maps))))
    return res.results


def _core_cols(hf):
    return np.r_[hf * 128:(hf + 1) * 128, CTX + hf * (S // 2): CTX + (hf + 1) * (S // 2)]


def kernel(**inputs):
    inp = {k: np.asarray(v) for k, v in inputs.items()}
    f32 = np.float32
    c5 = np.concatenate([inp["c"], inp["c_ctx"][None]], 0).astype(f32)
    cT = np.ascontiguousarray(c5.T.reshape(NCH, 128, 5).transpose(1, 0, 2))
    maps = [{"cT": cT,
             "w": np.ascontiguousarray(inp["ada_w"][:, :, j * ADA_COLS:(j + 1) * ADA_COLS]),
             "b": np.ascontiguousarray(inp["ada_b"][:, None, j * ADA_COLS:(j + 1) * ADA_COLS])} for j in range(8)]
    r = _run(_prog("ada", build_ada), maps)
    mod = np.concatenate([x["mod"] for x in r], axis=2)

    def modb(layer, b):
        return np.stack([mod[layer, 4].reshape(6, D), mod[layer, b].reshape(6, D)], 0)

    xa = [np.ascontiguousarray(np.concatenate([inp["ctx"][b], inp["x"][b]], 0).T.astype(f32)) for b in range(B)]
    yb = [np.zeros((D, TB), f32) for _ in range(B)]
    gprev = [np.zeros((2, D), f32) for _ in range(B)]
    cosT, sinT, Rm = rope_tables()
    for layer in range(DEPTH):
        kind, slot = layer % 3, layer // 3
        maps = []
        for core in range(8):
            b, hf = core // 2, core % 2
            m = modb(layer, b)
            if kind == 0:
                w = inp["lru_w_in"][slot]
                maps.append({"xa": xa[b], "yb": yb[b], "vec": lru_vec(gprev[b], m, layer, slot, hf, inp),
                             "w_in": np.ascontiguousarray(np.concatenate([w[:, hf * 1024:(hf + 1) * 1024],
                                                                          w[:, D + hf * 1024:D + (hf + 1) * 1024]], 1)),
                             "gw": np.ascontiguousarray(inp["lru_gate_w"][slot][:, :, hf * 8:(hf + 1) * 8])})
            elif kind == 1:
                wqkv = inp["diff_w_qkv"][slot]
                sl = slice(hf * 1024, (hf + 1) * 1024)
                maps.append({"xa": xa[b], "yb": yb[b], "vec": attn_vec(gprev[b], m, layer, slot, inp),
                             "lamv": np.ascontiguousarray(inp["diff_lambda"][slot].reshape(1, 256)),
                             "wq": np.ascontiguousarray(wqkv[:, 0:D][:, sl]),
                             "wk": np.ascontiguousarray(wqkv[:, D:2 * D][:, sl]),
                             "wv": np.ascontiguousarray(wqkv[:, 2 * D:3 * D][:, sl]),
                             "cosT": cosT, "sinT": sinT, "R": Rm})
            else:
                cols = _core_cols(hf)
                maps.append({"xa": np.ascontiguousarray(xa[b][:, cols]), "yb": np.ascontiguousarray(yb[b][:, cols]),
                             "vec": gmlp_vec(gprev[b], m, layer, slot, inp), "w_uv": inp["gmlp_w_uv"][slot],
                             "rows": gmlp_rows(slot, inp),
                             "wsT": np.ascontiguousarray(inp["gmlp_w_s"][slot].transpose(0, 2, 1))})
        if kind == 0:
            r = _run(_prog("lru", build_lru), maps)
            G = [np.concatenate([r[2 * b]["g"], r[2 * b + 1]["g"]], 0) for b in range(B)]
            w_out = inp["lru_w_out"][slot]
        elif kind == 1:
            li = 0.8 - 0.6 * math.exp(-0.3 * layer)
            r = _run(_prog("attn", lambda: build_attn(li)), maps)
            G = [np.concatenate([r[2 * b]["g"], r[2 * b + 1]["g"]], 0) for b in range(B)]
            w_out = inp["diff_w_out"][slot]
        else:
            r = _run(_prog("gmlp", build_gmlp), maps)
            G = []
            for b in range(B):
                g = np.empty((D, TB), dtype=r[0]["g"].dtype)
                for hf in range(2):
                    g[:, _core_cols(hf)] = r[2 * b + hf]["g"]
                G.append(g)
            w_out = inp["gmlp_w_out"][slot]
        del maps
        pw = post_weights(inp, layer)
        maps = [{"xa": xa[b], "yb": yb[b], "G": np.ascontiguousarray(G[b]),
                 "vec": post_vec(gprev[b], modb(layer, b), layer, inp), "w_out": w_out, **pw} for b in range(B)]
        r = _run(_prog("post", build_post), maps)
        del maps, pw
        for b in range(B):
            xa[b] = np.ascontiguousarray(r[b]["xmid"])
            yb[b] = np.ascontiguousarray(r[b]["ymoe"].T)
            m = modb(layer, b)
            gprev[b] = np.stack([m[0, 5], m[1, 5]], 0)
    maps = []
    for core in range(8):
        b, hf = core // 2, core % 2
        sl = slice(CTX + hf * TF, CTX + (hf + 1) * TF)
        vec = np.concatenate([pp(gprev[b][1]), pp(inp["norm_final_g"]), np.zeros((128, 16), f32)], 1).astype(f32)
        maps.append({"xa": np.ascontiguousarray(xa[b][:, sl]), "yb": np.ascontiguousarray(yb[b][:, sl]), "vec": vec})
    r = _run(_prog("final", build_final), maps)
    out = np.empty((B, S, D), f32)
    for core in range(8):
        b, hf = core // 2, core % 2
        out[b, hf * TF:(hf + 1) * TF, :] = r[core]["out"].T
    return out
```

```python
import contextlib
import math
import numpy as np
import concourse.bass as bass
import concourse.mybir as mybir
from concourse.bass_utils import run_bass_kernel_spmd

F32 = mybir.dt.float32
BF16 = mybir.dt.bfloat16
I32 = mybir.dt.int32
AF = mybir.ActivationFunctionType
ALU = mybir.AluOpType
AX = mybir.AxisListType

D = 2048
NCH = 16
B = 4
S = 4096
CTX = 256
DEPTH = 4
EPS = 1e-6


class Res:
    __slots__ = ("name", "ws", "rs", "excl", "multi")

    def __init__(self, name="", excl=False, multi=False):
        self.name = name
        self.ws = {}
        self.rs = {}
        self.excl = excl
        self.multi = multi


class Ins:
    __slots__ = ("eng", "fn", "deps", "needs_inc", "semval", "dkey", "idx", "group")

    def __init__(self, eng, fn, dkey=None):
        self.idx = 0
        self.group = None
        self.eng = eng
        self.fn = fn
        self.deps = []
        self.needs_inc = False
        self.semval = None
        self.dkey = dkey


class Buf:
    __slots__ = ("ap", "r")

    def __init__(self, ap, name=""):
        self.ap = ap
        self.r = Res(name)

    def __getitem__(self, k):
        return self.ap[k]


_DSZ = {F32: 4, BF16: 2, I32: 4}
ARENA_BYTES = 204 * 1024


class Prog:
    ENGS = ("pe", "act", "dve", "pool", "sp")

    def __init__(self, nc):
        self.nc = nc
        self.lists = {e: [] for e in self.ENGS}
        self.last = {}
        self.ngroup = 0
        self.stack = contextlib.ExitStack()
        self.arena = self.stack.enter_context(nc.sbuf_tensor("arena", [128, ARENA_BYTES // 4], F32))
        self.off = 0
        self.banks = []
        for i in range(8):
            t = self.stack.enter_context(nc.psum_tensor(f"bank{i}", [128, 512], F32))
            bk = Buf(t[:], f"bank{i}")
            bk.r.excl = True
            self.banks.append(bk)

    def sb(self, shape, dtype, name=""):
        n = 1
        for v in shape[1:]:
            n *= v
        nbytes = (n * _DSZ[dtype] + 63) // 64 * 64
        w = nbytes // 4
        assert self.off + w <= ARENA_BYTES // 4, f"SBUF arena overflow allocating {name} {shape}"
        v = self.arena[0:shape[0], self.off:self.off + w]
        self.off += w
        if dtype != F32:
            v = v.bitcast(dtype)
        v = v[:, 0:n]
        if len(shape) > 2:
            names = " ".join(f"d{i}" for i in range(len(shape) - 1))
            kw = {f"d{i}": shape[i + 1] for i in range(len(shape) - 2)}
            v = v.rearrange(f"p ({names}) -> p {names}", **kw)
        return Buf(v, name)

    def newgroup(self):
        self.ngroup += 1
        return self.ngroup

    def mark(self):
        return self.off

    def release(self, m):
        self.off = m

    def dram(self, name, shape, dtype, kind="Internal"):
        return self.nc.dram_tensor(name, list(shape), dtype, kind=kind).ap()

    def op(self, eng, fn, reads=(), writes=(), dkey=None, group=None):
        ins = Ins(eng, fn, dkey)
        ins.idx = len(self.lists[eng])
        ins.group = group
        deps = {}

        def add(d):
            if d is None or d is ins:
                return
            if d.eng == "pe" and eng == "pe" and d.dkey is None and dkey is None:
                return
            if group is not None and d.group == group:
                return
            key = (d.eng, d.dkey)
            o = deps.get(key)
            if o is None or o.idx < d.idx:
                deps[key] = d

        me = (eng, dkey)
        for r in reads:
            for d in r.ws.values():
                add(d)
            if r.excl:
                for k_, d in r.rs.items():
                    if k_ != me:
                        add(d)
        for w in writes:
            for d in w.rs.values():
                add(d)
            if not w.multi:
                for d in w.ws.values():
                    add(d)
        ins.deps = list(deps.values())
        for d in ins.deps:
            d.needs_inc = True
        for r in reads:
            r.rs[me] = ins
        for w in writes:
            if w.multi:
                w.ws[me] = ins
            else:
                w.ws = {me: ins}
            w.rs = {}
        self.lists[eng].append(ins)
        self.last[me] = ins
        return ins

    def barrier(self):
        lasts = list(self.last.values())
        for e in self.ENGS:
            ins = Ins(e, lambda eng: eng.nop(), None)
            ins.idx = len(self.lists[e])
            ins.deps = [l for l in lasts if not (l.eng == e and l.dkey is None)]
            for d in ins.deps:
                d.needs_inc = True
            self.lists[e].append(ins)
            self.last[(e, None)] = ins

    def dma(self, q, out, in_, r=(), w=(), key=None, group=None, **kw):
        side = None
        for x in list(w) + list(r):
            if not x.multi:
                side = x
                break
        dk = (q, id(side) if side is not None else key)
        return self.op(q, lambda e: e.dma_start(out=out, in_=in_, **kw), r, w, dkey=dk, group=group)

    def matmul(self, out, lhsT, rhs, start, stop, r=(), w=()):
        return self.op("pe", lambda e: e.matmul(out, lhsT, rhs, start=start, stop=stop), r, w)

    def transpose(self, out, in_, ident, r=(), w=()):
        return self.op("pe", lambda e: e.transpose(out, in_, ident), r, w)

    def act(self, out, in_, func, r=(), w=(), bias=None, scale=None, eng="act"):
        kw = {}
        if bias is not None:
            kw["bias"] = bias
        if scale is not None:
            kw["scale"] = scale
        return self.op(eng, lambda e: e.activation(out=out, in_=in_, func=func, **kw), r, w)

    def tt(self, eng, out, in0, in1, op, r=(), w=()):
        return self.op(eng, lambda e: e.tensor_tensor(out=out, in0=in0, in1=in1, op=op), r, w)

    def ts(self, eng, out, in0, s1, op0, s2=None, op1=None, r=(), w=()):
        if op1 is None:
            return self.op(eng, lambda e: e.tensor_scalar(out=out, in0=in0, scalar1=s1, scalar2=None, op0=op0), r, w)
        return self.op(eng, lambda e: e.tensor_scalar(out=out, in0=in0, scalar1=s1, scalar2=s2, op0=op0, op1=op1), r, w)

    def stt(self, eng, out, in0, scalar, in1, op0, op1, r=(), w=()):
        return self.op(eng, lambda e: e.scalar_tensor_tensor(out=out, in0=in0, scalar=scalar, in1=in1, op0=op0, op1=op1), r, w)

    def copy(self, eng, out, in_, r=(), w=()):
        if eng == "act":
            return self.op(eng, lambda e: e.copy(out=out, in_=in_), r, w)
        return self.op(eng, lambda e: e.tensor_copy(out=out, in_=in_), r, w)

    def memset(self, eng, out, val, w=()):
        return self.op(eng, lambda e: e.memset(out, val), (), w)

    def emit(self, final_waits=()):
        nc = self.nc
        final_waits = [ins for k, ins in self.last.items() if k[1] is not None]
        for ins in final_waits:
            ins.needs_inc = True
        esem = {}
        dsem = {}
        for e in self.ENGS:
            cnt = 0
            dcnt = {}
            for ins in self.lists[e]:
                if ins.dkey is not None:
                    dcnt[ins.dkey] = dcnt.get(ins.dkey, 0) + 16
                    ins.semval = dcnt[ins.dkey]
                    dsem.setdefault(ins.dkey, None)
                elif ins.needs_inc:
                    cnt += 1
                    ins.semval = cnt
            esem[e] = None
        for e in self.ENGS:
            esem[e] = self.stack.enter_context(nc.semaphore(f"s_{e}"))
        for i, k in enumerate(dsem):
            dsem[k] = self.stack.enter_context(nc.semaphore(f"d_{i}"))

        def sem_of(ins):
            return dsem[ins.dkey] if ins.dkey is not None else esem[ins.eng]

        def run(e, eng):
            waited = {}
            for ins in self.lists[e]:
                for d in ins.deps:
                    s = sem_of(d)
                    k = id(s)
                    if waited.get(k, 0) < d.semval:
                        eng.wait_ge(s, d.semval)
                        waited[k] = d.semval
                bi = ins.fn(eng)
                if ins.dkey is not None:
                    bi.then_inc(dsem[ins.dkey], 16)
                elif ins.needs_inc:
                    bi.then_inc(esem[e], 1)
            if e == "sp":
                for d in final_waits:
                    s = sem_of(d)
                    if waited.get(id(s), 0) < d.semval:
                        eng.wait_ge(s, d.semval)
                        waited[id(s)] = d.semval

        with nc.Block() as block:
            @block.tensor
            def _(eng):
                run("pe", eng)

            @block.scalar
            def _(eng):
                run("act", eng)

            @block.vector
            def _(eng):
                run("dve", eng)

            @block.gpsimd
            def _(eng):
                run("pool", eng)

            @block.sync
            def _(eng):
                run("sp", eng)
        self.stack.close()


def new_nc():
    return bass.Bass("TRN2", target_bir_lowering=False)


def make_consts(p):
    c = {}
    c["ones_bf"] = p.sb([128, 128], BF16, "ones_bf")
    p.memset("pool", c["ones_bf"].ap, 1.0, w=[c["ones_bf"].r])
    c["eps"] = p.sb([128, 1], F32, "eps")
    p.memset("pool", c["eps"].ap, EPS, w=[c["eps"].r])
    idf = p.sb([128, 128], F32, "ident_f")
    p.memset("pool", idf.ap, 0.0, w=[idf.r])
    p.op("pool", lambda e: e.affine_select(out=idf.ap, in_=idf.ap, pattern=[[-1, 128]], compare_op=ALU.not_equal,
                                           fill=1.0, base=0, channel_multiplier=1), [idf.r], [idf.r])
    c["ident_f"] = idf
    c["ident_bf"] = p.sb([128, 128], BF16, "ident_bf")
    p.copy("pool", c["ident_bf"].ap, idf.ap, r=[idf.r], w=[c["ident_bf"].r])
    return c


def emit_rstd(p, c, x, N, sq, bank, rstd):
    p.act(sq.ap[:, :, 0:N], x.ap[:, :, 0:N], AF.Square, r=[x.r], w=[sq.r])
    for kc in range(NCH):
        p.matmul(bank.ap[:, 0:N], c["ones_bf"].ap, sq.ap[:, kc, 0:N], kc == 0, kc == NCH - 1,
                 r=[c["ones_bf"].r, sq.r], w=[bank.r])
    p.act(rstd.ap[:, 0:N], bank.ap[:, 0:N], AF.Sqrt, r=[bank.r, c["eps"].r], w=[rstd.r], bias=c["eps"].ap, scale=1.0 / D)
    p.op("dve", lambda e: e.reciprocal(out=rstd.ap[:, 0:N], in_=rstd.ap[:, 0:N]), [rstd.r], [rstd.r])


def emit_mod(p, x, rstd, gs, sh, N, out_bf, tmps=None, out_f32=None):
    gs_ap, gs_r = gs
    sh_ap, sh_r = sh
    for cch in range(NCH):
        if out_f32 is not None:
            dst = out_f32.ap[:, cch, 0:N]
            dr = out_f32.r
        else:
            t = tmps[cch % len(tmps)]
            dst = t.ap[:, 0:N]
            dr = t.r
        p.stt("dve", dst, x.ap[:, cch, 0:N], gs_ap[:, cch:cch + 1], rstd.ap[:, 0:N], ALU.mult, ALU.mult,
              r=[x.r, gs_r, rstd.r], w=[dr])
        if out_f32 is not None:
            p.act(dst, dst, AF.Identity, r=[dr, sh_r], w=[dr], bias=sh_ap[:, cch:cch + 1])
        else:
            p.act(out_bf.ap[:, cch, 0:N], dst, AF.Identity, r=[dr, sh_r], w=[out_bf.r], bias=sh_ap[:, cch:cch + 1])
    if out_f32 is not None and out_bf is not None:
        p.copy("pool", out_bf.ap[:, :, 0:N], out_f32.ap[:, :, 0:N], r=[out_f32.r], w=[out_bf.r])


def load_w(p, dst, w2d, key, nsplit=4, q="pool"):
    K = w2d.shape[0]
    kc = K // 128
    src = w2d.rearrange("(kc p) n -> p kc n", p=128)
    step = max(1, kc // nsplit)
    grp = p.newgroup()
    for k0 in range(0, kc, step):
        p.dma(q, dst.ap[:, k0:k0 + step, :], src[:, k0:k0 + step, :], w=[dst.r], key=key, group=grp)


ADA_COLS = 6 * D // 8


def build_ada():
    nc = new_nc()
    p = Prog(nc)
    cT = nc.dram_tensor("cT", [128, NCH, 5], F32, kind="ExternalInput").ap()
    w = nc.dram_tensor("w", [DEPTH, D, ADA_COLS], F32, kind="ExternalInput").ap()
    bia = nc.dram_tensor("b", [DEPTH, 1, ADA_COLS], F32, kind="ExternalInput").ap()
    out = nc.dram_tensor("mod", [DEPTH, 5, ADA_COLS], F32, kind="ExternalOutput").ap()
    s = p.sb([128, NCH, 5], F32, "s")
    p.dma("sp", s.ap, cT, w=[s.r], key="in")
    p.act(s.ap, s.ap, AF.Silu, r=[s.r], w=[s.r])
    wb = [p.sb([128, NCH, 512], F32, f"wb{i}") for i in range(2)]
    bb = [p.sb([5, 512], F32, f"bb{i}") for i in range(2)]
    ob = [p.sb([5, 512], F32, f"ob{i}") for i in range(2)]
    outs = []
    it = 0
    for l in range(DEPTH):
        for nb in range(ADA_COLS // 512):
            W = wb[it % 2]
            bt = bb[it % 2]
            o = ob[it % 2]
            bank = p.banks[it % 2]
            src = w[l, :, nb * 512:(nb + 1) * 512].rearrange("(kc p) n -> p kc n", p=128)
            grp = p.newgroup()
            for k0 in range(0, NCH, 4):
                p.dma("sp", W.ap[:, k0:k0 + 4, :], src[:, k0:k0 + 4, :], w=[W.r], key="w", group=grp)
            p.dma("sp", bt.ap, bia[l, :, nb * 512:(nb + 1) * 512].partition_broadcast(5), w=[bt.r], key="b")
            for kc in range(NCH):
                p.matmul(bank.ap[0:5, :], s.ap[:, kc, :], W.ap[:, kc, :], kc == 0, kc == NCH - 1, r=[s.r, W.r], w=[bank.r])
            p.tt("dve", o.ap, bank.ap[0:5, :], bt.ap, ALU.add, r=[bank.r, bt.r], w=[o.r])
            outs.append(p.dma("sp", out[l, :, nb * 512:(nb + 1) * 512], o.ap, r=[o.r], key="out"))
            it += 1
    p.emit(final_waits=outs[-1:])
    return nc


def pp(v):
    v = np.asarray(v)
    return np.ascontiguousarray(v.reshape(-1, 128).T)


def fm_src(x2d, c0, n):
    return x2d[:, c0:c0 + n].rearrange("(kc p) n -> p kc n", p=128)


def load_fm(p, q, dst, x2d, c0, n, key, nsplit=2):
    src = fm_src(x2d, c0, n)
    step = NCH // nsplit
    grp = p.newgroup()
    for k0 in range(0, NCH, step):
        p.dma(q, dst.ap[:, k0:k0 + step, 0:n], src[:, k0:k0 + step, :], w=[dst.r], key=key, group=grp)


def store_fm(p, q, x2d, c0, n, src, key, nsplit=2):
    dst = fm_src(x2d, c0, n)
    step = NCH // nsplit
    out = None
    grp = p.newgroup()
    for k0 in range(0, NCH, step):
        out = p.dma(q, dst[:, k0:k0 + step, :], src.ap[:, k0:k0 + step, 0:n], r=[src.r], key=key, group=grp)
    return out


TB = CTX + S
LRU_NV = 328
LV_GPREV, LV_MOD, LV_GMIX, LV_CW, LV_CB, LV_GB, LV_LAM = 0, 32, 224, 240, 272, 280, 312


def build_lru():
    nc = new_nc()
    p = Prog(nc)
    xa = nc.dram_tensor("xa", [D, TB], F32, kind="ExternalInput").ap()
    yb = nc.dram_tensor("yb", [D, TB], F32, kind="ExternalInput").ap()
    vec = nc.dram_tensor("vec", [128, LRU_NV], F32, kind="ExternalInput").ap()
    w_in = nc.dram_tensor("w_in", [D, 2048], F32, kind="ExternalInput").ap()
    gw = nc.dram_tensor("gw", [2, 2, 8, 128, 128], F32, kind="ExternalInput").ap()
    gout = nc.dram_tensor("g", [1024, TB], BF16, kind="ExternalOutput").ap()
    scr = p.dram("scr", [16, 128, TB], F32)
    scr_r = Res("scr", multi=True)
    c = make_consts(p)
    V = p.sb([128, LRU_NV], F32, "V")
    p.dma("sp", V.ap, vec, w=[V.r], key="in")
    one = p.sb([128, 1], F32, "one")
    p.memset("pool", one.ap, 1.0, w=[one.r])
    gs = p.sb([128, 2, NCH], F32, "gs")
    for cls in range(2):
        sc = V.ap[:, LV_MOD + cls * 96 + 16: LV_MOD + cls * 96 + 32]
        p.stt("dve", gs.ap[:, cls, :], sc, 1.0, V.ap[:, LV_GMIX:LV_GMIX + 16], ALU.add, ALU.mult, r=[V.r], w=[gs.r])
    ca = p.sb([128, 16], F32, "ca")
    c2 = p.sb([128, 16], F32, "c2")
    p.act(ca.ap, V.ap[:, LV_LAM:LV_LAM + 16], AF.Exp, r=[V.r], w=[ca.r], scale=-1.0)
    p.act(ca.ap, ca.ap, AF.Ln, r=[ca.r, one.r], w=[ca.r], bias=one.ap)
    p.ts("dve", c2.ap, ca.ap, -16.0, ALU.mult, r=[ca.r], w=[c2.r])
    p.ts("dve", ca.ap, ca.ap, -8.0, ALU.mult, r=[ca.r], w=[ca.r])
    gw_sb = p.sb([128, 32, 128], BF16, "gw")
    p.dma("pool", gw_sb.ap, gw.rearrange("d r h i o -> i (d r h) o"), w=[gw_sb.r], key="gw")
    m0 = p.mark()

    NB = 256
    w_sb = p.sb([128, NCH, 2048], BF16, "w_in")
    load_w(p, w_sb, w_in, key="w", nsplit=8)
    xab = [p.sb([128, NCH, NB], F32, f"xa{i}") for i in range(2)]
    ybb = [p.sb([128, NCH, NB], F32, f"yb{i}") for i in range(2)]
    h = p.sb([128, NCH, NB], BF16, "h")
    sq = p.sb([128, NCH, NB], BF16, "sq")
    rstd = p.sb([128, NB], F32, "rstd")
    tmps = [p.sb([128, NB], F32, f"tmp{i}") for i in range(2)]
    stage = p.sb([128, NCH, NB], F32, "stage")
    nblk = TB // NB
    for bi in range(nblk):
        c0 = bi * NB
        cls = 0 if bi == 0 else 1
        X = xab[bi % 2]
        Y = ybb[bi % 2]
        load_fm(p, "sp", X, xa, c0, NB, "xa")
        load_fm(p, "sp", Y, yb, c0, NB, "yb")
        for kc in range(NCH):
            p.stt("dve", X.ap[:, kc, :], Y.ap[:, kc, :], V.ap[:, LV_GPREV + cls * 16 + kc: LV_GPREV + cls * 16 + kc + 1],
                  X.ap[:, kc, :], ALU.mult, ALU.add, r=[X.r, Y.r, V.r], w=[X.r])
        emit_rstd(p, c, X, NB, sq, p.banks[0], rstd)
        sh = (V.ap[:, LV_MOD + cls * 96: LV_MOD + cls * 96 + 16], V.r)
        emit_mod(p, X, rstd, (gs.ap[:, cls, :], gs.r), sh, NB, h, tmps=tmps)
        for oc in range(16):
            bank = p.banks[1 + oc % 4]
            for kc in range(NCH):
                p.matmul(bank.ap[:, 0:NB], w_sb.ap[:, kc, oc * 128:(oc + 1) * 128], h.ap[:, kc, :], kc == 0, kc == NCH - 1,
                         r=[w_sb.r, h.r], w=[bank.r])
            p.copy("act" if oc % 2 == 0 else "dve", stage.ap[:, oc, :], bank.ap[:, 0:NB], r=[bank.r], w=[stage.r])
        dst = scr[:, :, c0:c0 + NB].rearrange("oc p t -> p oc t")
        grp = p.newgroup()
        for k0 in range(0, 16, 8):
            p.dma("sp", dst[:, k0:k0 + 8, :], stage.ap[:, k0:k0 + 8, :], r=[stage.r], w=[scr_r], key="scr", group=grp)

    p.barrier()
    p.release(m0)
    rec = p.sb([128, TB], F32, "rec")
    gat = p.sb([128, TB], F32, "gat")
    xc = p.sb([128, TB], F32, "xc")
    xcb = p.sb([128, TB], BF16, "xcb")
    Rb = p.sb([128, TB], F32, "Rb")
    Ib = p.sb([128, TB], F32, "Ib")
    Eb = p.sb([128, TB], F32, "Eb")
    H = [p.sb([128, TB], F32, f"H{i}") for i in range(2)]
    ob = p.sb([128, TB], BF16, "ob")
    segs = [(0, CTX), (CTX, TB)]
    outs = []
    for j in range(8):
        p.dma("sp", rec.ap, scr[j], r=[scr_r], w=[rec.r], key="rec")
        p.dma("sp", gat.ap, scr[8 + j], r=[scr_r], w=[gat.r], key="gat")
        cw = lambda k: V.ap[:, LV_CW + j * 4 + k: LV_CW + j * 4 + k + 1]
        p.ts("dve", xc.ap, rec.ap, cw(2), ALU.mult, V.ap[:, LV_CB + j:LV_CB + j + 1], ALU.add, r=[rec.r, V.r], w=[xc.r])
        for (s0, e0) in segs:
            for k, off in ((0, -2), (1, -1), (3, 1)):
                if off < 0:
                    o_sl = slice(s0 - off, e0)
                    i_sl = slice(s0, e0 + off)
                else:
                    o_sl = slice(s0, e0 - off)
                    i_sl = slice(s0 + off, e0)
                p.stt("dve", xc.ap[:, o_sl], rec.ap[:, i_sl], cw(k), xc.ap[:, o_sl], ALU.mult, ALU.add,
                      r=[rec.r, xc.r, V.r], w=[xc.r])
        p.copy("act", xcb.ap, xc.ap, r=[xc.r], w=[xcb.r])
        for d in range(2):
            for which, dstb in ((0, Rb), (1, Ib)):
                gi = (d * 2 + which) * 8 + j
                bcol = V.ap[:, LV_GB + gi: LV_GB + gi + 1]
                for bi, t0 in enumerate(range(0, TB, 512)):
                    n = min(512, TB - t0)
                    bank = p.banks[bi % 4]
                    p.matmul(bank.ap[:, 0:n], gw_sb.ap[:, gi, :], xcb.ap[:, t0:t0 + n], True, True, r=[gw_sb.r, xcb.r], w=[bank.r])
                    p.act(dstb.ap[:, t0:t0 + n], bank.ap[:, 0:n], AF.Sigmoid, r=[bank.r, V.r], w=[dstb.r], bias=bcol)
            li = d * 8 + j
            p.act(Eb.ap, Rb.ap, AF.Exp, r=[Rb.r, c2.r], w=[Eb.r], scale=c2.ap[:, li:li + 1])
            p.act(Rb.ap, Rb.ap, AF.Exp, r=[Rb.r, ca.r], w=[Rb.r], scale=ca.ap[:, li:li + 1])
            p.act(Eb.ap, Eb.ap, AF.Sqrt, r=[Eb.r, one.r], w=[Eb.r], bias=one.ap, scale=-1.0)
            p.tt("dve", Ib.ap, Ib.ap, Eb.ap, ALU.mult, r=[Ib.r, Eb.r], w=[Ib.r])
            p.tt("dve", Ib.ap, Ib.ap, xc.ap, ALU.mult, r=[Ib.r, xc.r], w=[Ib.r])
            Hd = H[d]
            if d == 0:
                p.op("dve", lambda e, Hd=Hd: e.tensor_tensor_scan(out=Hd.ap, data0=Rb.ap, data1=Ib.ap, initial=0.0,
                                                                   op0=ALU.mult, op1=ALU.add), [Rb.r, Ib.r], [Hd.r])
            else:
                p.op("dve", lambda e, Hd=Hd: e.tensor_tensor_scan(out=Hd.ap[:, 0:CTX][:, ::-1], data0=Rb.ap[:, 0:CTX][:, ::-1],
                                                                   data1=Ib.ap[:, 0:CTX][:, ::-1], initial=0.0,
                                                                   op0=ALU.mult, op1=ALU.add), [Rb.r, Ib.r], [Hd.r])
                p.op("dve", lambda e, Hd=Hd: e.tensor_tensor_scan(out=Hd.ap[:, CTX:TB][:, ::-1], data0=Rb.ap[:, CTX:TB][:, ::-1],
                                                                   data1=Ib.ap[:, CTX:TB][:, ::-1], initial=Hd.ap[:, 0:1],
                                                                   op0=ALU.mult, op1=ALU.add), [Rb.r, Ib.r, Hd.r], [Hd.r])
        p.tt("dve", H[0].ap, H[0].ap, H[1].ap, ALU.add, r=[H[0].r, H[1].r], w=[H[0].r])
        p.act(gat.ap, gat.ap, AF.Gelu_apprx_tanh, r=[gat.r], w=[gat.r])
        p.tt("dve", ob.ap, H[0].ap, gat.ap, ALU.mult, r=[H[0].r, gat.r], w=[ob.r])
        outs.append(p.dma("sp", gout[j * 128:(j + 1) * 128, :], ob.ap, r=[ob.r], key="out"))
    p.emit(final_waits=outs[-1:])
    return nc


def lru_vec(gprev, mod, layer, slot, hf, inp):
    ch = slice(hf * 1024, (hf + 1) * 1024)
    cols = [pp(gprev[0]), pp(gprev[1])]
    for cls in range(2):
        for k in range(6):
            cols.append(pp(mod[cls, k]))
    cols.append(pp(inp["norm_mix_g"][layer]))
    cw = inp["lru_conv_w"][slot][:, ch]
    cols.append(np.ascontiguousarray(cw.reshape(4, 8, 128).transpose(2, 1, 0).reshape(128, 32)))
    cols.append(pp(inp["lru_conv_b"][slot][ch]))
    gb = inp["lru_gate_b"][slot][:, :, ch]
    cols.append(np.ascontiguousarray(gb.reshape(2, 2, 8, 128).transpose(3, 0, 1, 2).reshape(128, 32)))
    lam = inp["lru_lambda"][slot][:, ch]
    cols.append(np.ascontiguousarray(lam.reshape(2, 8, 128).transpose(2, 0, 1).reshape(128, 16)))
    v = np.concatenate(cols, axis=1).astype(np.float32)
    assert v.shape == (128, LRU_NV)
    return v


TC = 128 + S // 2
NT = TC // 128
PCORES = 4
TP = TB
NTP = TP // 128
NBP = (2 * TP + 32 * 127 + 127) // 128
NSP = NBP * 128
PV_GPREV, PV_MOD, PV_GFFN, POST_NV = 0, 32, 224, 240
BIG = 1.0e30
RC_N = 36 + NBP


def build_post():
    nc = new_nc()
    p = Prog(nc)
    xa = nc.dram_tensor("xa", [D, TP], F32, kind="ExternalInput").ap()
    yb = nc.dram_tensor("yb", [D, TP], F32, kind="ExternalInput").ap()
    G = nc.dram_tensor("G", [D, TP], BF16, kind="ExternalInput").ap()
    vec = nc.dram_tensor("vec", [128, POST_NV], F32, kind="ExternalInput").ap()
    w_out = nc.dram_tensor("w_out", [D, D], F32, kind="ExternalInput").ap()
    wr = nc.dram_tensor("wr", [D, 36], F32, kind="ExternalInput").ap()
    rc = nc.dram_tensor("rc", [1, RC_N], F32, kind="ExternalInput").ap()
    pcol = nc.dram_tensor("pcol", [128, 1], F32, kind="ExternalInput").ap()
    w1 = nc.dram_tensor("w1", [32 * 128, NCH * 512], F32, kind="ExternalInput").ap()
    w3 = nc.dram_tensor("w3", [32 * 128, NCH * 512], F32, kind="ExternalInput").ap()
    w2 = nc.dram_tensor("w2", [32 * 128, 4 * D], F32, kind="ExternalInput").ap()
    xmid = nc.dram_tensor("xmid", [D, TP], F32, kind="ExternalOutput").ap()
    ymoe = nc.dram_tensor("ymoe", [TP, D], F32, kind="ExternalOutput").ap()
    fd = p.dram("fd", [TP, D], BF16)
    xs = p.dram("xs", [NSP, D], BF16)
    ys = p.dram("ys", [NSP, D], F32)
    fd_r = Res("fd", multi=True)
    xs_r = Res("xs", multi=True)
    ys_r = Res("ys", multi=True)
    regs = {}

    def bnd_reg(e):
        if "b" not in regs:
            regs["b"] = e.to_reg(32 * 128 - 1)
        return regs["b"]

    c = make_consts(p)
    V = p.sb([128, POST_NV], F32, "V")
    p.dma("sp", V.ap, vec, w=[V.r], key="in")
    RC = p.sb([128, RC_N], F32, "RC")
    p.dma("sp", RC.ap, rc.partition_broadcast(128), w=[RC.r], key="in")
    PC = p.sb([128, 1], F32, "PC")
    p.dma("sp", PC.ap, pcol, w=[PC.r], key="in")
    gs = p.sb([128, 2, NCH], F32, "gs")
    for cls in range(2):
        sc = V.ap[:, PV_MOD + cls * 96 + 64: PV_MOD + cls * 96 + 80]
        p.stt("dve", gs.ap[:, cls, :], sc, 1.0, V.ap[:, PV_GFFN:PV_GFFN + 16], ALU.add, ALU.mult, r=[V.r], w=[gs.r])
    U = p.sb([128, 128], BF16, "U")
    uf = p.sb([128, 128], F32, "uf")
    p.memset("pool", uf.ap, 0.0, w=[uf.r])
    p.op("pool", lambda e: e.affine_select(out=uf.ap, in_=uf.ap, pattern=[[-1, 128]], compare_op=ALU.is_ge,
                                           fill=1.0, base=0, channel_multiplier=1), [uf.r], [uf.r])
    p.copy("pool", U.ap, uf.ap, r=[uf.r], w=[U.r])
    desti = p.sb([128, NTP, 2], I32, "desti")
    gall = p.sb([128, NTP, 2], F32, "gall")
    idxw = p.sb([128, NBP], I32, "idxw")
    m0 = p.mark()

    NB = 256
    cum = p.sb([128, 32], F32, "cum")
    p.memset("pool", cum.ap, 0.0, w=[cum.r])
    posall = p.sb([128, NTP, 32], F32, "posall")
    mkall = p.sb([128, NTP, 2, 32], F32, "mkall")
    wo = p.sb([128, NCH, D], BF16, "wo")
    load_w(p, wo, w_out, key="w", nsplit=8)
    wr_sb = p.sb([128, NCH, 36], F32, "wr")
    p.dma("sp", wr_sb.ap, wr.rearrange("(kc p) n -> p kc n", p=128), w=[wr_sb.r], key="in")
    Xb = [p.sb([128, NCH, NB], F32, f"X{i}") for i in range(2)]
    Y = p.sb([128, NCH, NB], F32, "Y")
    Gb = [p.sb([128, NCH, NB], BF16, f"G{i}") for i in range(2)]
    sq = p.sb([128, NCH, NB], BF16, "sq")
    fbf = p.sb([128, NCH, NB], BF16, "fbf")
    rstd = p.sb([128, NB], F32, "rstd")
    ftok = [p.sb([128, D], BF16, f"ftok{i}") for i in range(2)]
    Mt = [p.sb([128, 32], BF16, f"Mt{i}") for i in range(2)]

    def rt(name, shape, dt=F32):
        return [p.sb(shape, dt, f"{name}{i}") for i in range(2)]
    lg = rt("lg", [128, 36]); m4 = rt("m4", [128, 1]); d4 = rt("d4", [128, 4]); s4 = rt("s4", [128, 1])
    oh4 = rt("oh4", [128, 4]); lem = rt("lem", [128, 32]); lem2 = rt("lem2", [128, 32])
    top1 = rt("top1", [128, 1]); top2 = rt("top2", [128, 1]); d12 = rt("d12", [128, 1])
    blocks = [(CTX * 0, CTX)] + [(CTX + NB * i, NB) for i in range((TP - CTX) // NB)]
    xm_outs = []
    ti = 0
    for bi, (c0, N) in enumerate(blocks):
        cls = 0 if bi == 0 else 1
        X = Xb[bi % 2]
        Gt = Gb[bi % 2]
        load_fm(p, "sp", X, xa, c0, N, "xa")
        load_fm(p, "sp", Y, yb, c0, N, "yb")
        load_fm(p, "sp", Gt, G, c0, N, "G")
        for kc in range(NCH):
            p.stt("dve", X.ap[:, kc, 0:N], Y.ap[:, kc, 0:N], V.ap[:, PV_GPREV + cls * 16 + kc: PV_GPREV + cls * 16 + kc + 1],
                  X.ap[:, kc, 0:N], ALU.mult, ALU.add, r=[X.r, Y.r, V.r], w=[X.r])
        g1c = PV_MOD + cls * 96 + 32
        for oc in range(NCH):
            bank = p.banks[oc % 2]
            for kc in range(NCH):
                p.matmul(bank.ap[:, 0:N], wo.ap[:, kc, oc * 128:(oc + 1) * 128], Gt.ap[:, kc, 0:N], kc == 0, kc == NCH - 1,
                         r=[wo.r, Gt.r], w=[bank.r])
            p.stt("dve", X.ap[:, oc, 0:N], bank.ap[:, 0:N], V.ap[:, g1c + oc: g1c + oc + 1], X.ap[:, oc, 0:N], ALU.mult, ALU.add,
                  r=[bank.r, X.r, V.r], w=[X.r])
        xm_outs.append(store_fm(p, "sp", xmid, c0, N, X, "xmid"))
        emit_rstd(p, c, X, N, sq, p.banks[2], rstd)
        sh = (V.ap[:, PV_MOD + cls * 96 + 48: PV_MOD + cls * 96 + 64], V.r)
        emit_mod(p, X, rstd, (gs.ap[:, cls, :], gs.r), sh, N, fbf, out_f32=Y)
        for tt_ in range(N // 128):
            q = ti % 2
            tsl = slice(tt_ * 128, (tt_ + 1) * 128)
            rb = p.banks[3]
            for kc in range(NCH):
                p.matmul(rb.ap[:, 0:36], Y.ap[:, kc, tsl], wr_sb.ap[:, kc, :], kc == 0, kc == NCH - 1, r=[Y.r, wr_sb.r], w=[rb.r])
            L = lg[q]
            mk1 = mkall.ap[:, ti, 0, :]
            mk2 = mkall.ap[:, ti, 1, :]
            p.tt("dve", L.ap, rb.ap[:, 0:36], RC.ap[:, 0:36], ALU.add, r=[rb.r, RC.r], w=[L.r])
            p.op("dve", lambda e, o=m4[q], L=L: e.tensor_reduce(out=o.ap, in_=L.ap[:, 0:4], axis=AX.X, op=ALU.max), [L.r], [m4[q].r])
            p.ts("dve", d4[q].ap, L.ap[:, 0:4], m4[q].ap, ALU.subtract, r=[L.r, m4[q].r], w=[d4[q].r])
            p.act(d4[q].ap, d4[q].ap, AF.Exp, r=[d4[q].r], w=[d4[q].r])
            p.op("dve", lambda e, o=s4[q], i_=d4[q]: e.tensor_reduce(out=o.ap, in_=i_.ap, axis=AX.X, op=ALU.add), [d4[q].r], [s4[q].r])
            p.op("dve", lambda e, o=s4[q]: e.reciprocal(out=o.ap, in_=o.ap), [s4[q].r], [s4[q].r])
            p.ts("dve", oh4[q].ap, L.ap[:, 0:4], m4[q].ap, ALU.is_equal, r=[L.r, m4[q].r], w=[oh4[q].r])
            p.ts("dve", oh4[q].ap, oh4[q].ap, -1.0, ALU.add, BIG, ALU.mult, r=[oh4[q].r], w=[oh4[q].r])
            for g in range(4):
                p.ts("dve", lem[q].ap[:, 8 * g:8 * g + 8], L.ap[:, 4 + 8 * g:12 + 8 * g], oh4[q].ap[:, g:g + 1], ALU.add,
                     r=[L.r, oh4[q].r], w=[lem[q].r])
            p.op("dve", lambda e, o=top1[q], i_=lem[q]: e.tensor_reduce(out=o.ap, in_=i_.ap, axis=AX.X, op=ALU.max), [lem[q].r], [top1[q].r])
            p.ts("dve", mk1, lem[q].ap, top1[q].ap, ALU.is_equal, r=[lem[q].r, top1[q].r], w=[mkall.r])
            p.stt("dve", lem2[q].ap, mk1, -BIG, lem[q].ap, ALU.mult, ALU.add, r=[mkall.r, lem[q].r], w=[lem2[q].r])
            p.op("dve", lambda e, o=top2[q], i_=lem2[q]: e.tensor_reduce(out=o.ap, in_=i_.ap, axis=AX.X, op=ALU.max), [lem2[q].r], [top2[q].r])
            p.ts("dve", mk2, lem2[q].ap, top2[q].ap, ALU.is_equal, r=[lem2[q].r, top2[q].r], w=[mkall.r])
            p.tt("dve", d12[q].ap, top1[q].ap, top2[q].ap, ALU.subtract, r=[top1[q].r, top2[q].r], w=[d12[q].r])
            p.act(d12[q].ap, d12[q].ap, AF.Sigmoid, r=[d12[q].r], w=[d12[q].r])
            p.tt("dve", gall.ap[:, ti, 0:1], d12[q].ap, s4[q].ap, ALU.mult, r=[d12[q].r, s4[q].r], w=[gall.r])
            p.tt("dve", gall.ap[:, ti, 1:2], s4[q].ap, gall.ap[:, ti, 0:1], ALU.subtract, r=[s4[q].r, gall.r], w=[gall.r])
            p.tt("dve", Mt[q].ap, mk1, mk2, ALU.add, r=[mkall.r], w=[Mt[q].r])
            pb = p.banks[4]
            cb = p.banks[5]
            p.matmul(pb.ap[:, 0:32], U.ap, Mt[q].ap, True, True, r=[U.r, Mt[q].r], w=[pb.r])
            p.matmul(cb.ap[:, 0:32], c["ones_bf"].ap, Mt[q].ap, True, True, r=[c["ones_bf"].r, Mt[q].r], w=[cb.r])
            p.tt("dve", posall.ap[:, ti, :], pb.ap[:, 0:32], cum.ap, ALU.add, r=[pb.r, cum.r], w=[posall.r])
            p.tt("dve", cum.ap, cb.ap[:, 0:32], cum.ap, ALU.add, r=[cb.r, cum.r], w=[cum.r])
            F = ftok[q]
            for half in range(2):
                tb = p.banks[6 + half]
                tbv = tb.ap.bitcast(BF16)
                for k8 in range(8):
                    kc = half * 8 + k8
                    p.transpose(tbv[:, k8 * 128:(k8 + 1) * 128], fbf.ap[:, kc, tsl], c["ident_bf"].ap, r=[fbf.r, c["ident_bf"].r], w=[tb.r])
                p.copy("act", F.ap[:, half * 1024:(half + 1) * 1024], tbv[:, 0:1024], r=[tb.r], w=[F.r])
            p.dma("sp", fd[ti * 128:(ti + 1) * 128, :], F.ap, r=[F.r], w=[fd_r], key="fd")
            ti += 1
    assert ti == NTP

    padded = p.sb([128, 32], F32, "padded")
    pend = p.sb([128, 32], F32, "pend")
    pstart = p.sb([128, 32], F32, "pstart")
    onesr = p.sb([128, 32], F32, "onesr")
    be_f = p.sb([128, NBP], F32, "be_f")
    p.memset("pool", onesr.ap, 1.0, w=[onesr.r])
    p.memset("pool", padded.ap, 0.0, w=[padded.r])
    for m_ in range((2 * TP) // 128):
        p.stt("dve", padded.ap, cum.ap, float(128 * m_), padded.ap, ALU.is_gt, ALU.add, r=[cum.r, padded.r], w=[padded.r])
    p.ts("dve", padded.ap, padded.ap, 128.0, ALU.mult, r=[padded.r], w=[padded.r])
    p.op("dve", lambda e: e.tensor_tensor_scan(out=pend.ap, data0=onesr.ap, data1=padded.ap, initial=0.0, op0=ALU.mult, op1=ALU.add),
         [onesr.r, padded.r], [pend.r])
    p.tt("dve", pstart.ap, pend.ap, padded.ap, ALU.subtract, r=[pend.r, padded.r], w=[pstart.r])
    p.memset("pool", be_f.ap, 0.0, w=[be_f.r])
    for e_ in range(32):
        p.stt("dve", be_f.ap, RC.ap[:, 36:36 + NBP], pend.ap[:, e_:e_ + 1], be_f.ap, ALU.is_ge, ALU.add, r=[RC.r, pend.r, be_f.r], w=[be_f.r])
    p.ts("dve", be_f.ap, be_f.ap, 31.0, ALU.min, r=[be_f.r], w=[be_f.r])
    same = p.sb([128, NBP], F32, "same")
    p.memset("pool", same.ap, 0.0, w=[same.r])
    p.tt("dve", same.ap[:, 2:NBP], be_f.ap[:, 2:NBP], be_f.ap[:, 0:NBP - 2], ALU.is_equal, r=[be_f.r, same.r], w=[same.r])
    p.ts("dve", be_f.ap, be_f.ap, 128.0, ALU.mult, PC.ap[:, 0:1], ALU.add, r=[be_f.r, PC.r], w=[be_f.r])
    p.stt("dve", be_f.ap, same.ap, 1.0e6, be_f.ap, ALU.mult, ALU.add, r=[same.r, be_f.r], w=[be_f.r])
    p.copy("dve", idxw.ap, be_f.ap, r=[be_f.r], w=[idxw.r])
    pe_ = rt("pe", [128, 32]); t32 = rt("t32", [128, 32]); dstk = rt("dstk", [128, 1])
    for ti in range(NTP):
        q = ti % 2
        p.tt("dve", pe_[q].ap, posall.ap[:, ti, :], pstart.ap, ALU.add, r=[posall.r, pstart.r], w=[pe_[q].r])
        for k in range(2):
            p.tt("dve", t32[q].ap, mkall.ap[:, ti, k, :], pe_[q].ap, ALU.mult, r=[mkall.r, pe_[q].r], w=[t32[q].r])
            p.op("dve", lambda e, o=dstk[q], i_=t32[q]: e.tensor_reduce(out=o.ap, in_=i_.ap, axis=AX.X, op=ALU.add), [t32[q].r], [dstk[q].r])
            p.copy("dve", desti.ap[:, ti, k:k + 1], dstk[q].ap, r=[dstk[q].r], w=[desti.r])
        F = ftok[q]
        p.dma("sp", F.ap, fd[ti * 128:(ti + 1) * 128, :], r=[fd_r], w=[F.r], key="fdr")
        for k in range(2):
            p.op("pool", lambda e, F=F, ti=ti, k=k: e.indirect_dma_start(
                out=xs[:, :], out_offset=bass.IndirectOffsetOnAxis(ap=desti.ap[:, ti, k:k + 1], axis=0),
                in_=F.ap[:, :], in_offset=None), [F.r, desti.r], [xs_r], dkey=("pool", id(F.r)))

    p.barrier()
    p.release(m0)
    W1 = [p.sb([128, NCH * 512], BF16, f"W1{i}") for i in range(2)]
    W3 = [p.sb([128, NCH * 512], BF16, f"W3{i}") for i in range(2)]
    W2 = [p.sb([128, 4 * D], BF16, f"W2{i}") for i in range(2)]
    XS = [p.sb([128, D], BF16, f"XS{i}") for i in range(2)]
    xsT = p.sb([128, NCH, 128], BF16, "xsT")
    hh = p.sb([128, 512], BF16, "hh")
    hT = p.sb([128, 4, 128], BF16, "hT")
    sil = p.sb([128, 512], F32, "sil")
    ysb = [p.sb([128, D], F32, f"ysb{i}") for i in range(2)]
    for b in range(NBP):
        q = b % 2
        for Wt, wsrc, key in ((W1[q], w1, "w1"), (W3[q], w3, "w3"), (W2[q], w2, "w2")):
            p.op("pool", lambda e, Wt=Wt, wsrc=wsrc, b=b: e.indirect_dma_start(
                out=Wt.ap[:, :], out_offset=None, in_=wsrc[:, :],
                in_offset=bass.IndirectOffsetOnAxis(ap=idxw.ap[:, b:b + 1], axis=0),
                bounds_check=bnd_reg(e), oob_is_err=False), [idxw.r], [Wt.r], dkey=("pool", id(Wt.r)))
        p.dma("sp", XS[q].ap, xs[b * 128:(b + 1) * 128, :], r=[xs_r], w=[XS[q].r], key="xs")
        for half in range(2):
            tb = p.banks[half]
            tbv = tb.ap.bitcast(BF16)
            for k8 in range(8):
                kc = half * 8 + k8
                p.transpose(tbv[:, k8 * 128:(k8 + 1) * 128], XS[q].ap[:, kc * 128:(kc + 1) * 128], c["ident_bf"].ap,
                            r=[XS[q].r, c["ident_bf"].r], w=[tb.r])
            p.copy("act" if half == 0 else "dve", xsT.ap[:, half * 8:(half + 1) * 8, :],
                   tbv[:, 0:1024].rearrange("p (a b) -> p a b", a=8), r=[tb.r], w=[xsT.r])
        b1 = p.banks[2]
        b3 = p.banks[3]
        for kc in range(NCH):
            p.matmul(b1.ap[:, 0:512], xsT.ap[:, kc, :], W1[q].ap[:, kc * 512:(kc + 1) * 512], kc == 0, kc == NCH - 1,
                     r=[W1[q].r, xsT.r], w=[b1.r])
        for kc in range(NCH):
            p.matmul(b3.ap[:, 0:512], xsT.ap[:, kc, :], W3[q].ap[:, kc * 512:(kc + 1) * 512], kc == 0, kc == NCH - 1,
                     r=[W3[q].r, xsT.r], w=[b3.r])
        p.act(sil.ap, b1.ap[:, 0:512], AF.Silu, r=[b1.r], w=[sil.r])
        p.tt("dve", hh.ap, sil.ap, b3.ap[:, 0:512], ALU.mult, r=[sil.r, b3.r], w=[hh.r])
        tb = p.banks[4]
        tbv = tb.ap.bitcast(BF16)
        for hc in range(4):
            p.transpose(tbv[:, hc * 128:(hc + 1) * 128], hh.ap[:, hc * 128:(hc + 1) * 128], c["ident_bf"].ap,
                        r=[hh.r, c["ident_bf"].r], w=[tb.r])
        p.copy("act", hT.ap, tbv[:, 0:512].rearrange("p (a b) -> p a b", a=4), r=[tb.r], w=[hT.r])
        yb_ = ysb[q]
        for db in range(4):
            bank = p.banks[5 + db % 3]
            for hc in range(4):
                p.matmul(bank.ap[:, 0:512], hT.ap[:, hc, :], W2[q].ap[:, hc * D + db * 512: hc * D + (db + 1) * 512],
                         hc == 0, hc == 3, r=[hT.r, W2[q].r], w=[bank.r])
            p.copy("act" if db % 2 == 0 else "dve", yb_.ap[:, db * 512:(db + 1) * 512], bank.ap[:, 0:512], r=[bank.r], w=[yb_.r])
        p.dma("sp", ys[b * 128:(b + 1) * 128, :], yb_.ap, r=[yb_.r], w=[ys_r], key="ys")

    p.barrier()
    p.release(m0)
    R1 = [p.sb([128, D], F32, f"R1{i}") for i in range(2)]
    R2 = [p.sb([128, D], F32, f"R2{i}") for i in range(2)]
    YO = [p.sb([128, D], F32, f"YO{i}") for i in range(2)]
    outs = []
    for ti in range(NTP):
        q = ti % 2
        for k, Rk in ((0, R1[q]), (1, R2[q])):
            p.op("pool", lambda e, Rk=Rk, ti=ti, k=k: e.indirect_dma_start(
                out=Rk.ap[:, :], out_offset=None, in_=ys[:, :],
                in_offset=bass.IndirectOffsetOnAxis(ap=desti.ap[:, ti, k:k + 1], axis=0)),
                [ys_r, desti.r], [Rk.r], dkey=("pool", id(Rk.r)))
        p.ts("dve", YO[q].ap, R1[q].ap, gall.ap[:, ti, 0:1], ALU.mult, r=[R1[q].r, gall.r], w=[YO[q].r])
        p.stt("dve", YO[q].ap, R2[q].ap, gall.ap[:, ti, 1:2], YO[q].ap, ALU.mult, ALU.add, r=[R2[q].r, gall.r, YO[q].r], w=[YO[q].r])
        outs.append(p.dma("sp", ymoe[ti * 128:(ti + 1) * 128, :], YO[q].ap, r=[YO[q].r], key="out"))
    p.emit(final_waits=[xm_outs[-1], outs[-1]])
    return nc


def post_vec(gprev, mod, layer, inp):
    cols = [pp(gprev[0]), pp(gprev[1])]
    for cls in range(2):
        for k in range(6):
            cols.append(pp(mod[cls, k]))
    cols.append(pp(inp["norm_ffn_g"][layer]))
    v = np.concatenate(cols, axis=1).astype(np.float32)
    assert v.shape == (128, POST_NV)
    return v


def post_weights(inp, layer):
    w1 = np.ascontiguousarray(inp["expert_w1"][layer].reshape(32, NCH, 128, 512).transpose(0, 2, 1, 3)).reshape(32 * 128, NCH * 512)
    w3 = np.ascontiguousarray(inp["expert_w3"][layer].reshape(32, NCH, 128, 512).transpose(0, 2, 1, 3)).reshape(32 * 128, NCH * 512)
    w2 = np.ascontiguousarray(inp["expert_w2"][layer].reshape(32, 4, 128, D).transpose(0, 2, 1, 3)).reshape(32 * 128, 4 * D)
    wr = np.ascontiguousarray(np.concatenate([inp["router_group_w"][layer], inp["router_expert_w"][layer]], 1))
    rc = np.concatenate([inp["router_group_b"][layer], inp["router_expert_b"][layer],
                         np.arange(NBP, dtype=np.float32) * 128.0])[None].astype(np.float32)
    pcol = np.arange(128, dtype=np.float32).reshape(128, 1)
    return {"w1": w1, "w3": w3, "w2": w2, "wr": wr, "rc": rc, "pcol": pcol}


AT_NV = 242
AV_GPREV, AV_MOD, AV_GMIX, AV_SUBG, AV_PAD = 0, 32, 224, 240, 241
HD = 64
DIFF_SCALE = HD ** -0.5


def rope_tables():
    t = np.arange(S)
    row = (t // 64).astype(np.float32)
    col = (t % 64).astype(np.float32)
    nf = HD // 4
    inv = (10000.0 ** (-np.arange(nf, dtype=np.float32) / nf)).astype(np.float32)
    ang = np.concatenate([row[:, None] * inv, col[:, None] * inv], axis=-1).astype(np.float32)
    cos = np.cos(ang).astype(np.float32)
    sin = np.sin(ang).astype(np.float32)
    jj = np.arange(128) % 32
    cosT = np.ascontiguousarray(cos[:, jj].T)
    sinT = np.ascontiguousarray(sin[:, jj].T)
    R = np.zeros((128, 128), np.float32)
    for p_ in range(128):
        j = p_ % 64
        if j < 32:
            R[p_ + 32, p_] = -1.0
        else:
            R[p_ - 32, p_] = 1.0
    return cosT, sinT, R


def build_attn(lambda_init, dbg_heads=8, dbg_blocks=None, dbg_skip=()):
    nc = new_nc()
    p = Prog(nc)
    xa = nc.dram_tensor("xa", [D, TB], F32, kind="ExternalInput").ap()
    yb = nc.dram_tensor("yb", [D, TB], F32, kind="ExternalInput").ap()
    vec = nc.dram_tensor("vec", [128, AT_NV], F32, kind="ExternalInput").ap()
    lamv = nc.dram_tensor("lamv", [1, 256], F32, kind="ExternalInput").ap()
    wq = nc.dram_tensor("wq", [D, 1024], F32, kind="ExternalInput").ap()
    wk = nc.dram_tensor("wk", [D, 1024], F32, kind="ExternalInput").ap()
    wv = nc.dram_tensor("wv", [D, 1024], F32, kind="ExternalInput").ap()
    cosd = nc.dram_tensor("cosT", [128, S], F32, kind="ExternalInput").ap()
    sind = nc.dram_tensor("sinT", [128, S], F32, kind="ExternalInput").ap()
    Rd = nc.dram_tensor("R", [128, 128], F32, kind="ExternalInput").ap()
    gout = nc.dram_tensor("g", [1024, TB], BF16, kind="ExternalOutput").ap()
    qs = p.dram("qs", [8, 128, TB], BF16)
    ks = p.dram("ks", [8, 128, TB], BF16)
    vs = p.dram("vs", [TB, 1024], BF16)
    qs_r, ks_r, vs_r = Res("qs", multi=True), Res("ks", multi=True), Res("vs", multi=True)
    c = make_consts(p)
    V = p.sb([128, AT_NV], F32, "V")
    p.dma("sp", V.ap, vec, w=[V.r], key="in")
    gs = p.sb([128, 2, NCH], F32, "gs")
    for cls in range(2):
        sc = V.ap[:, AV_MOD + cls * 96 + 16: AV_MOD + cls * 96 + 32]
        p.stt("dve", gs.ap[:, cls, :], sc, 1.0, V.ap[:, AV_GMIX:AV_GMIX + 16], ALU.add, ALU.mult, r=[V.r], w=[gs.r])
    LV = p.sb([128, 256], F32, "LV")
    p.dma("sp", LV.ap, lamv.partition_broadcast(128), w=[LV.r], key="in")
    lt = p.sb([128, 2, 64], F32, "lt")
    le = p.sb([128, 2], F32, "le")
    nlam = p.sb([128, 1], F32, "nlam")
    gsub = p.sb([128, 1], F32, "gsub")
    for i in range(2):
        p.tt("dve", lt.ap[:, i, :], LV.ap[:, 128 * i:128 * i + 64], LV.ap[:, 128 * i + 64:128 * i + 128], ALU.mult, r=[LV.r], w=[lt.r])
        p.op("dve", lambda e, i=i: e.tensor_reduce(out=le.ap[:, i:i + 1], in_=lt.ap[:, i, :], axis=AX.X, op=ALU.add), [lt.r], [le.r])
    p.act(le.ap, le.ap, AF.Exp, r=[le.r], w=[le.r])
    p.tt("dve", nlam.ap, le.ap[:, 1:2], le.ap[:, 0:1], ALU.subtract, r=[le.r], w=[nlam.r])
    p.ts("dve", nlam.ap, nlam.ap, -float(lambda_init), ALU.add, r=[nlam.r], w=[nlam.r])
    p.ts("dve", gsub.ap, V.ap[:, AV_SUBG:AV_SUBG + 1], float(1.0 - lambda_init), ALU.mult, r=[V.r], w=[gsub.r])
    Rb = p.sb([128, 128], BF16, "Rb")
    p.dma("pool", Rb.ap, Rd, w=[Rb.r], key="R")
    m0 = p.mark()

    NB = 256
    Wq = p.sb([128, NCH, 1024], BF16, "Wq")
    Wk = p.sb([128, NCH, 1024], BF16, "Wk")
    Wv = p.sb([128, NCH, 1024], BF16, "Wv")
    load_w(p, Wq, wq, key="wq", nsplit=8)
    load_w(p, Wk, wk, key="wk", nsplit=8)
    load_w(p, Wv, wv, key="wv", nsplit=8)
    X = p.sb([128, NCH, NB], F32, "X")
    Y = p.sb([128, NCH, NB], F32, "Y")
    h = p.sb([128, NCH, NB], BF16, "h")
    sq = p.sb([128, NCH, NB], BF16, "sq")
    rstd = p.sb([128, NB], F32, "rstd")
    tmps = [p.sb([128, NB], F32, f"tmp{i}") for i in range(2)]
    qst = p.sb([128, 8, NB], BF16, "qst")
    kst = p.sb([128, 8, NB], BF16, "kst")
    vst = p.sb([128, NB // 128, 1024], BF16, "vst")
    cosb = p.sb([128, NB], F32, "cosb")
    sinb = p.sb([128, NB], F32, "sinb")
    qb16 = [p.sb([128, NB], BF16, f"qb{i}") for i in range(2)]
    t1 = [p.sb([128, NB], F32, f"t1{i}") for i in range(2)]
    t2 = [p.sb([128, NB], F32, f"t2{i}") for i in range(2)]
    it = 0
    for bi in (range(TB // NB) if dbg_blocks is None else dbg_blocks):
        c0 = bi * NB
        cls = 0 if bi == 0 else 1
        load_fm(p, "sp", X, xa, c0, NB, "xa")
        load_fm(p, "sp", Y, yb, c0, NB, "yb")
        if cls == 1:
            p.dma("sp", cosb.ap, cosd[:, c0 - CTX:c0 - CTX + NB], w=[cosb.r], key="cs")
            p.dma("sp", sinb.ap, sind[:, c0 - CTX:c0 - CTX + NB], w=[sinb.r], key="cs")
        for kc in range(NCH):
            p.stt("dve", X.ap[:, kc, :], Y.ap[:, kc, :], V.ap[:, AV_GPREV + cls * 16 + kc: AV_GPREV + cls * 16 + kc + 1],
                  X.ap[:, kc, :], ALU.mult, ALU.add, r=[X.r, Y.r, V.r], w=[X.r])
        emit_rstd(p, c, X, NB, sq, p.banks[0], rstd)
        sh = (V.ap[:, AV_MOD + cls * 96: AV_MOD + cls * 96 + 16], V.r)
        emit_mod(p, X, rstd, (gs.ap[:, cls, :], gs.r), sh, NB, h, tmps=tmps)
        for W, st in ((Wq, qst), (Wk, kst)):
            for hd in range(8):
                bank = p.banks[1 + it % 3]
                q_ = it % 2
                it += 1
                for kc in range(NCH):
                    p.matmul(bank.ap[:, 0:NB], W.ap[:, kc, hd * 128:(hd + 1) * 128], h.ap[:, kc, :], kc == 0, kc == NCH - 1,
                             r=[W.r, h.r], w=[bank.r])
                if cls == 0:
                    p.copy("act", st.ap[:, hd, :], bank.ap[:, 0:NB], r=[bank.r], w=[st.r])
                else:
                    rbk = p.banks[4 + q_]
                    p.copy("dve", qb16[q_].ap, bank.ap[:, 0:NB], r=[bank.r], w=[qb16[q_].r])
                    p.matmul(rbk.ap[:, 0:NB], Rb.ap, qb16[q_].ap, True, True, r=[Rb.r, qb16[q_].r], w=[rbk.r])
                    p.tt("dve", t1[q_].ap, bank.ap[:, 0:NB], cosb.ap, ALU.mult, r=[bank.r, cosb.r], w=[t1[q_].r])
                    p.tt("dve", t2[q_].ap, rbk.ap[:, 0:NB], sinb.ap, ALU.mult, r=[rbk.r, sinb.r], w=[t2[q_].r])
                    p.tt("pool", st.ap[:, hd, :], t1[q_].ap, t2[q_].ap, ALU.add, r=[t1[q_].r, t2[q_].r], w=[st.r])
        for tt_ in range(NB // 128):
            for nb in range(2):
                bank = p.banks[6 + nb]
                for kc in range(NCH):
                    p.matmul(bank.ap[:, 0:512], h.ap[:, kc, tt_ * 128:(tt_ + 1) * 128], Wv.ap[:, kc, nb * 512:(nb + 1) * 512],
                             kc == 0, kc == NCH - 1, r=[Wv.r, h.r], w=[bank.r])
                p.copy("act" if nb == 0 else "dve", vst.ap[:, tt_, nb * 512:(nb + 1) * 512], bank.ap[:, 0:512], r=[bank.r], w=[vst.r])
        p.dma("sp", qs[:, :, c0:c0 + NB].rearrange("h p t -> p h t"), qst.ap, r=[qst.r], w=[qs_r], key="qs")
        p.dma("sp", ks[:, :, c0:c0 + NB].rearrange("h p t -> p h t"), kst.ap, r=[kst.r], w=[ks_r], key="ks")
        p.dma("sp", vs[c0:c0 + NB, :].rearrange("(t p) v -> p t v", p=128), vst.ap, r=[vst.r], w=[vs_r], key="vs")

    p.barrier()
    p.release(m0)
    NKT = TB // 128
    qT = [p.sb([128, TB], BF16, f"qT{i}") for i in range(2)]
    kT = [p.sb([128, TB], BF16, f"kT{i}") for i in range(2)]
    Vt = [p.sb([128, NKT, 128], BF16, f"Vt{i}") for i in range(2)]
    Pt = [p.sb([128, 512], BF16, f"Pt{i}") for i in range(4)]
    ost = [p.sb([128, TB], BF16, f"ost{i}") for i in range(2)]
    r0 = p.sb([128, 512], F32, "r0")
    o0 = p.sb([128, 512], F32, "o0")
    o1 = p.sb([128, 512], F32, "o1")
    sqo = p.sb([128, 512], BF16, "sqo")
    rs = p.sb([128, 512], F32, "rs")
    qblocks = [(0, CTX, CTX // 128)] + [(CTX + 512 * i, 512, NKT) for i in range(S // 512)]
    outs = []
    pi = 0
    for hd in range(dbg_heads):
        q_ = hd % 2
        p.dma("sp", qT[q_].ap, qs[hd], r=[qs_r], w=[qT[q_].r], key="lq")
        p.dma("sp", kT[q_].ap, ks[hd], r=[ks_r], w=[kT[q_].r], key="lk")
        vsrc = vs[:, hd * 128:(hd + 1) * 128].rearrange("(kt p) v -> p kt v", p=128)
        half = NKT // 2
        grp = p.newgroup()
        p.dma("sp", Vt[q_].ap[:, 0:half, :], vsrc[:, 0:half, :], r=[vs_r], w=[Vt[q_].r], key="lv", group=grp)
        p.dma("sp", Vt[q_].ap[:, half:NKT, :], vsrc[:, half:NKT, :], r=[vs_r], w=[Vt[q_].r], key="lv", group=grp)
        O = ost[q_]
        for (q0, N, nkt) in qblocks:
            Ob = [p.banks[0], p.banks[1]]
            Db = [p.banks[2], p.banks[3]]
            steps = [(kt, cc) for kt in range(nkt) for cc in range(2)]
            slots = {}
            LOOK = 2
            for i in range(len(steps) + LOOK):
                if i < len(steps):
                    kt, cc = steps[i]
                    sbk = p.banks[4 + pi % 3]
                    P_ = Pt[pi % 4]
                    pi += 1
                    slots[i] = P_
                    p.matmul(sbk.ap[:, 0:N], kT[q_].ap[cc * 64:(cc + 1) * 64, kt * 128:(kt + 1) * 128],
                             qT[q_].ap[cc * 64:(cc + 1) * 64, q0:q0 + N], True, True, r=[kT[q_].r, qT[q_].r], w=[sbk.r])
                    p.act(P_.ap[:, 0:N], sbk.ap[:, 0:N], AF.Exp, r=[sbk.r], w=[P_.r], scale=float(DIFF_SCALE))
                j = i - LOOK
                if j >= 0:
                    kt, cc = steps[j]
                    P_ = slots.pop(j)
                    p.matmul(Ob[cc].ap[:, 0:N], Vt[q_].ap[:, kt, :], P_.ap[:, 0:N], kt == 0, kt == nkt - 1, r=[Vt[q_].r, P_.r], w=[Ob[cc].r])
                    p.matmul(Db[cc].ap[:, 0:N], c["ones_bf"].ap, P_.ap[:, 0:N], kt == 0, kt == nkt - 1, r=[c["ones_bf"].r, P_.r], w=[Db[cc].r])
            p.op("dve", lambda e, N=N, Db=Db: e.reciprocal(out=r0.ap[:, 0:N], in_=Db[0].ap[:, 0:N]), [Db[0].r], [r0.r])
            p.tt("dve", o0.ap[:, 0:N], Ob[0].ap[:, 0:N], r0.ap[:, 0:N], ALU.mult, r=[Ob[0].r, r0.r], w=[o0.r])
            p.op("dve", lambda e, N=N, Db=Db: e.reciprocal(out=r0.ap[:, 0:N], in_=Db[1].ap[:, 0:N]), [Db[1].r], [r0.r])
            p.tt("dve", o1.ap[:, 0:N], Ob[1].ap[:, 0:N], r0.ap[:, 0:N], ALU.mult, r=[Ob[1].r, r0.r], w=[o1.r])
            p.stt("dve", o0.ap[:, 0:N], o1.ap[:, 0:N], nlam.ap[:, 0:1], o0.ap[:, 0:N], ALU.mult, ALU.add, r=[o1.r, nlam.r, o0.r], w=[o0.r])
            p.act(sqo.ap[:, 0:N], o0.ap[:, 0:N], AF.Square, r=[o0.r], w=[sqo.r])
            nbk = p.banks[7]
            p.matmul(nbk.ap[:, 0:N], c["ones_bf"].ap, sqo.ap[:, 0:N], True, True, r=[c["ones_bf"].r, sqo.r], w=[nbk.r])
            p.act(rs.ap[:, 0:N], nbk.ap[:, 0:N], AF.Sqrt, r=[nbk.r, c["eps"].r], w=[rs.r], bias=c["eps"].ap, scale=1.0 / 128.0)
            p.op("dve", lambda e, N=N: e.reciprocal(out=rs.ap[:, 0:N], in_=rs.ap[:, 0:N]), [rs.r], [rs.r])
            p.stt("dve", O.ap[:, q0:q0 + N], o0.ap[:, 0:N], gsub.ap[:, 0:1], rs.ap[:, 0:N], ALU.mult, ALU.mult, r=[o0.r, gsub.r, rs.r], w=[O.r])
        outs.append(p.dma("sp", gout[hd * 128:(hd + 1) * 128, :], O.ap, r=[O.r], key="out"))
    p.emit(final_waits=outs[-1:] if outs else [])
    return nc


def attn_vec(gprev, mod, layer, slot, inp):
    cols = [pp(gprev[0]), pp(gprev[1])]
    for cls in range(2):
        for k in range(6):
            cols.append(pp(mod[cls, k]))
    cols.append(pp(inp["norm_mix_g"][layer]))
    cols.append(np.asarray(inp["diff_subln_g"][slot]).reshape(128, 1))
    cols.append(np.zeros((128, 1), np.float32))
    v = np.concatenate(cols, axis=1).astype(np.float32)
    assert v.shape == (128, AT_NV)
    return v


GM_NV = 240 + 16
GV_GPREV, GV_MOD, GV_GMIX, GV_BS = 0, 32, 224, 240


def build_gmlp():
    nc = new_nc()
    p = Prog(nc)
    xa = nc.dram_tensor("xa", [D, TC], F32, kind="ExternalInput").ap()
    yb = nc.dram_tensor("yb", [D, TC], F32, kind="ExternalInput").ap()
    vec = nc.dram_tensor("vec", [128, GM_NV], F32, kind="ExternalInput").ap()
    w_uv = nc.dram_tensor("w_uv", [D, 2 * D], F32, kind="ExternalInput").ap()
    rows = nc.dram_tensor("rows", [3, 2 * D], F32, kind="ExternalInput").ap()
    wsT = nc.dram_tensor("wsT", [16, 128, 128], F32, kind="ExternalInput").ap()
    zout = nc.dram_tensor("g", [D, TC], BF16, kind="ExternalOutput").ap()
    c = make_consts(p)
    V = p.sb([128, GM_NV], F32, "V")
    p.dma("sp", V.ap, vec, w=[V.r], key="in")
    gs = p.sb([128, 2, NCH], F32, "gs")
    for cls in range(2):
        sc = V.ap[:, GV_MOD + cls * 96 + 16: GV_MOD + cls * 96 + 32]
        p.stt("dve", gs.ap[:, cls, :], sc, 1.0, V.ap[:, GV_GMIX:GV_GMIX + 16], ALU.add, ALU.mult, r=[V.r], w=[gs.r])
    buvb = p.sb([1, 2 * D], BF16, "buvb")
    p.dma("pool", buvb.ap, rows[0:1, :], w=[buvb.r], key="rows")
    lng = p.sb([128, D], F32, "lng")
    lnb = p.sb([128, D], F32, "lnb")
    p.dma("sp", lng.ap, rows[1:2, 0:D].partition_broadcast(128), w=[lng.r], key="in")
    p.dma("sp", lnb.ap, rows[2:3, 0:D].partition_broadcast(128), w=[lnb.r], key="in")
    ws = p.sb([128, 16, 128], BF16, "ws")
    p.dma("pool", ws.ap, wsT.rearrange("g q p -> q g p"), w=[ws.r], key="ws")
    Wuv = p.sb([128, NCH, 2 * D], BF16, "Wuv")
    load_w(p, Wuv, w_uv, key="w", nsplit=16)
    X = p.sb([128, NCH, 128], F32, "X")
    Y = p.sb([128, NCH, 128], F32, "Y")
    h = p.sb([128, NCH, 128], BF16, "h")
    sq = p.sb([128, NCH, 128], BF16, "sq")
    rstd = p.sb([128, 128], F32, "rstd")
    tmps = [p.sb([128, 128], F32, f"tmp{i}") for i in range(2)]
    u = p.sb([128, D], BF16, "u")
    v = p.sb([128, D], F32, "v")
    vln = p.sb([128, D], BF16, "vln")
    z = p.sb([128, D], BF16, "z")
    zst = sq
    st1 = p.sb([128, 1], F32, "st1")
    st2 = p.sb([128, 1], F32, "st2")
    outs = []
    for ti in range(NT):
        c0 = ti * 128
        cls = 0 if ti == 0 else 1
        load_fm(p, "sp", X, xa, c0, 128, "xa")
        load_fm(p, "sp", Y, yb, c0, 128, "yb")
        for kc in range(NCH):
            p.stt("dve", X.ap[:, kc, :], Y.ap[:, kc, :], V.ap[:, GV_GPREV + cls * 16 + kc: GV_GPREV + cls * 16 + kc + 1],
                  X.ap[:, kc, :], ALU.mult, ALU.add, r=[X.r, Y.r, V.r], w=[X.r])
        emit_rstd(p, c, X, 128, sq, p.banks[0], rstd)
        sh = (V.ap[:, GV_MOD + cls * 96: GV_MOD + cls * 96 + 16], V.r)
        emit_mod(p, X, rstd, (gs.ap[:, cls, :], gs.r), sh, 128, h, tmps=tmps)
        for cb in range(8):
            bank = p.banks[1 + cb % 3]
            for kc in range(NCH):
                p.matmul(bank.ap[:, 0:512], h.ap[:, kc, :], Wuv.ap[:, kc, cb * 512:(cb + 1) * 512], kc == 0, False,
                         r=[h.r, Wuv.r], w=[bank.r])
            p.matmul(bank.ap[:, 0:512], c["ones_bf"].ap[0:1, :], buvb.ap[0:1, cb * 512:(cb + 1) * 512], False, True,
                     r=[c["ones_bf"].r, buvb.r], w=[bank.r])
            if cb < 4:
                p.act(u.ap[:, cb * 512:(cb + 1) * 512], bank.ap[:, 0:512], AF.Gelu_apprx_tanh, r=[bank.r], w=[u.r])
            else:
                p.act(v.ap[:, (cb - 4) * 512:(cb - 3) * 512], bank.ap[:, 0:512], AF.Gelu_apprx_tanh, r=[bank.r], w=[v.r])
        p.op("dve", lambda e: e.tensor_reduce(out=st1.ap, in_=v.ap, axis=AX.X, op=ALU.add), [v.r], [st1.r])
        p.ts("dve", st1.ap, st1.ap, 1.0 / D, ALU.mult, r=[st1.r], w=[st1.r])
        p.ts("dve", v.ap, v.ap, st1.ap[:, 0:1], ALU.subtract, r=[v.r, st1.r], w=[v.r])
        p.tt("dve", z.ap, v.ap, v.ap, ALU.mult, r=[v.r], w=[z.r])
        p.op("dve", lambda e: e.tensor_reduce(out=st2.ap, in_=z.ap, axis=AX.X, op=ALU.add), [z.r], [st2.r])
        p.act(st2.ap, st2.ap, AF.Sqrt, r=[st2.r, c["eps"].r], w=[st2.r], bias=c["eps"].ap, scale=1.0 / D)
        p.op("dve", lambda e: e.reciprocal(out=st2.ap, in_=st2.ap), [st2.r], [st2.r])
        p.stt("dve", v.ap, v.ap, st2.ap[:, 0:1], lng.ap, ALU.mult, ALU.mult, r=[v.r, st2.r, lng.r], w=[v.r])
        p.tt("dve", vln.ap, v.ap, lnb.ap, ALU.add, r=[v.r, lnb.r], w=[vln.r])
        for g in range(16):
            bank = p.banks[4 + (g // 4) % 2]
            j = g % 4
            p.matmul(bank.ap[:, j * 128:(j + 1) * 128], ws.ap[:, g, :], vln.ap[:, g * 128:(g + 1) * 128], True, True,
                     r=[ws.r, vln.r], w=[bank.r])
            p.stt("dve", z.ap[:, g * 128:(g + 1) * 128], bank.ap[:, j * 128:(j + 1) * 128], V.ap[:, GV_BS + g: GV_BS + g + 1],
                  u.ap[:, g * 128:(g + 1) * 128], ALU.add, ALU.mult, r=[bank.r, V.r, u.r], w=[z.r])
        for half in range(2):
            tb = p.banks[6 + half]
            tbv = tb.ap.bitcast(BF16)
            for k8 in range(8):
                kc = half * 8 + k8
                p.transpose(tbv[:, k8 * 128:(k8 + 1) * 128], z.ap[:, kc * 128:(kc + 1) * 128], c["ident_bf"].ap,
                            r=[z.r, c["ident_bf"].r], w=[tb.r])
            p.copy("act", zst.ap[:, half * 8:(half + 1) * 8, :], tbv[:, 0:1024].rearrange("p (a b) -> p a b", a=8), r=[tb.r], w=[zst.r])
        outs.append(store_fm(p, "sp", zout, c0, 128, zst, "out"))
    p.emit(final_waits=outs[-1:])
    return nc


def gmlp_vec(gprev, mod, layer, slot, inp):
    cols = [pp(gprev[0]), pp(gprev[1])]
    for cls in range(2):
        for k in range(6):
            cols.append(pp(mod[cls, k]))
    cols.append(pp(inp["norm_mix_g"][layer]))
    cols.append(np.ascontiguousarray(np.asarray(inp["gmlp_b_s"][slot]).T))
    v = np.concatenate(cols, axis=1).astype(np.float32)
    assert v.shape == (128, GM_NV)
    return v


def gmlp_rows(slot, inp):
    r = np.zeros((3, 2 * D), np.float32)
    r[0] = inp["gmlp_b_uv"][slot]
    r[1, :D] = inp["gmlp_ln_g"][slot]
    r[2, :D] = inp["gmlp_ln_b"][slot]
    return r


TF = S // 2


def build_final():
    nc = new_nc()
    p = Prog(nc)
    xa = nc.dram_tensor("xa", [D, TF], F32, kind="ExternalInput").ap()
    yb = nc.dram_tensor("yb", [D, TF], F32, kind="ExternalInput").ap()
    vec = nc.dram_tensor("vec", [128, 48], F32, kind="ExternalInput").ap()
    out = nc.dram_tensor("out", [D, TF], F32, kind="ExternalOutput").ap()
    c = make_consts(p)
    V = p.sb([128, 48], F32, "V")
    p.dma("sp", V.ap, vec, w=[V.r], key="in")
    NB = 256
    Xb = [p.sb([128, NCH, NB], F32, f"X{i}") for i in range(2)]
    Yb = [p.sb([128, NCH, NB], F32, f"Y{i}") for i in range(2)]
    sq = p.sb([128, NCH, NB], BF16, "sq")
    rstd = p.sb([128, NB], F32, "rstd")
    outs = []
    for bi in range(TF // NB):
        c0 = bi * NB
        X = Xb[bi % 2]
        Y = Yb[bi % 2]
        load_fm(p, "sp", X, xa, c0, NB, "xa")
        load_fm(p, "sp", Y, yb, c0, NB, "yb")
        for kc in range(NCH):
            p.stt("dve", X.ap[:, kc, :], Y.ap[:, kc, :], V.ap[:, kc:kc + 1], X.ap[:, kc, :], ALU.mult, ALU.add,
                  r=[X.r, Y.r, V.r], w=[X.r])
        emit_rstd(p, c, X, NB, sq, p.banks[bi % 2], rstd)
        emit_mod(p, X, rstd, (V.ap[:, 16:32], V.r), (V.ap[:, 32:48], V.r), NB, None, out_f32=Y)
        outs.append(store_fm(p, "sp", out, c0, NB, Y, "out"))
    p.emit(final_waits=outs[-1:])
    return nc


_PROGS = {}


def _prog(name, fn):
    if name not in _PROGS:
        _PROGS[name] = fn()
    return _PROGS[name]


def _run(nc, maps):
    res = run_bass_kernel_spmd(nc, maps, core_ids=list(range(len(maps))))
    return res.results


def _core_cols(hf):
    return np.r_[hf * 128:(hf + 1) * 128, CTX + hf * (S // 2): CTX + (hf + 1) * (S // 2)]


def kernel(**inputs):
    inp = {k: np.asarray(v) for k, v in inputs.items()}
    f32 = np.float32
    c5 = np.concatenate([inp["c"], inp["c_ctx"][None]], 0).astype(f32)
    cT = np.ascontiguousarray(c5.T.reshape(NCH, 128, 5).transpose(1, 0, 2))
    maps = [{"cT": cT,
             "w": np.ascontiguousarray(inp["ada_w"][:, :, j * ADA_COLS:(j + 1) * ADA_COLS]),
             "b": np.ascontiguousarray(inp["ada_b"][:, None, j * ADA_COLS:(j + 1) * ADA_COLS])} for j in range(8)]
    r = _run(_prog("ada", build_ada), maps)
    mod = np.concatenate([x["mod"] for x in r], axis=2)

    def modb(layer, b):
        return np.stack([mod[layer, 4].reshape(6, D), mod[layer, b].reshape(6, D)], 0)

    xa = [np.ascontiguousarray(np.concatenate([inp["ctx"][b], inp["x"][b]], 0).T.astype(f32)) for b in range(B)]
    yb = [np.zeros((D, TB), f32) for _ in range(B)]
    gprev = [np.zeros((2, D), f32) for _ in range(B)]
    cosT, sinT, Rm = rope_tables()
    for layer in range(DEPTH):
        kind, slot = layer % 3, layer // 3
        maps = []
        for core in range(8):
            b, hf = core // 2, core % 2
            m = modb(layer, b)
            if kind == 0:
                w = inp["lru_w_in"][slot]
                maps.append({"xa": xa[b], "yb": yb[b], "vec": lru_vec(gprev[b], m, layer, slot, hf, inp),
                             "w_in": np.ascontiguousarray(np.concatenate([w[:, hf * 1024:(hf + 1) * 1024],
                                                                          w[:, D + hf * 1024:D + (hf + 1) * 1024]], 1)),
                             "gw": np.ascontiguousarray(inp["lru_gate_w"][slot][:, :, hf * 8:(hf + 1) * 8])})
            elif kind == 1:
                wqkv = inp["diff_w_qkv"][slot]
                sl = slice(hf * 1024, (hf + 1) * 1024)
                maps.append({"xa": xa[b], "yb": yb[b], "vec": attn_vec(gprev[b], m, layer, slot, inp),
                             "lamv": np.ascontiguousarray(inp["diff_lambda"][slot].reshape(1, 256)),
                             "wq": np.ascontiguousarray(wqkv[:, 0:D][:, sl]),
                             "wk": np.ascontiguousarray(wqkv[:, D:2 * D][:, sl]),
                             "wv": np.ascontiguousarray(wqkv[:, 2 * D:3 * D][:, sl]),
                             "cosT": cosT, "sinT": sinT, "R": Rm})
            else:
                cols = _core_cols(hf)
                maps.append({"xa": np.ascontiguousarray(xa[b][:, cols]), "yb": np.ascontiguousarray(yb[b][:, cols]),
                             "vec": gmlp_vec(gprev[b], m, layer, slot, inp), "w_uv": inp["gmlp_w_uv"][slot],
                             "rows": gmlp_rows(slot, inp),
                             "wsT": np.ascontiguousarray(inp["gmlp_w_s"][slot].transpose(0, 2, 1))})
        if kind == 0:
            r = _run(_prog("lru", build_lru), maps)
            G = [np.concatenate([r[2 * b]["g"], r[2 * b + 1]["g"]], 0) for b in range(B)]
            w_out = inp["lru_w_out"][slot]
        elif kind == 1:
            li = 0.8 - 0.6 * math.exp(-0.3 * layer)
            r = _run(_prog("attn", lambda: build_attn(li)), maps)
            G = [np.concatenate([r[2 * b]["g"], r[2 * b + 1]["g"]], 0) for b in range(B)]
            w_out = inp["diff_w_out"][slot]
        else:
            r = _run(_prog("gmlp", build_gmlp), maps)
            G = []
            for b in range(B):
                g = np.empty((D, TB), dtype=r[0]["g"].dtype)
                for hf in range(2):
                    g[:, _core_cols(hf)] = r[2 * b + hf]["g"]
                G.append(g)
            w_out = inp["gmlp_w_out"][slot]
        del maps
        pw = post_weights(inp, layer)
        maps = [{"xa": xa[b], "yb": yb[b], "G": np.ascontiguousarray(G[b]),
                 "vec": post_vec(gprev[b], modb(layer, b), layer, inp), "w_out": w_out, **pw} for b in range(B)]
        r = _run(_prog("post", build_post), maps)
        del maps, pw
        for b in range(B):
            xa[b] = np.ascontiguousarray(r[b]["xmid"])
            yb[b] = np.ascontiguousarray(r[b]["ymoe"].T)
            m = modb(layer, b)
            gprev[b] = np.stack([m[0, 5], m[1, 5]], 0)
    maps = []
    for core in range(8):
        b, hf = core // 2, core % 2
        sl = slice(CTX + hf * TF, CTX + (hf + 1) * TF)
        vec = np.concatenate([pp(gprev[b][1]), pp(inp["norm_final_g"]), np.zeros((128, 16), f32)], 1).astype(f32)
        maps.append({"xa": np.ascontiguousarray(xa[b][:, sl]), "yb": np.ascontiguousarray(yb[b][:, sl]), "vec": vec})
    r = _run(_prog("final", build_final), maps)
    out = np.empty((B, S, D), f32)
    for core in range(8):
        b, hf = core // 2, core % 2
        out[b, hf * TF:(hf + 1) * TF, :] = r[core]["out"].T
    return out
```

```python
import contextlib
import math
import numpy as np
import concourse.bass as bass
import concourse.mybir as mybir
from concourse.bass_utils import run_bass_kernel_spmd

F32 = mybir.dt.float32
BF16 = mybir.dt.bfloat16
I32 = mybir.dt.int32
AF = mybir.ActivationFunctionType
ALU = mybir.AluOpType
AX = mybir.AxisListType

D = 2048
NCH = 16
B = 4
S = 4096
CTX = 256
DEPTH = 4
EPS = 1e-6


class Res:
    __slots__ = ("name", "ws", "rs", "excl", "multi")

    def __init__(self, name="", excl=False, multi=False):
        self.name = name
        self.ws = {}
        self.rs = {}
        self.excl = excl
        self.multi = multi


class Ins:
    __slots__ = ("eng", "fn", "deps", "needs_inc", "semval", "dkey", "idx", "group")

    def __init__(self, eng, fn, dkey=None):
        self.idx = 0
        self.group = None
        self.eng = eng
        self.fn = fn
        self.deps = []
        self.needs_inc = False
        self.semval = None
        self.dkey = dkey


class Buf:
    __slots__ = ("ap", "r")

    def __init__(self, ap, name=""):
        self.ap = ap
        self.r = Res(name)

    def __getitem__(self, k):
        return self.ap[k]


_DSZ = {F32: 4, BF16: 2, I32: 4}
ARENA_BYTES = 204 * 1024


class Prog:
    ENGS = ("pe", "act", "dve", "pool", "sp")

    def __init__(self, nc):
        self.nc = nc
        self.lists = {e: [] for e in self.ENGS}
        self.last = {}
        self.ngroup = 0
        self.stack = contextlib.ExitStack()
        self.arena = self.stack.enter_context(nc.sbuf_tensor("arena", [128, ARENA_BYTES // 4], F32))
        self.off = 0
        self.banks = []
        for i in range(8):
            t = self.stack.enter_context(nc.psum_tensor(f"bank{i}", [128, 512], F32))
            bk = Buf(t[:], f"bank{i}")
            bk.r.excl = True
            self.banks.append(bk)

    def sb(self, shape, dtype, name=""):
        n = 1
        for v in shape[1:]:
            n *= v
        nbytes = (n * _DSZ[dtype] + 63) // 64 * 64
        w = nbytes // 4
        assert self.off + w <= ARENA_BYTES // 4, f"SBUF arena overflow allocating {name} {shape}"
        v = self.arena[0:shape[0], self.off:self.off + w]
        self.off += w
        if dtype != F32:
            v = v.bitcast(dtype)
        v = v[:, 0:n]
        if len(shape) > 2:
            names = " ".join(f"d{i}" for i in range(len(shape) - 1))
            kw = {f"d{i}": shape[i + 1] for i in range(len(shape) - 2)}
            v = v.rearrange(f"p ({names}) -> p {names}", **kw)
        return Buf(v, name)

    def newgroup(self):
        self.ngroup += 1
        return self.ngroup

    def mark(self):
        return self.off

    def release(self, m):
        self.off = m

    def dram(self, name, shape, dtype, kind="Internal"):
        return self.nc.dram_tensor(name, list(shape), dtype, kind=kind).ap()

    def op(self, eng, fn, reads=(), writes=(), dkey=None, group=None):
        ins = Ins(eng, fn, dkey)
        ins.idx = len(self.lists[eng])
        ins.group = group
        deps = {}

        def add(d):
            if d is None or d is ins:
                return
            if d.eng == "pe" and eng == "pe" and d.dkey is None and dkey is None:
                return
            if group is not None and d.group == group:
                return
            key = (d.eng, d.dkey)
            o = deps.get(key)
            if o is None or o.idx < d.idx:
                deps[key] = d

        me = (eng, dkey)
        for r in reads:
            for d in r.ws.values():
                add(d)
            if r.excl:
                for k_, d in r.rs.items():
                    if k_ != me:
                        add(d)
        for w in writes:
            for d in w.rs.values():
                add(d)
            if not w.multi:
                for d in w.ws.values():
                    add(d)
        ins.deps = list(deps.values())
        for d in ins.deps:
            d.needs_inc = True
        for r in reads:
            r.rs[me] = ins
        for w in writes:
            if w.multi:
                w.ws[me] = ins
            else:
                w.ws = {me: ins}
            w.rs = {}
        self.lists[eng].append(ins)
        self.last[me] = ins
        return ins

    def barrier(self):
        lasts = list(self.last.values())
        for e in self.ENGS:
            ins = Ins(e, lambda eng: eng.nop(), None)
            ins.idx = len(self.lists[e])
            ins.deps = [l for l in lasts if not (l.eng == e and l.dkey is None)]
            for d in ins.deps:
                d.needs_inc = True
            self.lists[e].append(ins)
            self.last[(e, None)] = ins

    def dma(self, q, out, in_, r=(), w=(), key=None, group=None, **kw):
        side = None
        for x in list(w) + list(r):
            if not x.multi:
                side = x
                break
        dk = (q, id(side) if side is not None else key)
        return self.op(q, lambda e: e.dma_start(out=out, in_=in_, **kw), r, w, dkey=dk, group=group)

    def matmul(self, out, lhsT, rhs, start, stop, r=(), w=()):
        return self.op("pe", lambda e: e.matmul(out, lhsT, rhs, start=start, stop=stop), r, w)

    def transpose(self, out, in_, ident, r=(), w=()):
        return self.op("pe", lambda e: e.transpose(out, in_, ident), r, w)

    def act(self, out, in_, func, r=(), w=(), bias=None, scale=None, eng="act"):
        kw = {}
        if bias is not None:
            kw["bias"] = bias
        if scale is not None:
            kw["scale"] = scale
        return self.op(eng, lambda e: e.activation(out=out, in_=in_, func=func, **kw), r, w)

    def tt(self, eng, out, in0, in1, op, r=(), w=()):
        return self.op(eng, lambda e: e.tensor_tensor(out=out, in0=in0, in1=in1, op=op), r, w)

    def ts(self, eng, out, in0, s1, op0, s2=None, op1=None, r=(), w=()):
        if op1 is None:
            return self.op(eng, lambda e: e.tensor_scalar(out=out, in0=in0, scalar1=s1, scalar2=None, op0=op0), r, w)
        return self.op(eng, lambda e: e.tensor_scalar(out=out, in0=in0, scalar1=s1, scalar2=s2, op0=op0, op1=op1), r, w)

    def stt(self, eng, out, in0, scalar, in1, op0, op1, r=(), w=()):
        return self.op(eng, lambda e: e.scalar_tensor_tensor(out=out, in0=in0, scalar=scalar, in1=in1, op0=op0, op1=op1), r, w)

    def copy(self, eng, out, in_, r=(), w=()):
        if eng == "act":
            return self.op(eng, lambda e: e.copy(out=out, in_=in_), r, w)
        return self.op(eng, lambda e: e.tensor_copy(out=out, in_=in_), r, w)

    def memset(self, eng, out, val, w=()):
        return self.op(eng, lambda e: e.memset(out, val), (), w)

    def emit(self, final_waits=()):
        nc = self.nc
        final_waits = [ins for k, ins in self.last.items() if k[1] is not None]
        for ins in final_waits:
            ins.needs_inc = True
        esem = {}
        dsem = {}
        for e in self.ENGS:
            cnt = 0
            dcnt = {}
            for ins in self.lists[e]:
                if ins.dkey is not None:
                    dcnt[ins.dkey] = dcnt.get(ins.dkey, 0) + 16
                    ins.semval = dcnt[ins.dkey]
                    dsem.setdefault(ins.dkey, None)
                elif ins.needs_inc:
                    cnt += 1
                    ins.semval = cnt
            esem[e] = None
        for e in self.ENGS:
            esem[e] = self.stack.enter_context(nc.semaphore(f"s_{e}"))
        for i, k in enumerate(dsem):
            dsem[k] = self.stack.enter_context(nc.semaphore(f"d_{i}"))

        def sem_of(ins):
            return dsem[ins.dkey] if ins.dkey is not None else esem[ins.eng]

        def run(e, eng):
            waited = {}
            for ins in self.lists[e]:
                for d in ins.deps:
                    s = sem_of(d)
                    k = id(s)
                    if waited.get(k, 0) < d.semval:
                        eng.wait_ge(s, d.semval)
                        waited[k] = d.semval
                bi = ins.fn(eng)
                if ins.dkey is not None:
                    bi.then_inc(dsem[ins.dkey], 16)
                elif ins.needs_inc:
                    bi.then_inc(esem[e], 1)
            if e == "sp":
                for d in final_waits:
                    s = sem_of(d)
                    if waited.get(id(s), 0) < d.semval:
                        eng.wait_ge(s, d.semval)
                        waited[id(s)] = d.semval

        with nc.Block() as block:
            @block.tensor
            def _(eng):
                run("pe", eng)

            @block.scalar
            def _(eng):
                run("act", eng)

            @block.vector
            def _(eng):
                run("dve", eng)

            @block.gpsimd
            def _(eng):
                run("pool", eng)

            @block.sync
            def _(eng):
                run("sp", eng)
        self.stack.close()


def new_nc():
    return bass.Bass("TRN2", target_bir_lowering=False)


def make_consts(p):
    c = {}
    c["ones_bf"] = p.sb([128, 128], BF16, "ones_bf")
    p.memset("pool", c["ones_bf"].ap, 1.0, w=[c["ones_bf"].r])
    c["eps"] = p.sb([128, 1], F32, "eps")
    p.memset("pool", c["eps"].ap, EPS, w=[c["eps"].r])
    idf = p.sb([128, 128], F32, "ident_f")
    p.memset("pool", idf.ap, 0.0, w=[idf.r])
    p.op("pool", lambda e: e.affine_select(out=idf.ap, in_=idf.ap, pattern=[[-1, 128]], compare_op=ALU.not_equal,
                                           fill=1.0, base=0, channel_multiplier=1), [idf.r], [idf.r])
    c["ident_f"] = idf
    c["ident_bf"] = p.sb([128, 128], BF16, "ident_bf")
    p.copy("pool", c["ident_bf"].ap, idf.ap, r=[idf.r], w=[c["ident_bf"].r])
    return c


def emit_rstd(p, c, x, N, sq, bank, rstd):
    p.act(sq.ap[:, :, 0:N], x.ap[:, :, 0:N], AF.Square, r=[x.r], w=[sq.r])
    for kc in range(NCH):
        p.matmul(bank.ap[:, 0:N], c["ones_bf"].ap, sq.ap[:, kc, 0:N], kc == 0, kc == NCH - 1,
                 r=[c["ones_bf"].r, sq.r], w=[bank.r])
    p.act(rstd.ap[:, 0:N], bank.ap[:, 0:N], AF.Sqrt, r=[bank.r, c["eps"].r], w=[rstd.r], bias=c["eps"].ap, scale=1.0 / D)
    p.op("dve", lambda e: e.reciprocal(out=rstd.ap[:, 0:N], in_=rstd.ap[:, 0:N]), [rstd.r], [rstd.r])


def emit_mod(p, x, rstd, gs, sh, N, out_bf, tmps=None, out_f32=None):
    gs_ap, gs_r = gs
    sh_ap, sh_r = sh
    for cch in range(NCH):
        if out_f32 is not None:
            dst = out_f32.ap[:, cch, 0:N]
            dr = out_f32.r
        else:
            t = tmps[cch % len(tmps)]
            dst = t.ap[:, 0:N]
            dr = t.r
        p.stt("dve", dst, x.ap[:, cch, 0:N], gs_ap[:, cch:cch + 1], rstd.ap[:, 0:N], ALU.mult, ALU.mult,
              r=[x.r, gs_r, rstd.r], w=[dr])
        if out_f32 is not None:
            p.act(dst, dst, AF.Identity, r=[dr, sh_r], w=[dr], bias=sh_ap[:, cch:cch + 1])
        else:
            p.act(out_bf.ap[:, cch, 0:N], dst, AF.Identity, r=[dr, sh_r], w=[out_bf.r], bias=sh_ap[:, cch:cch + 1])
    if out_f32 is not None and out_bf is not None:
        p.copy("pool", out_bf.ap[:, :, 0:N], out_f32.ap[:, :, 0:N], r=[out_f32.r], w=[out_bf.r])


def load_w(p, dst, w2d, key, nsplit=4, q="pool"):
    K = w2d.shape[0]
    kc = K // 128
    src = w2d.rearrange("(kc p) n -> p kc n", p=128)
    step = max(1, kc // nsplit)
    grp = p.newgroup()
    for k0 in range(0, kc, step):
        p.dma(q, dst.ap[:, k0:k0 + step, :], src[:, k0:k0 + step, :], w=[dst.r], key=key, group=grp)


ADA_COLS = 6 * D // 8


def build_ada():
    nc = new_nc()
    p = Prog(nc)
    cT = nc.dram_tensor("cT", [128, NCH, 5], F32, kind="ExternalInput").ap()
    w = nc.dram_tensor("w", [DEPTH, D, ADA_COLS], F32, kind="ExternalInput").ap()
    bia = nc.dram_tensor("b", [DEPTH, 1, ADA_COLS], F32, kind="ExternalInput").ap()
    out = nc.dram_tensor("mod", [DEPTH, 5, ADA_COLS], F32, kind="ExternalOutput").ap()
    s = p.sb([128, NCH, 5], F32, "s")
    p.dma("sp", s.ap, cT, w=[s.r], key="in")
    p.act(s.ap, s.ap, AF.Silu, r=[s.r], w=[s.r])
    wb = [p.sb([128, NCH, 512], F32, f"wb{i}") for i in range(2)]
    bb = [p.sb([5, 512], F32, f"bb{i}") for i in range(2)]
    ob = [p.sb([5, 512], F32, f"ob{i}") for i in range(2)]
    outs = []
    it = 0
    for l in range(DEPTH):
        for nb in range(ADA_COLS // 512):
            W = wb[it % 2]
            bt = bb[it % 2]
            o = ob[it % 2]
            bank = p.banks[it % 2]
            src = w[l, :, nb * 512:(nb + 1) * 512].rearrange("(kc p) n -> p kc n", p=128)
            grp = p.newgroup()
            for k0 in range(0, NCH, 4):
                p.dma("sp", W.ap[:, k0:k0 + 4, :], src[:, k0:k0 + 4, :], w=[W.r], key="w", group=grp)
            p.dma("sp", bt.ap, bia[l, :, nb * 512:(nb + 1) * 512].partition_broadcast(5), w=[bt.r], key="b")
            for kc in range(NCH):
                p.matmul(bank.ap[0:5, :], s.ap[:, kc, :], W.ap[:, kc, :], kc == 0, kc == NCH - 1, r=[s.r, W.r], w=[bank.r])
            p.tt("dve", o.ap, bank.ap[0:5, :], bt.ap, ALU.add, r=[bank.r, bt.r], w=[o.r])
            outs.append(p.dma("sp", out[l, :, nb * 512:(nb + 1) * 512], o.ap, r=[o.r], key="out"))
            it += 1
    p.emit(final_waits=outs[-1:])
    return nc


def pp(v):
    v = np.asarray(v)
    return np.ascontiguousarray(v.reshape(-1, 128).T)


def fm_src(x2d, c0, n):
    return x2d[:, c0:c0 + n].rearrange("(kc p) n -> p kc n", p=128)


def load_fm(p, q, dst, x2d, c0, n, key, nsplit=2):
    src = fm_src(x2d, c0, n)
    step = NCH // nsplit
    grp = p.newgroup()
    for k0 in range(0, NCH, step):
        p.dma(q, dst.ap[:, k0:k0 + step, 0:n], src[:, k0:k0 + step, :], w=[dst.r], key=key, group=grp)


def store_fm(p, q, x2d, c0, n, src, key, nsplit=2):
    dst = fm_src(x2d, c0, n)
    step = NCH // nsplit
    out = None
    grp = p.newgroup()
    for k0 in range(0, NCH, step):
        out = p.dma(q, dst[:, k0:k0 + step, :], src.ap[:, k0:k0 + step, 0:n], r=[src.r], key=key, group=grp)
    return out


TB = CTX + S
LRU_NV = 328
LV_GPREV, LV_MOD, LV_GMIX, LV_CW, LV_CB, LV_GB, LV_LAM = 0, 32, 224, 240, 272, 280, 312


def build_lru():
    nc = new_nc()
    p = Prog(nc)
    xa = nc.dram_tensor("xa", [D, TB], F32, kind="ExternalInput").ap()
    yb = nc.dram_tensor("yb", [D, TB], F32, kind="ExternalInput").ap()
    vec = nc.dram_tensor("vec", [128, LRU_NV], F32, kind="ExternalInput").ap()
    w_in = nc.dram_tensor("w_in", [D, 2048], F32, kind="ExternalInput").ap()
    gw = nc.dram_tensor("gw", [2, 2, 8, 128, 128], F32, kind="ExternalInput").ap()
    gout = nc.dram_tensor("g", [1024, TB], BF16, kind="ExternalOutput").ap()
    scr = p.dram("scr", [16, 128, TB], F32)
    scr_r = Res("scr", multi=True)
    c = make_consts(p)
    V = p.sb([128, LRU_NV], F32, "V")
    p.dma("sp", V.ap, vec, w=[V.r], key="in")
    one = p.sb([128, 1], F32, "one")
    p.memset("pool", one.ap, 1.0, w=[one.r])
    gs = p.sb([128, 2, NCH], F32, "gs")
    for cls in range(2):
        sc = V.ap[:, LV_MOD + cls * 96 + 16: LV_MOD + cls * 96 + 32]
        p.stt("dve", gs.ap[:, cls, :], sc, 1.0, V.ap[:, LV_GMIX:LV_GMIX + 16], ALU.add, ALU.mult, r=[V.r], w=[gs.r])
    ca = p.sb([128, 16], F32, "ca")
    c2 = p.sb([128, 16], F32, "c2")
    p.act(ca.ap, V.ap[:, LV_LAM:LV_LAM + 16], AF.Exp, r=[V.r], w=[ca.r], scale=-1.0)
    p.act(ca.ap, ca.ap, AF.Ln, r=[ca.r, one.r], w=[ca.r], bias=one.ap)
    p.ts("dve", c2.ap, ca.ap, -16.0, ALU.mult, r=[ca.r], w=[c2.r])
    p.ts("dve", ca.ap, ca.ap, -8.0, ALU.mult, r=[ca.r], w=[ca.r])
    gw_sb = p.sb([128, 32, 128], BF16, "gw")
    p.dma("pool", gw_sb.ap, gw.rearrange("d r h i o -> i (d r h) o"), w=[gw_sb.r], key="gw")
    m0 = p.mark()

    NB = 256
    w_sb = p.sb([128, NCH, 2048], BF16, "w_in")
    load_w(p, w_sb, w_in, key="w", nsplit=8)
    xab = [p.sb([128, NCH, NB], F32, f"xa{i}") for i in range(2)]
    ybb = [p.sb([128, NCH, NB], F32, f"yb{i}") for i in range(2)]
    h = p.sb([128, NCH, NB], BF16, "h")
    sq = p.sb([128, NCH, NB], BF16, "sq")
    rstd = p.sb([128, NB], F32, "rstd")
    tmps = [p.sb([128, NB], F32, f"tmp{i}") for i in range(2)]
    stage = p.sb([128, NCH, NB], F32, "stage")
    nblk = TB // NB
    for bi in range(nblk):
        c0 = bi * NB
        cls = 0 if bi == 0 else 1
        X = xab[bi % 2]
        Y = ybb[bi % 2]
        load_fm(p, "sp", X, xa, c0, NB, "xa")
        load_fm(p, "sp", Y, yb, c0, NB, "yb")
        for kc in range(NCH):
            p.stt("dve", X.ap[:, kc, :], Y.ap[:, kc, :], V.ap[:, LV_GPREV + cls * 16 + kc: LV_GPREV + cls * 16 + kc + 1],
                  X.ap[:, kc, :], ALU.mult, ALU.add, r=[X.r, Y.r, V.r], w=[X.r])
        emit_rstd(p, c, X, NB, sq, p.banks[0], rstd)
        sh = (V.ap[:, LV_MOD + cls * 96: LV_MOD + cls * 96 + 16], V.r)
        emit_mod(p, X, rstd, (gs.ap[:, cls, :], gs.r), sh, NB, h, tmps=tmps)
        for oc in range(16):
            bank = p.banks[1 + oc % 4]
            for kc in range(NCH):
                p.matmul(bank.ap[:, 0:NB], w_sb.ap[:, kc, oc * 128:(oc + 1) * 128], h.ap[:, kc, :], kc == 0, kc == NCH - 1,
                         r=[w_sb.r, h.r], w=[bank.r])
            p.copy("act" if oc % 2 == 0 else "dve", stage.ap[:, oc, :], bank.ap[:, 0:NB], r=[bank.r], w=[stage.r])
        dst = scr[:, :, c0:c0 + NB].rearrange("oc p t -> p oc t")
        grp = p.newgroup()
        for k0 in range(0, 16, 8):
            p.dma("sp", dst[:, k0:k0 + 8, :], stage.ap[:, k0:k0 + 8, :], r=[stage.r], w=[scr_r], key="scr", group=grp)

    p.barrier()
    p.release(m0)
    rec = p.sb([128, TB], F32, "rec")
    gat = p.sb([128, TB], F32, "gat")
    xc = p.sb([128, TB], F32, "xc")
    xcb = p.sb([128, TB], BF16, "xcb")
    Rb = p.sb([128, TB], F32, "Rb")
    Ib = p.sb([128, TB], F32, "Ib")
    Eb = p.sb([128, TB], F32, "Eb")
    H = [p.sb([128, TB], F32, f"H{i}") for i in range(2)]
    ob = p.sb([128, TB], BF16, "ob")
    segs = [(0, CTX), (CTX, TB)]
    outs = []
    for j in range(8):
        p.dma("sp", rec.ap, scr[j], r=[scr_r], w=[rec.r], key="rec")
        p.dma("sp", gat.ap, scr[8 + j], r=[scr_r], w=[gat.r], key="gat")
        cw = lambda k: V.ap[:, LV_CW + j * 4 + k: LV_CW + j * 4 + k + 1]
        p.ts("dve", xc.ap, rec.ap, cw(2), ALU.mult, V.ap[:, LV_CB + j:LV_CB + j + 1], ALU.add, r=[rec.r, V.r], w=[xc.r])
        for (s0, e0) in segs:
            for k, off in ((0, -2), (1, -1), (3, 1)):
                if off < 0:
                    o_sl = slice(s0 - off, e0)
                    i_sl = slice(s0, e0 + off)
                else:
                    o_sl = slice(s0, e0 - off)
                    i_sl = slice(s0 + off, e0)
                p.stt("dve", xc.ap[:, o_sl], rec.ap[:, i_sl], cw(k), xc.ap[:, o_sl], ALU.mult, ALU.add,
                      r=[rec.r, xc.r, V.r], w=[xc.r])
        p.copy("act", xcb.ap, xc.ap, r=[xc.r], w=[xcb.r])
        for d in range(2):
            for which, dstb in ((0, Rb), (1, Ib)):
                gi = (d * 2 + which) * 8 + j
                bcol = V.ap[:, LV_GB + gi: LV_GB + gi + 1]
                for bi, t0 in enumerate(range(0, TB, 512)):
                    n = min(512, TB - t0)
                    bank = p.banks[bi % 4]
                    p.matmul(bank.ap[:, 0:n], gw_sb.ap[:, gi, :], xcb.ap[:, t0:t0 + n], True, True, r=[gw_sb.r, xcb.r], w=[bank.r])
                    p.act(dstb.ap[:, t0:t0 + n], bank.ap[:, 0:n], AF.Sigmoid, r=[bank.r, V.r], w=[dstb.r], bias=bcol)
            li = d * 8 + j
            p.act(Eb.ap, Rb.ap, AF.Exp, r=[Rb.r, c2.r], w=[Eb.r], scale=c2.ap[:, li:li + 1])
            p.act(Rb.ap, Rb.ap, AF.Exp, r=[Rb.r, ca.r], w=[Rb.r], scale=ca.ap[:, li:li + 1])
            p.act(Eb.ap, Eb.ap, AF.Sqrt, r=[Eb.r, one.r], w=[Eb.r], bias=one.ap, scale=-1.0)
            p.tt("dve", Ib.ap, Ib.ap, Eb.ap, ALU.mult, r=[Ib.r, Eb.r], w=[Ib.r])
            p.tt("dve", Ib.ap, Ib.ap, xc.ap, ALU.mult, r=[Ib.r, xc.r], w=[Ib.r])
            Hd = H[d]
            if d == 0:
                p.op("dve", lambda e, Hd=Hd: e.tensor_tensor_scan(out=Hd.ap, data0=Rb.ap, data1=Ib.ap, initial=0.0,
                                                                   op0=ALU.mult, op1=ALU.add), [Rb.r, Ib.r], [Hd.r])
            else:
                p.op("dve", lambda e, Hd=Hd: e.tensor_tensor_scan(out=Hd.ap[:, 0:CTX][:, ::-1], data0=Rb.ap[:, 0:CTX][:, ::-1],
                                                                   data1=Ib.ap[:, 0:CTX][:, ::-1], initial=0.0,
                                                                   op0=ALU.mult, op1=ALU.add), [Rb.r, Ib.r], [Hd.r])
                p.op("dve", lambda e, Hd=Hd: e.tensor_tensor_scan(out=Hd.ap[:, CTX:TB][:, ::-1], data0=Rb.ap[:, CTX:TB][:, ::-1],
                                                                   data1=Ib.ap[:, CTX:TB][:, ::-1], initial=Hd.ap[:, 0:1],
                                                                   op0=ALU.mult, op1=ALU.add), [Rb.r, Ib.r, Hd.r], [Hd.r])
        p.tt("dve", H[0].ap, H[0].ap, H[1].ap, ALU.add, r=[H[0].r, H[1].r], w=[H[0].r])
        p.act(gat.ap, gat.ap, AF.Gelu_apprx_tanh, r=[gat.r], w=[gat.r])
        p.tt("dve", ob.ap, H[0].ap, gat.ap, ALU.mult, r=[H[0].r, gat.r], w=[ob.r])
        outs.append(p.dma("sp", gout[j * 128:(j + 1) * 128, :], ob.ap, r=[ob.r], key="out"))
    p.emit(final_waits=outs[-1:])
    return nc


def lru_vec(gprev, mod, layer, slot, hf, inp):
    ch = slice(hf * 1024, (hf + 1) * 1024)
    cols = [pp(gprev[0]), pp(gprev[1])]
    for cls in range(2):
        for k in range(6):
            cols.append(pp(mod[cls, k]))
    cols.append(pp(inp["norm_mix_g"][layer]))
    cw = inp["lru_conv_w"][slot][:, ch]
    cols.append(np.ascontiguousarray(cw.reshape(4, 8, 128).transpose(2, 1, 0).reshape(128, 32)))
    cols.append(pp(inp["lru_conv_b"][slot][ch]))
    gb = inp["lru_gate_b"][slot][:, :, ch]
    cols.append(np.ascontiguousarray(gb.reshape(2, 2, 8, 128).transpose(3, 0, 1, 2).reshape(128, 32)))
    lam = inp["lru_lambda"][slot][:, ch]
    cols.append(np.ascontiguousarray(lam.reshape(2, 8, 128).transpose(2, 0, 1).reshape(128, 16)))
    v = np.concatenate(cols, axis=1).astype(np.float32)
    assert v.shape == (128, LRU_NV)
    return v


TC = 128 + S // 2
NT = TC // 128
PCORES = 4
TP = TB
NTP = TP // 128
NBP = (2 * TP + 32 * 127 + 127) // 128
NSP = NBP * 128
PV_GPREV, PV_MOD, PV_GFFN, POST_NV = 0, 32, 224, 240
BIG = 1.0e30
RC_N = 36 + NBP


def build_post():
    nc = new_nc()
    p = Prog(nc)
    xa = nc.dram_tensor("xa", [D, TP], F32, kind="ExternalInput").ap()
    yb = nc.dram_tensor("yb", [D, TP], F32, kind="ExternalInput").ap()
    G = nc.dram_tensor("G", [D, TP], BF16, kind="ExternalInput").ap()
    vec = nc.dram_tensor("vec", [128, POST_NV], F32, kind="ExternalInput").ap()
    w_out = nc.dram_tensor("w_out", [D, D], F32, kind="ExternalInput").ap()
    wr = nc.dram_tensor("wr", [D, 36], F32, kind="ExternalInput").ap()
    rc = nc.dram_tensor("rc", [1, RC_N], F32, kind="ExternalInput").ap()
    pcol = nc.dram_tensor("pcol", [128, 1], F32, kind="ExternalInput").ap()
    w1 = nc.dram_tensor("w1", [32 * 128, NCH * 512], F32, kind="ExternalInput").ap()
    w3 = nc.dram_tensor("w3", [32 * 128, NCH * 512], F32, kind="ExternalInput").ap()
    w2 = nc.dram_tensor("w2", [32 * 128, 4 * D], F32, kind="ExternalInput").ap()
    xmid = nc.dram_tensor("xmid", [D, TP], F32, kind="ExternalOutput").ap()
    ymoe = nc.dram_tensor("ymoe", [TP, D], F32, kind="ExternalOutput").ap()
    fd = p.dram("fd", [TP, D], BF16)
    xs = p.dram("xs", [NSP, D], BF16)
    ys = p.dram("ys", [NSP, D], F32)
    fd_r = Res("fd", multi=True)
    xs_r = Res("xs", multi=True)
    ys_r = Res("ys", multi=True)
    regs = {}

    def bnd_reg(e):
        if "b" not in regs:
            regs["b"] = e.to_reg(32 * 128 - 1)
        return regs["b"]

    c = make_consts(p)
    V = p.sb([128, POST_NV], F32, "V")
    p.dma("sp", V.ap, vec, w=[V.r], key="in")
    RC = p.sb([128, RC_N], F32, "RC")
    p.dma("sp", RC.ap, rc.partition_broadcast(128), w=[RC.r], key="in")
    PC = p.sb([128, 1], F32, "PC")
    p.dma("sp", PC.ap, pcol, w=[PC.r], key="in")
    gs = p.sb([128, 2, NCH], F32, "gs")
    for cls in range(2):
        sc = V.ap[:, PV_MOD + cls * 96 + 64: PV_MOD + cls * 96 + 80]
        p.stt("dve", gs.ap[:, cls, :], sc, 1.0, V.ap[:, PV_GFFN:PV_GFFN + 16], ALU.add, ALU.mult, r=[V.r], w=[gs.r])
    U = p.sb([128, 128], BF16, "U")
    uf = p.sb([128, 128], F32, "uf")
    p.memset("pool", uf.ap, 0.0, w=[uf.r])
    p.op("pool", lambda e: e.affine_select(out=uf.ap, in_=uf.ap, pattern=[[-1, 128]], compare_op=ALU.is_ge,
                                           fill=1.0, base=0, channel_multiplier=1), [uf.r], [uf.r])
    p.copy("pool", U.ap, uf.ap, r=[uf.r], w=[U.r])
    desti = p.sb([128, NTP, 2], I32, "desti")
    gall = p.sb([128, NTP, 2], F32, "gall")
    idxw = p.sb([128, NBP], I32, "idxw")
    m0 = p.mark()

    NB = 256
    cum = p.sb([128, 32], F32, "cum")
    p.memset("pool", cum.ap, 0.0, w=[cum.r])
    posall = p.sb([128, NTP, 32], F32, "posall")
    mkall = p.sb([128, NTP, 2, 32], F32, "mkall")
    wo = p.sb([128, NCH, D], BF16, "wo")
    load_w(p, wo, w_out, key="w", nsplit=8)
    wr_sb = p.sb([128, NCH, 36], F32, "wr")
    p.dma("sp", wr_sb.ap, wr.rearrange("(kc p) n -> p kc n", p=128), w=[wr_sb.r], key="in")
    Xb = [p.sb([128, NCH, NB], F32, f"X{i}") for i in range(2)]
    Y = p.sb([128, NCH, NB], F32, "Y")
    Gb = [p.sb([128, NCH, NB], BF16, f"G{i}") for i in range(2)]
    sq = p.sb([128, NCH, NB], BF16, "sq")
    fbf = p.sb([128, NCH, NB], BF16, "fbf")
    rstd = p.sb([128, NB], F32, "rstd")
    ftok = [p.sb([128, D], BF16, f"ftok{i}") for i in range(2)]
    Mt = [p.sb([128, 32], BF16, f"Mt{i}") for i in range(2)]

    def rt(name, shape, dt=F32):
        return [p.sb(shape, dt, f"{name}{i}") for i in range(2)]
    lg = rt("lg", [128, 36]); m4 = rt("m4", [128, 1]); d4 = rt("d4", [128, 4]); s4 = rt("s4", [128, 1])
    oh4 = rt("oh4", [128, 4]); lem = rt("lem", [128, 32]); lem2 = rt("lem2", [128, 32])
    top1 = rt("top1", [128, 1]); top2 = rt("top2", [128, 1]); d12 = rt("d12", [128, 1])
    blocks = [(CTX * 0, CTX)] + [(CTX + NB * i, NB) for i in range((TP - CTX) // NB)]
    xm_outs = []
    ti = 0
    for bi, (c0, N) in enumerate(blocks):
        cls = 0 if bi == 0 else 1
        X = Xb[bi % 2]
        Gt = Gb[bi % 2]
        load_fm(p, "sp", X, xa, c0, N, "xa")
        load_fm(p, "sp", Y, yb, c0, N, "yb")
        load_fm(p, "sp", Gt, G, c0, N, "G")
        for kc in range(NCH):
            p.stt("dve", X.ap[:, kc, 0:N], Y.ap[:, kc, 0:N], V.ap[:, PV_GPREV + cls * 16 + kc: PV_GPREV + cls * 16 + kc + 1],
                  X.ap[:, kc, 0:N], ALU.mult, ALU.add, r=[X.r, Y.r, V.r], w=[X.r])
        g1c = PV_MOD + cls * 96 + 32
        for oc in range(NCH):
            bank = p.banks[oc % 2]
            for kc in range(NCH):
                p.matmul(bank.ap[:, 0:N], wo.ap[:, kc, oc * 128:(oc + 1) * 128], Gt.ap[:, kc, 0:N], kc == 0, kc == NCH - 1,
                         r=[wo.r, Gt.r], w=[bank.r])
            p.stt("dve", X.ap[:, oc, 0:N], bank.ap[:, 0:N], V.ap[:, g1c + oc: g1c + oc + 1], X.ap[:, oc, 0:N], ALU.mult, ALU.add,
                  r=[bank.r, X.r, V.r], w=[X.r])
        xm_outs.append(store_fm(p, "sp", xmid, c0, N, X, "xmid"))
        emit_rstd(p, c, X, N, sq, p.banks[2], rstd)
        sh = (V.ap[:, PV_MOD + cls * 96 + 48: PV_MOD + cls * 96 + 64], V.r)
        emit_mod(p, X, rstd, (gs.ap[:, cls, :], gs.r), sh, N, fbf, out_f32=Y)
        for tt_ in range(N // 128):
            q = ti % 2
            tsl = slice(tt_ * 128, (tt_ + 1) * 128)
            rb = p.banks[3]
            for kc in range(NCH):
                p.matmul(rb.ap[:, 0:36], Y.ap[:, kc, tsl], wr_sb.ap[:, kc, :], kc == 0, kc == NCH - 1, r=[Y.r, wr_sb.r], w=[rb.r])
            L = lg[q]
            mk1 = mkall.ap[:, ti, 0, :]
            mk2 = mkall.ap[:, ti, 1, :]
            p.tt("dve", L.ap, rb.ap[:, 0:36], RC.ap[:, 0:36], ALU.add, r=[rb.r, RC.r], w=[L.r])
            p.op("dve", lambda e, o=m4[q], L=L: e.tensor_reduce(out=o.ap, in_=L.ap[:, 0:4], axis=AX.X, op=ALU.max), [L.r], [m4[q].r])
            p.ts("dve", d4[q].ap, L.ap[:, 0:4], m4[q].ap, ALU.subtract, r=[L.r, m4[q].r], w=[d4[q].r])
            p.act(d4[q].ap, d4[q].ap, AF.Exp, r=[d4[q].r], w=[d4[q].r])
            p.op("dve", lambda e, o=s4[q], i_=d4[q]: e.tensor_reduce(out=o.ap, in_=i_.ap, axis=AX.X, op=ALU.add), [d4[q].r], [s4[q].r])
            p.op("dve", lambda e, o=s4[q]: e.reciprocal(out=o.ap, in_=o.ap), [s4[q].r], [s4[q].r])
            p.ts("dve", oh4[q].ap, L.ap[:, 0:4], m4[q].ap, ALU.is_equal, r=[L.r, m4[q].r], w=[oh4[q].r])
            p.ts("dve", oh4[q].ap, oh4[q].ap, -1.0, ALU.add, BIG, ALU.mult, r=[oh4[q].r], w=[oh4[q].r])
            for g in range(4):
                p.ts("dve", lem[q].ap[:, 8 * g:8 * g + 8], L.ap[:, 4 + 8 * g:12 + 8 * g], oh4[q].ap[:, g:g + 1], ALU.add,
                     r=[L.r, oh4[q].r], w=[lem[q].r])
            p.op("dve", lambda e, o=top1[q], i_=lem[q]: e.tensor_reduce(out=o.ap, in_=i_.ap, axis=AX.X, op=ALU.max), [lem[q].r], [top1[q].r])
            p.ts("dve", mk1, lem[q].ap, top1[q].ap, ALU.is_equal, r=[lem[q].r, top1[q].r], w=[mkall.r])
            p.stt("dve", lem2[q].ap, mk1, -BIG, lem[q].ap, ALU.mult, ALU.add, r=[mkall.r, lem[q].r], w=[lem2[q].r])
            p.op("dve", lambda e, o=top2[q], i_=lem2[q]: e.tensor_reduce(out=o.ap, in_=i_.ap, axis=AX.X, op=ALU.max), [lem2[q].r], [top2[q].r])
            p.ts("dve", mk2, lem2[q].ap, top2[q].ap, ALU.is_equal, r=[lem2[q].r, top2[q].r], w=[mkall.r])
            p.tt("dve", d12[q].ap, top1[q].ap, top2[q].ap, ALU.subtract, r=[top1[q].r, top2[q].r], w=[d12[q].r])
            p.act(d12[q].ap, d12[q].ap, AF.Sigmoid, r=[d12[q].r], w=[d12[q].r])
            p.tt("dve", gall.ap[:, ti, 0:1], d12[q].ap, s4[q].ap, ALU.mult, r=[d12[q].r, s4[q].r], w=[gall.r])
            p.tt("dve", gall.ap[:, ti, 1:2], s4[q].ap, gall.ap[:, ti, 0:1], ALU.subtract, r=[s4[q].r, gall.r], w=[gall.r])
            p.tt("dve", Mt[q].ap, mk1, mk2, ALU.add, r=[mkall.r], w=[Mt[q].r])
            pb = p.banks[4]
            cb = p.banks[5]
            p.matmul(pb.ap[:, 0:32], U.ap, Mt[q].ap, True, True, r=[U.r, Mt[q].r], w=[pb.r])
            p.matmul(cb.ap[:, 0:32], c["ones_bf"].ap, Mt[q].ap, True, True, r=[c["ones_bf"].r, Mt[q].r], w=[cb.r])
            p.tt("dve", posall.ap[:, ti, :], pb.ap[:, 0:32], cum.ap, ALU.add, r=[pb.r, cum.r], w=[posall.r])
            p.tt("dve", cum.ap, cb.ap[:, 0:32], cum.ap, ALU.add, r=[cb.r, cum.r], w=[cum.r])
            F = ftok[q]
            for half in range(2):
                tb = p.banks[6 + half]
                tbv = tb.ap.bitcast(BF16)
                for k8 in range(8):
                    kc = half * 8 + k8
                    p.transpose(tbv[:, k8 * 128:(k8 + 1) * 128], fbf.ap[:, kc, tsl], c["ident_bf"].ap, r=[fbf.r, c["ident_bf"].r], w=[tb.r])
                p.copy("act", F.ap[:, half * 1024:(half + 1) * 1024], tbv[:, 0:1024], r=[tb.r], w=[F.r])
            p.dma("sp", fd[ti * 128:(ti + 1) * 128, :], F.ap, r=[F.r], w=[fd_r], key="fd")
            ti += 1
    assert ti == NTP

    padded = p.sb([128, 32], F32, "padded")
    pend = p.sb([128, 32], F32, "pend")
    pstart = p.sb([128, 32], F32, "pstart")
    onesr = p.sb([128, 32], F32, "onesr")
    be_f = p.sb([128, NBP], F32, "be_f")
    p.memset("pool", onesr.ap, 1.0, w=[onesr.r])
    p.memset("pool", padded.ap, 0.0, w=[padded.r])
    for m_ in range((2 * TP) // 128):
        p.stt("dve", padded.ap, cum.ap, float(128 * m_), padded.ap, ALU.is_gt, ALU.add, r=[cum.r, padded.r], w=[padded.r])
    p.ts("dve", padded.ap, padded.ap, 128.0, ALU.mult, r=[padded.r], w=[padded.r])
    p.op("dve", lambda e: e.tensor_tensor_scan(out=pend.ap, data0=onesr.ap, data1=padded.ap, initial=0.0, op0=ALU.mult, op1=ALU.add),
         [onesr.r, padded.r], [pend.r])
    p.tt("dve", pstart.ap, pend.ap, padded.ap, ALU.subtract, r=[pend.r, padded.r], w=[pstart.r])
    p.memset("pool", be_f.ap, 0.0, w=[be_f.r])
    for e_ in range(32):
        p.stt("dve", be_f.ap, RC.ap[:, 36:36 + NBP], pend.ap[:, e_:e_ + 1], be_f.ap, ALU.is_ge, ALU.add, r=[RC.r, pend.r, be_f.r], w=[be_f.r])
    p.ts("dve", be_f.ap, be_f.ap, 31.0, ALU.min, r=[be_f.r], w=[be_f.r])
    same = p.sb([128, NBP], F32, "same")
    p.memset("pool", same.ap, 0.0, w=[same.r])
    p.tt("dve", same.ap[:, 2:NBP], be_f.ap[:, 2:NBP], be_f.ap[:, 0:NBP - 2], ALU.is_equal, r=[be_f.r, same.r], w=[same.r])
    p.ts("dve", be_f.ap, be_f.ap, 128.0, ALU.mult, PC.ap[:, 0:1], ALU.add, r=[be_f.r, PC.r], w=[be_f.r])
    p.stt("dve", be_f.ap, same.ap, 1.0e6, be_f.ap, ALU.mult, ALU.add, r=[same.r, be_f.r], w=[be_f.r])
    p.copy("dve", idxw.ap, be_f.ap, r=[be_f.r], w=[idxw.r])
    pe_ = rt("pe", [128, 32]); t32 = rt("t32", [128, 32]); dstk = rt("dstk", [128, 1])
    for ti in range(NTP):
        q = ti % 2
        p.tt("dve", pe_[q].ap, posall.ap[:, ti, :], pstart.ap, ALU.add, r=[posall.r, pstart.r], w=[pe_[q].r])
        for k in range(2):
            p.tt("dve", t32[q].ap, mkall.ap[:, ti, k, :], pe_[q].ap, ALU.mult, r=[mkall.r, pe_[q].r], w=[t32[q].r])
            p.op("dve", lambda e, o=dstk[q], i_=t32[q]: e.tensor_reduce(out=o.ap, in_=i_.ap, axis=AX.X, op=ALU.add), [t32[q].r], [dstk[q].r])
            p.copy("dve", desti.ap[:, ti, k:k + 1], dstk[q].ap, r=[dstk[q].r], w=[desti.r])
        F = ftok[q]
        p.dma("sp", F.ap, fd[ti * 128:(ti + 1) * 128, :], r=[fd_r], w=[F.r], key="fdr")
        for k in range(2):
            p.op("pool", lambda e, F=F, ti=ti, k=k: e.indirect_dma_start(
                out=xs[:, :], out_offset=bass.IndirectOffsetOnAxis(ap=desti.ap[:, ti, k:k + 1], axis=0),
                in_=F.ap[:, :], in_offset=None), [F.r, desti.r], [xs_r], dkey=("pool", id(F.r)))

    p.barrier()
    p.release(m0)
    W1 = [p.sb([128, NCH * 512], BF16, f"W1{i}") for i in range(2)]
    W3 = [p.sb([128, NCH * 512], BF16, f"W3{i}") for i in range(2)]
    W2 = [p.sb([128, 4 * D], BF16, f"W2{i}") for i in range(2)]
    XS = [p.sb([128, D], BF16, f"XS{i}") for i in range(2)]
    xsT = p.sb([128, NCH, 128], BF16, "xsT")
    hh = p.sb([128, 512], BF16, "hh")
    hT = p.sb([128, 4, 128], BF16, "hT")
    sil = p.sb([128, 512], F32, "sil")
    ysb = [p.sb([128, D], F32, f"ysb{i}") for i in range(2)]
    for b in range(NBP):
        q = b % 2
        for Wt, wsrc, key in ((W1[q], w1, "w1"), (W3[q], w3, "w3"), (W2[q], w2, "w2")):
            p.op("pool", lambda e, Wt=Wt, wsrc=wsrc, b=b: e.indirect_dma_start(
                out=Wt.ap[:, :], out_offset=None, in_=wsrc[:, :],
                in_offset=bass.IndirectOffsetOnAxis(ap=idxw.ap[:, b:b + 1], axis=0),
                bounds_check=bnd_reg(e), oob_is_err=False), [idxw.r], [Wt.r], dkey=("pool", id(Wt.r)))
        p.dma("sp", XS[q].ap, xs[b * 128:(b + 1) * 128, :], r=[xs_r], w=[XS[q].r], key="xs")
        for half in range(2):
            tb = p.banks[half]
            tbv = tb.ap.bitcast(BF16)
            for k8 in range(8):
                kc = half * 8 + k8
                p.transpose(tbv[:, k8 * 128:(k8 + 1) * 128], XS[q].ap[:, kc * 128:(kc + 1) * 128], c["ident_bf"].ap,
                            r=[XS[q].r, c["ident_bf"].r], w=[tb.r])
            p.copy("act" if half == 0 else "dve", xsT.ap[:, half * 8:(half + 1) * 8, :],
                   tbv[:, 0:1024].rearrange("p (a b) -> p a b", a=8), r=[tb.r], w=[xsT.r])
        b1 = p.banks[2]
        b3 = p.banks[3]
        for kc in range(NCH):
            p.matmul(b1.ap[:, 0:512], xsT.ap[:, kc, :], W1[q].ap[:, kc * 512:(kc + 1) * 512], kc == 0, kc == NCH - 1,
                     r=[W1[q].r, xsT.r], w=[b1.r])
        for kc in range(NCH):
            p.matmul(b3.ap[:, 0:512], xsT.ap[:, kc, :], W3[q].ap[:, kc * 512:(kc + 1) * 512], kc == 0, kc == NCH - 1,
                     r=[W3[q].r, xsT.r], w=[b3.r])
        p.act(sil.ap, b1.ap[:, 0:512], AF.Silu, r=[b1.r], w=[sil.r])
        p.tt("dve", hh.ap, sil.ap, b3.ap[:, 0:512], ALU.mult, r=[sil.r, b3.r], w=[hh.r])
        tb = p.banks[4]
        tbv = tb.ap.bitcast(BF16)
        for hc in range(4):
            p.transpose(tbv[:, hc * 128:(hc + 1) * 128], hh.ap[:, hc * 128:(hc + 1) * 128], c["ident_bf"].ap,
                        r=[hh.r, c["ident_bf"].r], w=[tb.r])
        p.copy("act", hT.ap, tbv[:, 0:512].rearrange("p (a b) -> p a b", a=4), r=[tb.r], w=[hT.r])
        yb_ = ysb[q]
        for db in range(4):
            bank = p.banks[5 + db % 3]
            for hc in range(4):
                p.matmul(bank.ap[:, 0:512], hT.ap[:, hc, :], W2[q].ap[:, hc * D + db * 512: hc * D + (db + 1) * 512],
                         hc == 0, hc == 3, r=[hT.r, W2[q].r], w=[bank.r])
            p.copy("act" if db % 2 == 0 else "dve", yb_.ap[:, db * 512:(db + 1) * 512], bank.ap[:, 0:512], r=[bank.r], w=[yb_.r])
        p.dma("sp", ys[b * 128:(b + 1) * 128, :], yb_.ap, r=[yb_.r], w=[ys_r], key="ys")

    p.barrier()
    p.release(m0)
    R1 = [p.sb([128, D], F32, f"R1{i}") for i in range(2)]
    R2 = [p.sb([128, D], F32, f"R2{i}") for i in range(2)]
    YO = [p.sb([128, D], F32, f"YO{i}") for i in range(2)]
    outs = []
    for ti in range(NTP):
        q = ti % 2
        for k, Rk in ((0, R1[q]), (1, R2[q])):
            p.op("pool", lambda e, Rk=Rk, ti=ti, k=k: e.indirect_dma_start(
                out=Rk.ap[:, :], out_offset=None, in_=ys[:, :],
                in_offset=bass.IndirectOffsetOnAxis(ap=desti.ap[:, ti, k:k + 1], axis=0)),
                [ys_r, desti.r], [Rk.r], dkey=("pool", id(Rk.r)))
        p.ts("dve", YO[q].ap, R1[q].ap, gall.ap[:, ti, 0:1], ALU.mult, r=[R1[q].r, gall.r], w=[YO[q].r])
        p.stt("dve", YO[q].ap, R2[q].ap, gall.ap[:, ti, 1:2], YO[q].ap, ALU.mult, ALU.add, r=[R2[q].r, gall.r, YO[q].r], w=[YO[q].r])
        outs.append(p.dma("sp", ymoe[ti * 128:(ti + 1) * 128, :], YO[q].ap, r=[YO[q].r], key="out"))
    p.emit(final_waits=[xm_outs[-1], outs[-1]])
    return nc


def post_vec(gprev, mod, layer, inp):
    cols = [pp(gprev[0]), pp(gprev[1])]
    for cls in range(2):
        for k in range(6):
            cols.append(pp(mod[cls, k]))
    cols.append(pp(inp["norm_ffn_g"][layer]))
    v = np.concatenate(cols, axis=1).astype(np.float32)
    assert v.shape == (128, POST_NV)
    return v


def post_weights(inp, layer):
    w1 = np.ascontiguousarray(inp["expert_w1"][layer].reshape(32, NCH, 128, 512).transpose(0, 2, 1, 3)).reshape(32 * 128, NCH * 512)
    w3 = np.ascontiguousarray(inp["expert_w3"][layer].reshape(32, NCH, 128, 512).transpose(0, 2, 1, 3)).reshape(32 * 128, NCH * 512)
    w2 = np.ascontiguousarray(inp["expert_w2"][layer].reshape(32, 4, 128, D).transpose(0, 2, 1, 3)).reshape(32 * 128, 4 * D)
    wr = np.ascontiguousarray(np.concatenate([inp["router_group_w"][layer], inp["router_expert_w"][layer]], 1))
    rc = np.concatenate([inp["router_group_b"][layer], inp["router_expert_b"][layer],
                         np.arange(NBP, dtype=np.float32) * 128.0])[None].astype(np.float32)
    pcol = np.arange(128, dtype=np.float32).reshape(128, 1)
    return {"w1": w1, "w3": w3, "w2": w2, "wr": wr, "rc": rc, "pcol": pcol}


AT_NV = 242
AV_GPREV, AV_MOD, AV_GMIX, AV_SUBG, AV_PAD = 0, 32, 224, 240, 241
HD = 64
DIFF_SCALE = HD ** -0.5


def rope_tables():
    t = np.arange(S)
    row = (t // 64).astype(np.float32)
    col = (t % 64).astype(np.float32)
    nf = HD // 4
    inv = (10000.0 ** (-np.arange(nf, dtype=np.float32) / nf)).astype(np.float32)
    ang = np.concatenate([row[:, None] * inv, col[:, None] * inv], axis=-1).astype(np.float32)
    cos = np.cos(ang).astype(np.float32)
    sin = np.sin(ang).astype(np.float32)
    jj = np.arange(128) % 32
    cosT = np.ascontiguousarray(cos[:, jj].T)
    sinT = np.ascontiguousarray(sin[:, jj].T)
    R = np.zeros((128, 128), np.float32)
    for p_ in range(128):
        j = p_ % 64
        if j < 32:
            R[p_ + 32, p_] = -1.0
        else:
            R[p_ - 32, p_] = 1.0
    return cosT, sinT, R


def build_attn(lambda_init, dbg_heads=8, dbg_blocks=None, dbg_skip=(), LOOK=1):
    nc = new_nc()
    p = Prog(nc)
    xa = nc.dram_tensor("xa", [D, TB], F32, kind="ExternalInput").ap()
    yb = nc.dram_tensor("yb", [D, TB], F32, kind="ExternalInput").ap()
    vec = nc.dram_tensor("vec", [128, AT_NV], F32, kind="ExternalInput").ap()
    lamv = nc.dram_tensor("lamv", [1, 256], F32, kind="ExternalInput").ap()
    wq = nc.dram_tensor("wq", [D, 1024], F32, kind="ExternalInput").ap()
    wk = nc.dram_tensor("wk", [D, 1024], F32, kind="ExternalInput").ap()
    wv = nc.dram_tensor("wv", [D, 1024], F32, kind="ExternalInput").ap()
    cosd = nc.dram_tensor("cosT", [128, S], F32, kind="ExternalInput").ap()
    sind = nc.dram_tensor("sinT", [128, S], F32, kind="ExternalInput").ap()
    Rd = nc.dram_tensor("R", [128, 128], F32, kind="ExternalInput").ap()
    gout = nc.dram_tensor("g", [1024, TB], BF16, kind="ExternalOutput").ap()
    qs = p.dram("qs", [8, 128, TB], BF16)
    ks = p.dram("ks", [8, 128, TB], BF16)
    vs = p.dram("vs", [TB, 1024], BF16)
    qs_r, ks_r, vs_r = Res("qs", multi=True), Res("ks", multi=True), Res("vs", multi=True)
    c = make_consts(p)
    V = p.sb([128, AT_NV], F32, "V")
    p.dma("sp", V.ap, vec, w=[V.r], key="in")
    gs = p.sb([128, 2, NCH], F32, "gs")
    for cls in range(2):
        sc = V.ap[:, AV_MOD + cls * 96 + 16: AV_MOD + cls * 96 + 32]
        p.stt("dve", gs.ap[:, cls, :], sc, 1.0, V.ap[:, AV_GMIX:AV_GMIX + 16], ALU.add, ALU.mult, r=[V.r], w=[gs.r])
    LV = p.sb([128, 256], F32, "LV")
    p.dma("sp", LV.ap, lamv.partition_broadcast(128), w=[LV.r], key="in")
    lt = p.sb([128, 2, 64], F32, "lt")
    le = p.sb([128, 2], F32, "le")
    nlam = p.sb([128, 1], F32, "nlam")
    gsub = p.sb([128, 1], F32, "gsub")
    for i in range(2):
        p.tt("dve", lt.ap[:, i, :], LV.ap[:, 128 * i:128 * i + 64], LV.ap[:, 128 * i + 64:128 * i + 128], ALU.mult, r=[LV.r], w=[lt.r])
        p.op("dve", lambda e, i=i: e.tensor_reduce(out=le.ap[:, i:i + 1], in_=lt.ap[:, i, :], axis=AX.X, op=ALU.add), [lt.r], [le.r])
    p.act(le.ap, le.ap, AF.Exp, r=[le.r], w=[le.r])
    p.tt("dve", nlam.ap, le.ap[:, 1:2], le.ap[:, 0:1], ALU.subtract, r=[le.r], w=[nlam.r])
    p.ts("dve", nlam.ap, nlam.ap, -float(lambda_init), ALU.add, r=[nlam.r], w=[nlam.r])
    p.ts("dve", gsub.ap, V.ap[:, AV_SUBG:AV_SUBG + 1], float(1.0 - lambda_init), ALU.mult, r=[V.r], w=[gsub.r])
    Rb = p.sb([128, 128], BF16, "Rb")
    p.dma("pool", Rb.ap, Rd, w=[Rb.r], key="R")
    m0 = p.mark()

    NB = 256
    Wq = p.sb([128, NCH, 1024], BF16, "Wq")
    Wk = p.sb([128, NCH, 1024], BF16, "Wk")
    Wv = p.sb([128, NCH, 1024], BF16, "Wv")
    load_w(p, Wq, wq, key="wq", nsplit=8)
    load_w(p, Wk, wk, key="wk", nsplit=8)
    load_w(p, Wv, wv, key="wv", nsplit=8)
    X = p.sb([128, NCH, NB], F32, "X")
    Y = p.sb([128, NCH, NB], F32, "Y")
    h = p.sb([128, NCH, NB], BF16, "h")
    sq = p.sb([128, NCH, NB], BF16, "sq")
    rstd = p.sb([128, NB], F32, "rstd")
    tmps = [p.sb([128, NB], F32, f"tmp{i}") for i in range(2)]
    qst = p.sb([128, 8, NB], BF16, "qst")
    kst = p.sb([128, 8, NB], BF16, "kst")
    vst = p.sb([128, NB // 128, 1024], BF16, "vst")
    cosb = p.sb([128, NB], F32, "cosb")
    sinb = p.sb([128, NB], F32, "sinb")
    qb16 = [p.sb([128, NB], BF16, f"qb{i}") for i in range(2)]
    t1 = [p.sb([128, NB], F32, f"t1{i}") for i in range(2)]
    t2 = [p.sb([128, NB], F32, f"t2{i}") for i in range(2)]
    it = 0
    for bi in (range(TB // NB) if dbg_blocks is None else dbg_blocks):
        c0 = bi * NB
        cls = 0 if bi == 0 else 1
        load_fm(p, "sp", X, xa, c0, NB, "xa")
        load_fm(p, "sp", Y, yb, c0, NB, "yb")
        if cls == 1:
            p.dma("sp", cosb.ap, cosd[:, c0 - CTX:c0 - CTX + NB], w=[cosb.r], key="cs")
            p.dma("sp", sinb.ap, sind[:, c0 - CTX:c0 - CTX + NB], w=[sinb.r], key="cs")
        for kc in range(NCH):
            p.stt("dve", X.ap[:, kc, :], Y.ap[:, kc, :], V.ap[:, AV_GPREV + cls * 16 + kc: AV_GPREV + cls * 16 + kc + 1],
                  X.ap[:, kc, :], ALU.mult, ALU.add, r=[X.r, Y.r, V.r], w=[X.r])
        emit_rstd(p, c, X, NB, sq, p.banks[0], rstd)
        sh = (V.ap[:, AV_MOD + cls * 96: AV_MOD + cls * 96 + 16], V.r)
        emit_mod(p, X, rstd, (gs.ap[:, cls, :], gs.r), sh, NB, h, tmps=tmps)
        for W, st in ((Wq, qst), (Wk, kst)):
            for hd in range(8):
                bank = p.banks[1 + it % 3]
                q_ = it % 2
                it += 1
                for kc in range(NCH):
                    p.matmul(bank.ap[:, 0:NB], W.ap[:, kc, hd * 128:(hd + 1) * 128], h.ap[:, kc, :], kc == 0, kc == NCH - 1,
                             r=[W.r, h.r], w=[bank.r])
                if cls == 0:
                    p.copy("act", st.ap[:, hd, :], bank.ap[:, 0:NB], r=[bank.r], w=[st.r])
                else:
                    rbk = p.banks[4 + q_]
                    p.copy("dve", qb16[q_].ap, bank.ap[:, 0:NB], r=[bank.r], w=[qb16[q_].r])
                    p.matmul(rbk.ap[:, 0:NB], Rb.ap, qb16[q_].ap, True, True, r=[Rb.r, qb16[q_].r], w=[rbk.r])
                    p.tt("dve", t1[q_].ap, bank.ap[:, 0:NB], cosb.ap, ALU.mult, r=[bank.r, cosb.r], w=[t1[q_].r])
                    p.tt("dve", t2[q_].ap, rbk.ap[:, 0:NB], sinb.ap, ALU.mult, r=[rbk.r, sinb.r], w=[t2[q_].r])
                    p.tt("pool", st.ap[:, hd, :], t1[q_].ap, t2[q_].ap, ALU.add, r=[t1[q_].r, t2[q_].r], w=[st.r])
        for tt_ in range(NB // 128):
            for nb in range(2):
                bank = p.banks[6 + nb]
                for kc in range(NCH):
                    p.matmul(bank.ap[:, 0:512], h.ap[:, kc, tt_ * 128:(tt_ + 1) * 128], Wv.ap[:, kc, nb * 512:(nb + 1) * 512],
                             kc == 0, kc == NCH - 1, r=[Wv.r, h.r], w=[bank.r])
                p.copy("act" if nb == 0 else "dve", vst.ap[:, tt_, nb * 512:(nb + 1) * 512], bank.ap[:, 0:512], r=[bank.r], w=[vst.r])
        p.dma("sp", qs[:, :, c0:c0 + NB].rearrange("h p t -> p h t"), qst.ap, r=[qst.r], w=[qs_r], key="qs")
        p.dma("sp", ks[:, :, c0:c0 + NB].rearrange("h p t -> p h t"), kst.ap, r=[kst.r], w=[ks_r], key="ks")
        p.dma("sp", vs[c0:c0 + NB, :].rearrange("(t p) v -> p t v", p=128), vst.ap, r=[vst.r], w=[vs_r], key="vs")

    p.barrier()
    p.release(m0)
    NKT = TB // 128
    qT = [p.sb([128, TB], BF16, f"qT{i}") for i in range(2)]
    kT = [p.sb([128, TB], BF16, f"kT{i}") for i in range(2)]
    Vt = [p.sb([128, NKT, 128], BF16, f"Vt{i}") for i in range(2)]
    Pt = [p.sb([128, 512], BF16, f"Pt{i}") for i in range(4)]
    dacc = [p.sb([128, 512], F32, f"dacc{i}") for i in range(2)]
    ones_f = p.sb([128, 128], F32, "ones_f")
    p.memset("pool", ones_f.ap, 1.0, w=[ones_f.r])
    ost = [p.sb([128, TB], BF16, f"ost{i}") for i in range(2)]
    r0 = p.sb([128, 512], F32, "r0")
    o0 = p.sb([128, 512], F32, "o0")
    o1 = p.sb([128, 512], F32, "o1")
    sqo = p.sb([128, 512], BF16, "sqo")
    rs = p.sb([128, 512], F32, "rs")
    qblocks = [(0, CTX, CTX // 128)] + [(CTX + 512 * i, 512, NKT) for i in range(S // 512)]
    outs = []
    pi = 0
    for hd in range(dbg_heads):
        q_ = hd % 2
        p.dma("sp", qT[q_].ap, qs[hd], r=[qs_r], w=[qT[q_].r], key="lq")
        p.dma("sp", kT[q_].ap, ks[hd], r=[ks_r], w=[kT[q_].r], key="lk")
        vsrc = vs[:, hd * 128:(hd + 1) * 128].rearrange("(kt p) v -> p kt v", p=128)
        half = NKT // 2
        grp = p.newgroup()
        p.dma("sp", Vt[q_].ap[:, 0:half, :], vsrc[:, 0:half, :], r=[vs_r], w=[Vt[q_].r], key="lv", group=grp)
        p.dma("sp", Vt[q_].ap[:, half:NKT, :], vsrc[:, half:NKT, :], r=[vs_r], w=[Vt[q_].r], key="lv", group=grp)
        O = ost[q_]
        for (q0, N, nkt) in qblocks:
            Ob = [p.banks[0], p.banks[1]]
            Db = [p.banks[2], p.banks[7]]
            slots = {}
            for i in range(nkt + LOOK):
                if i < nkt:
                    kt = i
                    cur = []
                    for cc in range(2):
                        sbk = p.banks[3 + pi % 4]
                        P_ = Pt[pi % 4]
                        pi += 1
                        cur.append((sbk, P_))
                        p.matmul(sbk.ap[:, 0:N], kT[q_].ap[cc * 64:(cc + 1) * 64, kt * 128:(kt + 1) * 128],
                                 qT[q_].ap[cc * 64:(cc + 1) * 64, q0:q0 + N], True, True, r=[kT[q_].r, qT[q_].r], w=[sbk.r])
                    for cc in range(2):
                        sbk, P_ = cur[cc]
                        p.act(P_.ap[:, 0:N], sbk.ap[:, 0:N], AF.Exp, r=[sbk.r], w=[P_.r], scale=float(DIFF_SCALE))
                    slots[i] = cur
                j = i - LOOK
                if j >= 0:
                    kt = j
                    cur = slots.pop(j)
                    for cc in range(2):
                        P_ = cur[cc][1]
                        p.matmul(Ob[cc].ap[:, 0:N], Vt[q_].ap[:, kt, :], P_.ap[:, 0:N], kt == 0, kt == nkt - 1, r=[Vt[q_].r, P_.r], w=[Ob[cc].r])
                        if kt == 0:
                            p.copy("dve", dacc[cc].ap[:, 0:N], P_.ap[:, 0:N], r=[P_.r], w=[dacc[cc].r])
                        else:
                            p.tt("dve", dacc[cc].ap[:, 0:N], dacc[cc].ap[:, 0:N], P_.ap[:, 0:N], ALU.add, r=[dacc[cc].r, P_.r], w=[dacc[cc].r])
            for cc in range(2):
                p.matmul(Db[cc].ap[:, 0:N], ones_f.ap, dacc[cc].ap[:, 0:N], True, True, r=[ones_f.r, dacc[cc].r], w=[Db[cc].r])
            p.op("dve", lambda e, N=N, Db=Db: e.reciprocal(out=r0.ap[:, 0:N], in_=Db[0].ap[:, 0:N]), [Db[0].r], [r0.r])
            p.tt("dve", o0.ap[:, 0:N], Ob[0].ap[:, 0:N], r0.ap[:, 0:N], ALU.mult, r=[Ob[0].r, r0.r], w=[o0.r])
            p.op("dve", lambda e, N=N, Db=Db: e.reciprocal(out=r0.ap[:, 0:N], in_=Db[1].ap[:, 0:N]), [Db[1].r], [r0.r])
            p.tt("dve", o1.ap[:, 0:N], Ob[1].ap[:, 0:N], r0.ap[:, 0:N], ALU.mult, r=[Ob[1].r, r0.r], w=[o1.r])
            p.stt("dve", o0.ap[:, 0:N], o1.ap[:, 0:N], nlam.ap[:, 0:1], o0.ap[:, 0:N], ALU.mult, ALU.add, r=[o1.r, nlam.r, o0.r], w=[o0.r])
            p.act(sqo.ap[:, 0:N], o0.ap[:, 0:N], AF.Square, r=[o0.r], w=[sqo.r])
            nbk = p.banks[2]
            p.matmul(nbk.ap[:, 0:N], c["ones_bf"].ap, sqo.ap[:, 0:N], True, True, r=[c["ones_bf"].r, sqo.r], w=[nbk.r])
            p.act(rs.ap[:, 0:N], nbk.ap[:, 0:N], AF.Sqrt, r=[nbk.r, c["eps"].r], w=[rs.r], bias=c["eps"].ap, scale=1.0 / 128.0)
            p.op("dve", lambda e, N=N: e.reciprocal(out=rs.ap[:, 0:N], in_=rs.ap[:, 0:N]), [rs.r], [rs.r])
            p.stt("dve", O.ap[:, q0:q0 + N], o0.ap[:, 0:N], gsub.ap[:, 0:1], rs.ap[:, 0:N], ALU.mult, ALU.mult, r=[o0.r, gsub.r, rs.r], w=[O.r])
        outs.append(p.dma("sp", gout[hd * 128:(hd + 1) * 128, :], O.ap, r=[O.r], key="out"))
    p.emit(final_waits=outs[-1:] if outs else [])
    return nc


def attn_vec(gprev, mod, layer, slot, inp):
    cols = [pp(gprev[0]), pp(gprev[1])]
    for cls in range(2):
        for k in range(6):
            cols.append(pp(mod[cls, k]))
    cols.append(pp(inp["norm_mix_g"][layer]))
    cols.append(np.asarray(inp["diff_subln_g"][slot]).reshape(128, 1))
    cols.append(np.zeros((128, 1), np.float32))
    v = np.concatenate(cols, axis=1).astype(np.float32)
    assert v.shape == (128, AT_NV)
    return v


GM_NV = 240 + 16
GV_GPREV, GV_MOD, GV_GMIX, GV_BS = 0, 32, 224, 240


def build_gmlp():
    nc = new_nc()
    p = Prog(nc)
    xa = nc.dram_tensor("xa", [D, TC], F32, kind="ExternalInput").ap()
    yb = nc.dram_tensor("yb", [D, TC], F32, kind="ExternalInput").ap()
    vec = nc.dram_tensor("vec", [128, GM_NV], F32, kind="ExternalInput").ap()
    w_uv = nc.dram_tensor("w_uv", [D, 2 * D], F32, kind="ExternalInput").ap()
    rows = nc.dram_tensor("rows", [3, 2 * D], F32, kind="ExternalInput").ap()
    wsT = nc.dram_tensor("wsT", [16, 128, 128], F32, kind="ExternalInput").ap()
    zout = nc.dram_tensor("g", [D, TC], BF16, kind="ExternalOutput").ap()
    c = make_consts(p)
    V = p.sb([128, GM_NV], F32, "V")
    p.dma("sp", V.ap, vec, w=[V.r], key="in")
    gs = p.sb([128, 2, NCH], F32, "gs")
    for cls in range(2):
        sc = V.ap[:, GV_MOD + cls * 96 + 16: GV_MOD + cls * 96 + 32]
        p.stt("dve", gs.ap[:, cls, :], sc, 1.0, V.ap[:, GV_GMIX:GV_GMIX + 16], ALU.add, ALU.mult, r=[V.r], w=[gs.r])
    buvb = p.sb([1, 2 * D], BF16, "buvb")
    p.dma("pool", buvb.ap, rows[0:1, :], w=[buvb.r], key="rows")
    lng = p.sb([128, D], F32, "lng")
    lnb = p.sb([128, D], F32, "lnb")
    p.dma("sp", lng.ap, rows[1:2, 0:D].partition_broadcast(128), w=[lng.r], key="in")
    p.dma("sp", lnb.ap, rows[2:3, 0:D].partition_broadcast(128), w=[lnb.r], key="in")
    ws = p.sb([128, 16, 128], BF16, "ws")
    p.dma("pool", ws.ap, wsT.rearrange("g q p -> q g p"), w=[ws.r], key="ws")
    Wuv = p.sb([128, NCH, 2 * D], BF16, "Wuv")
    load_w(p, Wuv, w_uv, key="w", nsplit=16)
    X = p.sb([128, NCH, 128], F32, "X")
    Y = p.sb([128, NCH, 128], F32, "Y")
    h = p.sb([128, NCH, 128], BF16, "h")
    sq = p.sb([128, NCH, 128], BF16, "sq")
    rstd = p.sb([128, 128], F32, "rstd")
    tmps = [p.sb([128, 128], F32, f"tmp{i}") for i in range(2)]
    u = p.sb([128, D], BF16, "u")
    v = p.sb([128, D], F32, "v")
    vln = p.sb([128, D], BF16, "vln")
    z = p.sb([128, D], BF16, "z")
    zst = sq
    st1 = p.sb([128, 1], F32, "st1")
    st2 = p.sb([128, 1], F32, "st2")
    outs = []
    for ti in range(NT):
        c0 = ti * 128
        cls = 0 if ti == 0 else 1
        load_fm(p, "sp", X, xa, c0, 128, "xa")
        load_fm(p, "sp", Y, yb, c0, 128, "yb")
        for kc in range(NCH):
            p.stt("dve", X.ap[:, kc, :], Y.ap[:, kc, :], V.ap[:, GV_GPREV + cls * 16 + kc: GV_GPREV + cls * 16 + kc + 1],
                  X.ap[:, kc, :], ALU.mult, ALU.add, r=[X.r, Y.r, V.r], w=[X.r])
        emit_rstd(p, c, X, 128, sq, p.banks[0], rstd)
        sh = (V.ap[:, GV_MOD + cls * 96: GV_MOD + cls * 96 + 16], V.r)
        emit_mod(p, X, rstd, (gs.ap[:, cls, :], gs.r), sh, 128, h, tmps=tmps)
        for cb in range(8):
            bank = p.banks[1 + cb % 3]
            for kc in range(NCH):
                p.matmul(bank.ap[:, 0:512], h.ap[:, kc, :], Wuv.ap[:, kc, cb * 512:(cb + 1) * 512], kc == 0, False,
                         r=[h.r, Wuv.r], w=[bank.r])
            p.matmul(bank.ap[:, 0:512], c["ones_bf"].ap[0:1, :], buvb.ap[0:1, cb * 512:(cb + 1) * 512], False, True,
                     r=[c["ones_bf"].r, buvb.r], w=[bank.r])
            if cb < 4:
                p.act(u.ap[:, cb * 512:(cb + 1) * 512], bank.ap[:, 0:512], AF.Gelu_apprx_tanh, r=[bank.r], w=[u.r])
            else:
                p.act(v.ap[:, (cb - 4) * 512:(cb - 3) * 512], bank.ap[:, 0:512], AF.Gelu_apprx_tanh, r=[bank.r], w=[v.r])
        p.op("dve", lambda e: e.tensor_reduce(out=st1.ap, in_=v.ap, axis=AX.X, op=ALU.add), [v.r], [st1.r])
        p.ts("dve", st1.ap, st1.ap, 1.0 / D, ALU.mult, r=[st1.r], w=[st1.r])
        p.ts("dve", v.ap, v.ap, st1.ap[:, 0:1], ALU.subtract, r=[v.r, st1.r], w=[v.r])
        p.tt("dve", z.ap, v.ap, v.ap, ALU.mult, r=[v.r], w=[z.r])
        p.op("dve", lambda e: e.tensor_reduce(out=st2.ap, in_=z.ap, axis=AX.X, op=ALU.add), [z.r], [st2.r])
        p.act(st2.ap, st2.ap, AF.Sqrt, r=[st2.r, c["eps"].r], w=[st2.r], bias=c["eps"].ap, scale=1.0 / D)
        p.op("dve", lambda e: e.reciprocal(out=st2.ap, in_=st2.ap), [st2.r], [st2.r])
        p.stt("dve", v.ap, v.ap, st2.ap[:, 0:1], lng.ap, ALU.mult, ALU.mult, r=[v.r, st2.r, lng.r], w=[v.r])
        p.tt("dve", vln.ap, v.ap, lnb.ap, ALU.add, r=[v.r, lnb.r], w=[vln.r])
        for g in range(16):
            bank = p.banks[4 + (g // 4) % 2]
            j = g % 4
            p.matmul(bank.ap[:, j * 128:(j + 1) * 128], ws.ap[:, g, :], vln.ap[:, g * 128:(g + 1) * 128], True, True,
                     r=[ws.r, vln.r], w=[bank.r])
            p.stt("dve", z.ap[:, g * 128:(g + 1) * 128], bank.ap[:, j * 128:(j + 1) * 128], V.ap[:, GV_BS + g: GV_BS + g + 1],
                  u.ap[:, g * 128:(g + 1) * 128], ALU.add, ALU.mult, r=[bank.r, V.r, u.r], w=[z.r])
        for half in range(2):
            tb = p.banks[6 + half]
            tbv = tb.ap.bitcast(BF16)
            for k8 in range(8):
                kc = half * 8 + k8
                p.transpose(tbv[:, k8 * 128:(k8 + 1) * 128], z.ap[:, kc * 128:(kc + 1) * 128], c["ident_bf"].ap,
                            r=[z.r, c["ident_bf"].r], w=[tb.r])
            p.copy("act", zst.ap[:, half * 8:(half + 1) * 8, :], tbv[:, 0:1024].rearrange("p (a b) -> p a b", a=8), r=[tb.r], w=[zst.r])
        outs.append(store_fm(p, "sp", zout, c0, 128, zst, "out"))
    p.emit(final_waits=outs[-1:])
    return nc


def gmlp_vec(gprev, mod, layer, slot, inp):
    cols = [pp(gprev[0]), pp(gprev[1])]
    for cls in range(2):
        for k in range(6):
            cols.append(pp(mod[cls, k]))
    cols.append(pp(inp["norm_mix_g"][layer]))
    cols.append(np.ascontiguousarray(np.asarray(inp["gmlp_b_s"][slot]).T))
    v = np.concatenate(cols, axis=1).astype(np.float32)
    assert v.shape == (128, GM_NV)
    return v


def gmlp_rows(slot, inp):
    r = np.zeros((3, 2 * D), np.float32)
    r[0] = inp["gmlp_b_uv"][slot]
    r[1, :D] = inp["gmlp_ln_g"][slot]
    r[2, :D] = inp["gmlp_ln_b"][slot]
    return r


TF = S // 2


def build_final():
    nc = new_nc()
    p = Prog(nc)
    xa = nc.dram_tensor("xa", [D, TF], F32, kind="ExternalInput").ap()
    yb = nc.dram_tensor("yb", [D, TF], F32, kind="ExternalInput").ap()
    vec = nc.dram_tensor("vec", [128, 48], F32, kind="ExternalInput").ap()
    out = nc.dram_tensor("out", [D, TF], F32, kind="ExternalOutput").ap()
    c = make_consts(p)
    V = p.sb([128, 48], F32, "V")
    p.dma("sp", V.ap, vec, w=[V.r], key="in")
    NB = 256
    Xb = [p.sb([128, NCH, NB], F32, f"X{i}") for i in range(2)]
    Yb = [p.sb([128, NCH, NB], F32, f"Y{i}") for i in range(2)]
    sq = p.sb([128, NCH, NB], BF16, "sq")
    rstd = p.sb([128, NB], F32, "rstd")
    outs = []
    for bi in range(TF // NB):
        c0 = bi * NB
        X = Xb[bi % 2]
        Y = Yb[bi % 2]
        load_fm(p, "sp", X, xa, c0, NB, "xa")
        load_fm(p, "sp", Y, yb, c0, NB, "yb")
        for kc in range(NCH):
            p.stt("dve", X.ap[:, kc, :], Y.ap[:, kc, :], V.ap[:, kc:kc + 1], X.ap[:, kc, :], ALU.mult, ALU.add,
                  r=[X.r, Y.r, V.r], w=[X.r])
        emit_rstd(p, c, X, NB, sq, p.banks[bi % 2], rstd)
        emit_mod(p, X, rstd, (V.ap[:, 16:32], V.r), (V.ap[:, 32:48], V.r), NB, None, out_f32=Y)
        outs.append(store_fm(p, "sp", out, c0, NB, Y, "out"))
    p.emit(final_waits=outs[-1:])
    return nc


_PROGS = {}


def _prog(name, fn):
    if name not in _PROGS:
        _PROGS[name] = fn()
    return _PROGS[name]


def _run(nc, maps):
    res = run_bass_kernel_spmd(nc, maps, core_ids=list(range(len(maps))))
    return res.results


def _core_cols(hf):
    return np.r_[hf * 128:(hf + 1) * 128, CTX + hf * (S // 2): CTX + (hf + 1) * (S // 2)]


def kernel(**inputs):
    inp = {k: np.asarray(v) for k, v in inputs.items()}
    f32 = np.float32
    c5 = np.concatenate([inp["c"], inp["c_ctx"][None]], 0).astype(f32)
    cT = np.ascontiguousarray(c5.T.reshape(NCH, 128, 5).transpose(1, 0, 2))
    maps = [{"cT": cT,
             "w": np.ascontiguousarray(inp["ada_w"][:, :, j * ADA_COLS:(j + 1) * ADA_COLS]),
             "b": np.ascontiguousarray(inp["ada_b"][:, None, j * ADA_COLS:(j + 1) * ADA_COLS])} for j in range(8)]
    r = _run(_prog("ada", build_ada), maps)
    mod = np.concatenate([x["mod"] for x in r], axis=2)

    def modb(layer, b):
        return np.stack([mod[layer, 4].reshape(6, D), mod[layer, b].reshape(6, D)], 0)

    xa = [np.ascontiguousarray(np.concatenate([inp["ctx"][b], inp["x"][b]], 0).T.astype(f32)) for b in range(B)]
    yb = [np.zeros((D, TB), f32) for _ in range(B)]
    gprev = [np.zeros((2, D), f32) for _ in range(B)]
    cosT, sinT, Rm = rope_tables()
    for layer in range(DEPTH):
        kind, slot = layer % 3, layer // 3
        maps = []
        for core in range(8):
            b, hf = core // 2, core % 2
            m = modb(layer, b)
            if kind == 0:
                w = inp["lru_w_in"][slot]
                maps.append({"xa": xa[b], "yb": yb[b], "vec": lru_vec(gprev[b], m, layer, slot, hf, inp),
                             "w_in": np.ascontiguousarray(np.concatenate([w[:, hf * 1024:(hf + 1) * 1024],
                                                                          w[:, D + hf * 1024:D + (hf + 1) * 1024]], 1)),
                             "gw": np.ascontiguousarray(inp["lru_gate_w"][slot][:, :, hf * 8:(hf + 1) * 8])})
            elif kind == 1:
                wqkv = inp["diff_w_qkv"][slot]
                sl = slice(hf * 1024, (hf + 1) * 1024)
                maps.append({"xa": xa[b], "yb": yb[b], "vec": attn_vec(gprev[b], m, layer, slot, inp),
                             "lamv": np.ascontiguousarray(inp["diff_lambda"][slot].reshape(1, 256)),
                             "wq": np.ascontiguousarray(wqkv[:, 0:D][:, sl]),
                             "wk": np.ascontiguousarray(wqkv[:, D:2 * D][:, sl]),
                             "wv": np.ascontiguousarray(wqkv[:, 2 * D:3 * D][:, sl]),
                             "cosT": cosT, "sinT": sinT, "R": Rm})
            else:
                cols = _core_cols(hf)
                maps.append({"xa": np.ascontiguousarray(xa[b][:, cols]), "yb": np.ascontiguousarray(yb[b][:, cols]),
                             "vec": gmlp_vec(gprev[b], m, layer, slot, inp), "w_uv": inp["gmlp_w_uv"][slot],
                             "rows": gmlp_rows(slot, inp),
                             "wsT": np.ascontiguousarray(inp["gmlp_w_s"][slot].transpose(0, 2, 1))})
        if kind == 0:
            r = _run(_prog("lru", build_lru), maps)
            G = [np.concatenate([r[2 * b]["g"], r[2 * b + 1]["g"]], 0) for b in range(B)]
            w_out = inp["lru_w_out"][slot]
        elif kind == 1:
            li = 0.8 - 0.6 * math.exp(-0.3 * layer)
            r = _run(_prog("attn", lambda: build_attn(li)), maps)
            G = [np.concatenate([r[2 * b]["g"], r[2 * b + 1]["g"]], 0) for b in range(B)]
            w_out = inp["diff_w_out"][slot]
        else:
            r = _run(_prog("gmlp", build_gmlp), maps)
            G = []
            for b in range(B):
                g = np.empty((D, TB), dtype=r[0]["g"].dtype)
                for hf in range(2):
                    g[:, _core_cols(hf)] = r[2 * b + hf]["g"]
                G.append(g)
            w_out = inp["gmlp_w_out"][slot]
        del maps
        pw = post_weights(inp, layer)
        maps = [{"xa": xa[b], "yb": yb[b], "G": np.ascontiguousarray(G[b]),
                 "vec": post_vec(gprev[b], modb(layer, b), layer, inp), "w_out": w_out, **pw} for b in range(B)]
        r = _run(_prog("post", build_post), maps)
        del maps, pw
        for b in range(B):
            xa[b] = np.ascontiguousarray(r[b]["xmid"])
            yb[b] = np.ascontiguousarray(r[b]["ymoe"].T)
            m = modb(layer, b)
            gprev[b] = np.stack([m[0, 5], m[1, 5]], 0)
    maps = []
    for core in range(8):
        b, hf = core // 2, core % 2
        sl = slice(CTX + hf * TF, CTX + (hf + 1) * TF)
        vec = np.concatenate([pp(gprev[b][1]), pp(inp["norm_final_g"]), np.zeros((128, 16), f32)], 1).astype(f32)
        maps.append({"xa": np.ascontiguousarray(xa[b][:, sl]), "yb": np.ascontiguousarray(yb[b][:, sl]), "vec": vec})
    r = _run(_prog("final", build_final), maps)
    out = np.empty((B, S, D), f32)
    for core in range(8):
        b, hf = core // 2, core % 2
        out[b, hf * TF:(hf + 1) * TF, :] = r[core]["out"].T
    return out
```

```python
import contextlib
import math
import numpy as np
import concourse.bass as bass
import concourse.mybir as mybir
from concourse.bass_utils import run_bass_kernel_spmd

F32 = mybir.dt.float32
BF16 = mybir.dt.bfloat16
I32 = mybir.dt.int32
AF = mybir.ActivationFunctionType
ALU = mybir.AluOpType
AX = mybir.AxisListType

D = 2048
NCH = 16
B = 4
S = 4096
CTX = 256
DEPTH = 4
EPS = 1e-6


class Res:
    __slots__ = ("name", "ws", "rs", "excl", "multi")

    def __init__(self, name="", excl=False, multi=False):
        self.name = name
        self.ws = {}
        self.rs = {}
        self.excl = excl
        self.multi = multi


class Ins:
    __slots__ = ("eng", "fn", "deps", "needs_inc", "semval", "dkey", "idx", "group")

    def __init__(self, eng, fn, dkey=None):
        self.idx = 0
        self.group = None
        self.eng = eng
        self.fn = fn
        self.deps = []
        self.needs_inc = False
        self.semval = None
        self.dkey = dkey


class Buf:
    __slots__ = ("ap", "r")

    def __init__(self, ap, name=""):
        self.ap = ap
        self.r = Res(name)

    def __getitem__(self, k):
        return self.ap[k]


_DSZ = {F32: 4, BF16: 2, I32: 4}
ARENA_BYTES = 204 * 1024


class Prog:
    ENGS = ("pe", "act", "dve", "pool", "sp")

    def __init__(self, nc):
        self.nc = nc
        self.lists = {e: [] for e in self.ENGS}
        self.last = {}
        self.ngroup = 0
        self.stack = contextlib.ExitStack()
        self.arena = self.stack.enter_context(nc.sbuf_tensor("arena", [128, ARENA_BYTES // 4], F32))
        self.off = 0
        self.banks = []
        for i in range(8):
            t = self.stack.enter_context(nc.psum_tensor(f"bank{i}", [128, 512], F32))
            bk = Buf(t[:], f"bank{i}")
            bk.r.excl = True
            self.banks.append(bk)

    def sb(self, shape, dtype, name=""):
        n = 1
        for v in shape[1:]:
            n *= v
        nbytes = (n * _DSZ[dtype] + 63) // 64 * 64
        w = nbytes // 4
        assert self.off + w <= ARENA_BYTES // 4, f"SBUF arena overflow allocating {name} {shape}"
        v = self.arena[0:shape[0], self.off:self.off + w]
        self.off += w
        if dtype != F32:
            v = v.bitcast(dtype)
        v = v[:, 0:n]
        if len(shape) > 2:
            names = " ".join(f"d{i}" for i in range(len(shape) - 1))
            kw = {f"d{i}": shape[i + 1] for i in range(len(shape) - 2)}
            v = v.rearrange(f"p ({names}) -> p {names}", **kw)
        return Buf(v, name)

    def newgroup(self):
        self.ngroup += 1
        return self.ngroup

    def mark(self):
        return self.off

    def release(self, m):
        self.off = m

    def dram(self, name, shape, dtype, kind="Internal"):
        return self.nc.dram_tensor(name, list(shape), dtype, kind=kind).ap()

    def op(self, eng, fn, reads=(), writes=(), dkey=None, group=None):
        ins = Ins(eng, fn, dkey)
        ins.idx = len(self.lists[eng])
        ins.group = group
        deps = {}

        def add(d):
            if d is None or d is ins:
                return
            if d.eng == "pe" and eng == "pe" and d.dkey is None and dkey is None:
                return
            if group is not None and d.group == group:
                return
            key = (d.eng, d.dkey)
            o = deps.get(key)
            if o is None or o.idx < d.idx:
                deps[key] = d

        me = (eng, dkey)
        for r in reads:
            for d in r.ws.values():
                add(d)
            if r.excl:
                for k_, d in r.rs.items():
                    if k_ != me:
                        add(d)
        for w in writes:
            for d in w.rs.values():
                add(d)
            if not w.multi:
                for d in w.ws.values():
                    add(d)
        ins.deps = list(deps.values())
        for d in ins.deps:
            d.needs_inc = True
        for r in reads:
            r.rs[me] = ins
        for w in writes:
            if w.multi:
                w.ws[me] = ins
            else:
                w.ws = {me: ins}
            w.rs = {}
        self.lists[eng].append(ins)
        self.last[me] = ins
        return ins

    def barrier(self):
        lasts = list(self.last.values())
        for e in self.ENGS:
            ins = Ins(e, lambda eng: eng.nop(), None)
            ins.idx = len(self.lists[e])
            ins.deps = [l for l in lasts if not (l.eng == e and l.dkey is None)]
            for d in ins.deps:
                d.needs_inc = True
            self.lists[e].append(ins)
            self.last[(e, None)] = ins

    def dma(self, q, out, in_, r=(), w=(), key=None, group=None, **kw):
        side = None
        for x in list(w) + list(r):
            if not x.multi:
                side = x
                break
        dk = (q, id(side) if side is not None else key)
        return self.op(q, lambda e: e.dma_start(out=out, in_=in_, **kw), r, w, dkey=dk, group=group)

    def matmul(self, out, lhsT, rhs, start, stop, r=(), w=()):
        return self.op("pe", lambda e: e.matmul(out, lhsT, rhs, start=start, stop=stop), r, w)

    def transpose(self, out, in_, ident, r=(), w=()):
        return self.op("pe", lambda e: e.transpose(out, in_, ident), r, w)

    def act(self, out, in_, func, r=(), w=(), bias=None, scale=None, eng="act"):
        kw = {}
        if bias is not None:
            kw["bias"] = bias
        if scale is not None:
            kw["scale"] = scale
        return self.op(eng, lambda e: e.activation(out=out, in_=in_, func=func, **kw), r, w)

    def tt(self, eng, out, in0, in1, op, r=(), w=()):
        return self.op(eng, lambda e: e.tensor_tensor(out=out, in0=in0, in1=in1, op=op), r, w)

    def ts(self, eng, out, in0, s1, op0, s2=None, op1=None, r=(), w=()):
        if op1 is None:
            return self.op(eng, lambda e: e.tensor_scalar(out=out, in0=in0, scalar1=s1, scalar2=None, op0=op0), r, w)
        return self.op(eng, lambda e: e.tensor_scalar(out=out, in0=in0, scalar1=s1, scalar2=s2, op0=op0, op1=op1), r, w)

    def stt(self, eng, out, in0, scalar, in1, op0, op1, r=(), w=()):
        return self.op(eng, lambda e: e.scalar_tensor_tensor(out=out, in0=in0, scalar=scalar, in1=in1, op0=op0, op1=op1), r, w)

    def copy(self, eng, out, in_, r=(), w=()):
        if eng == "act":
            return self.op(eng, lambda e: e.copy(out=out, in_=in_), r, w)
        return self.op(eng, lambda e: e.tensor_copy(out=out, in_=in_), r, w)

    def memset(self, eng, out, val, w=()):
        return self.op(eng, lambda e: e.memset(out, val), (), w)

    def emit(self, final_waits=()):
        nc = self.nc
        final_waits = [ins for k, ins in self.last.items() if k[1] is not None]
        for ins in final_waits:
            ins.needs_inc = True
        esem = {}
        dsem = {}
        for e in self.ENGS:
            cnt = 0
            dcnt = {}
            for ins in self.lists[e]:
                if ins.dkey is not None:
                    dcnt[ins.dkey] = dcnt.get(ins.dkey, 0) + 16
                    ins.semval = dcnt[ins.dkey]
                    dsem.setdefault(ins.dkey, None)
                elif ins.needs_inc:
                    cnt += 1
                    ins.semval = cnt
            esem[e] = None
        for e in self.ENGS:
            esem[e] = self.stack.enter_context(nc.semaphore(f"s_{e}"))
        for i, k in enumerate(dsem):
            dsem[k] = self.stack.enter_context(nc.semaphore(f"d_{i}"))

        def sem_of(ins):
            return dsem[ins.dkey] if ins.dkey is not None else esem[ins.eng]

        def run(e, eng):
            waited = {}
            for ins in self.lists[e]:
                for d in ins.deps:
                    s = sem_of(d)
                    k = id(s)
                    if waited.get(k, 0) < d.semval:
                        eng.wait_ge(s, d.semval)
                        waited[k] = d.semval
                bi = ins.fn(eng)
                if ins.dkey is not None:
                    bi.then_inc(dsem[ins.dkey], 16)
                elif ins.needs_inc:
                    bi.then_inc(esem[e], 1)
            if e == "sp":
                for d in final_waits:
                    s = sem_of(d)
                    if waited.get(id(s), 0) < d.semval:
                        eng.wait_ge(s, d.semval)
                        waited[id(s)] = d.semval

        with nc.Block() as block:
            @block.tensor
            def _(eng):
                run("pe", eng)

            @block.scalar
            def _(eng):
                run("act", eng)

            @block.vector
            def _(eng):
                run("dve", eng)

            @block.gpsimd
            def _(eng):
                run("pool", eng)

            @block.sync
            def _(eng):
                run("sp", eng)
        self.stack.close()


def new_nc():
    return bass.Bass("TRN2", target_bir_lowering=False)


def make_consts(p):
    c = {}
    c["ones_bf"] = p.sb([128, 128], BF16, "ones_bf")
    p.memset("pool", c["ones_bf"].ap, 1.0, w=[c["ones_bf"].r])
    c["eps"] = p.sb([128, 1], F32, "eps")
    p.memset("pool", c["eps"].ap, EPS, w=[c["eps"].r])
    idf = p.sb([128, 128], F32, "ident_f")
    p.memset("pool", idf.ap, 0.0, w=[idf.r])
    p.op("pool", lambda e: e.affine_select(out=idf.ap, in_=idf.ap, pattern=[[-1, 128]], compare_op=ALU.not_equal,
                                           fill=1.0, base=0, channel_multiplier=1), [idf.r], [idf.r])
    c["ident_f"] = idf
    c["ident_bf"] = p.sb([128, 128], BF16, "ident_bf")
    p.copy("pool", c["ident_bf"].ap, idf.ap, r=[idf.r], w=[c["ident_bf"].r])
    return c


def emit_rstd(p, c, x, N, sq, bank, rstd):
    p.act(sq.ap[:, :, 0:N], x.ap[:, :, 0:N], AF.Square, r=[x.r], w=[sq.r])
    for kc in range(NCH):
        p.matmul(bank.ap[:, 0:N], c["ones_bf"].ap, sq.ap[:, kc, 0:N], kc == 0, kc == NCH - 1,
                 r=[c["ones_bf"].r, sq.r], w=[bank.r])
    p.act(rstd.ap[:, 0:N], bank.ap[:, 0:N], AF.Sqrt, r=[bank.r, c["eps"].r], w=[rstd.r], bias=c["eps"].ap, scale=1.0 / D)
    p.op("dve", lambda e: e.reciprocal(out=rstd.ap[:, 0:N], in_=rstd.ap[:, 0:N]), [rstd.r], [rstd.r])


def emit_mod(p, x, rstd, gs, sh, N, out_bf, tmps=None, out_f32=None):
    gs_ap, gs_r = gs
    sh_ap, sh_r = sh
    for cch in range(NCH):
        if out_f32 is not None:
            dst = out_f32.ap[:, cch, 0:N]
            dr = out_f32.r
        else:
            t = tmps[cch % len(tmps)]
            dst = t.ap[:, 0:N]
            dr = t.r
        p.stt("dve", dst, x.ap[:, cch, 0:N], gs_ap[:, cch:cch + 1], rstd.ap[:, 0:N], ALU.mult, ALU.mult,
              r=[x.r, gs_r, rstd.r], w=[dr])
        if out_f32 is not None:
            p.act(dst, dst, AF.Identity, r=[dr, sh_r], w=[dr], bias=sh_ap[:, cch:cch + 1])
        else:
            p.act(out_bf.ap[:, cch, 0:N], dst, AF.Identity, r=[dr, sh_r], w=[out_bf.r], bias=sh_ap[:, cch:cch + 1])
    if out_f32 is not None and out_bf is not None:
        p.copy("pool", out_bf.ap[:, :, 0:N], out_f32.ap[:, :, 0:N], r=[out_f32.r], w=[out_bf.r])


def load_w(p, dst, w2d, key, nsplit=4, q="pool"):
    K = w2d.shape[0]
    kc = K // 128
    src = w2d.rearrange("(kc p) n -> p kc n", p=128)
    step = max(1, kc // nsplit)
    grp = p.newgroup()
    for k0 in range(0, kc, step):
        p.dma(q, dst.ap[:, k0:k0 + step, :], src[:, k0:k0 + step, :], w=[dst.r], key=key, group=grp)


ADA_COLS = 6 * D // 8


def build_ada():
    nc = new_nc()
    p = Prog(nc)
    cT = nc.dram_tensor("cT", [128, NCH, 5], F32, kind="ExternalInput").ap()
    w = nc.dram_tensor("w", [DEPTH, D, ADA_COLS], F32, kind="ExternalInput").ap()
    bia = nc.dram_tensor("b", [DEPTH, 1, ADA_COLS], F32, kind="ExternalInput").ap()
    out = nc.dram_tensor("mod", [DEPTH, 5, ADA_COLS], F32, kind="ExternalOutput").ap()
    s = p.sb([128, NCH, 5], F32, "s")
    p.dma("sp", s.ap, cT, w=[s.r], key="in")
    p.act(s.ap, s.ap, AF.Silu, r=[s.r], w=[s.r])
    wb = [p.sb([128, NCH, 512], F32, f"wb{i}") for i in range(2)]
    bb = [p.sb([5, 512], F32, f"bb{i}") for i in range(2)]
    ob = [p.sb([5, 512], F32, f"ob{i}") for i in range(2)]
    outs = []
    it = 0
    for l in range(DEPTH):
        for nb in range(ADA_COLS // 512):
            W = wb[it % 2]
            bt = bb[it % 2]
            o = ob[it % 2]
            bank = p.banks[it % 2]
            src = w[l, :, nb * 512:(nb + 1) * 512].rearrange("(kc p) n -> p kc n", p=128)
            grp = p.newgroup()
            for k0 in range(0, NCH, 4):
                p.dma("sp", W.ap[:, k0:k0 + 4, :], src[:, k0:k0 + 4, :], w=[W.r], key="w", group=grp)
            p.dma("sp", bt.ap, bia[l, :, nb * 512:(nb + 1) * 512].partition_broadcast(5), w=[bt.r], key="b")
            for kc in range(NCH):
                p.matmul(bank.ap[0:5, :], s.ap[:, kc, :], W.ap[:, kc, :], kc == 0, kc == NCH - 1, r=[s.r, W.r], w=[bank.r])
            p.tt("dve", o.ap, bank.ap[0:5, :], bt.ap, ALU.add, r=[bank.r, bt.r], w=[o.r])
            outs.append(p.dma("sp", out[l, :, nb * 512:(nb + 1) * 512], o.ap, r=[o.r], key="out"))
            it += 1
    p.emit(final_waits=outs[-1:])
    return nc


def pp(v):
    v = np.asarray(v)
    return np.ascontiguousarray(v.reshape(-1, 128).T)


def fm_src(x2d, c0, n):
    return x2d[:, c0:c0 + n].rearrange("(kc p) n -> p kc n", p=128)


def load_fm(p, q, dst, x2d, c0, n, key, nsplit=2):
    src = fm_src(x2d, c0, n)
    step = NCH // nsplit
    grp = p.newgroup()
    for k0 in range(0, NCH, step):
        p.dma(q, dst.ap[:, k0:k0 + step, 0:n], src[:, k0:k0 + step, :], w=[dst.r], key=key, group=grp)


def store_fm(p, q, x2d, c0, n, src, key, nsplit=2):
    dst = fm_src(x2d, c0, n)
    step = NCH // nsplit
    out = None
    grp = p.newgroup()
    for k0 in range(0, NCH, step):
        out = p.dma(q, dst[:, k0:k0 + step, :], src.ap[:, k0:k0 + step, 0:n], r=[src.r], key=key, group=grp)
    return out


TB = CTX + S
LRU_NV = 328
LV_GPREV, LV_MOD, LV_GMIX, LV_CW, LV_CB, LV_GB, LV_LAM = 0, 32, 224, 240, 272, 280, 312


def build_lru():
    nc = new_nc()
    p = Prog(nc)
    xa = nc.dram_tensor("xa", [D, TB], F32, kind="ExternalInput").ap()
    yb = nc.dram_tensor("yb", [D, TB], F32, kind="ExternalInput").ap()
    vec = nc.dram_tensor("vec", [128, LRU_NV], F32, kind="ExternalInput").ap()
    w_in = nc.dram_tensor("w_in", [D, 2048], F32, kind="ExternalInput").ap()
    gw = nc.dram_tensor("gw", [2, 2, 8, 128, 128], F32, kind="ExternalInput").ap()
    gout = nc.dram_tensor("g", [1024, TB], BF16, kind="ExternalOutput").ap()
    scr = p.dram("scr", [16, 128, TB], F32)
    scr_r = Res("scr", multi=True)
    c = make_consts(p)
    V = p.sb([128, LRU_NV], F32, "V")
    p.dma("sp", V.ap, vec, w=[V.r], key="in")
    one = p.sb([128, 1], F32, "one")
    p.memset("pool", one.ap, 1.0, w=[one.r])
    gs = p.sb([128, 2, NCH], F32, "gs")
    for cls in range(2):
        sc = V.ap[:, LV_MOD + cls * 96 + 16: LV_MOD + cls * 96 + 32]
        p.stt("dve", gs.ap[:, cls, :], sc, 1.0, V.ap[:, LV_GMIX:LV_GMIX + 16], ALU.add, ALU.mult, r=[V.r], w=[gs.r])
    ca = p.sb([128, 16], F32, "ca")
    c2 = p.sb([128, 16], F32, "c2")
    p.act(ca.ap, V.ap[:, LV_LAM:LV_LAM + 16], AF.Exp, r=[V.r], w=[ca.r], scale=-1.0)
    p.act(ca.ap, ca.ap, AF.Ln, r=[ca.r, one.r], w=[ca.r], bias=one.ap)
    p.ts("dve", c2.ap, ca.ap, -16.0, ALU.mult, r=[ca.r], w=[c2.r])
    p.ts("dve", ca.ap, ca.ap, -8.0, ALU.mult, r=[ca.r], w=[ca.r])
    gw_sb = p.sb([128, 32, 128], BF16, "gw")
    p.dma("pool", gw_sb.ap, gw.rearrange("d r h i o -> i (d r h) o"), w=[gw_sb.r], key="gw")
    m0 = p.mark()

    NB = 256
    w_sb = p.sb([128, NCH, 2048], BF16, "w_in")
    load_w(p, w_sb, w_in, key="w", nsplit=8)
    xab = [p.sb([128, NCH, NB], F32, f"xa{i}") for i in range(2)]
    ybb = [p.sb([128, NCH, NB], F32, f"yb{i}") for i in range(2)]
    h = p.sb([128, NCH, NB], BF16, "h")
    sq = p.sb([128, NCH, NB], BF16, "sq")
    rstd = p.sb([128, NB], F32, "rstd")
    tmps = [p.sb([128, NB], F32, f"tmp{i}") for i in range(2)]
    stage = p.sb([128, NCH, NB], F32, "stage")
    nblk = TB // NB

    def issue_loads(bj):
        load_fm(p, "sp", xab[bj % 2], xa, bj * NB, NB, "xa")
        load_fm(p, "sp", ybb[bj % 2], yb, bj * NB, NB, "yb")

    issue_loads(0)
    for bi in range(nblk):
        c0 = bi * NB
        cls = 0 if bi == 0 else 1
        X = xab[bi % 2]
        Y = ybb[bi % 2]
        if bi + 1 < nblk:
            issue_loads(bi + 1)
        for kc in range(NCH):
            p.stt("dve", X.ap[:, kc, :], Y.ap[:, kc, :], V.ap[:, LV_GPREV + cls * 16 + kc: LV_GPREV + cls * 16 + kc + 1],
                  X.ap[:, kc, :], ALU.mult, ALU.add, r=[X.r, Y.r, V.r], w=[X.r])
        emit_rstd(p, c, X, NB, sq, p.banks[0], rstd)
        sh = (V.ap[:, LV_MOD + cls * 96: LV_MOD + cls * 96 + 16], V.r)
        emit_mod(p, X, rstd, (gs.ap[:, cls, :], gs.r), sh, NB, h, tmps=tmps)
        for oc in range(16):
            bank = p.banks[1 + oc % 4]
            for kc in range(NCH):
                p.matmul(bank.ap[:, 0:NB], w_sb.ap[:, kc, oc * 128:(oc + 1) * 128], h.ap[:, kc, :], kc == 0, kc == NCH - 1,
                         r=[w_sb.r, h.r], w=[bank.r])
            p.copy("act" if oc % 2 == 0 else "dve", stage.ap[:, oc, :], bank.ap[:, 0:NB], r=[bank.r], w=[stage.r])
        dst = scr[:, :, c0:c0 + NB].rearrange("oc p t -> p oc t")
        grp = p.newgroup()
        for k0 in range(0, 16, 8):
            p.dma("sp", dst[:, k0:k0 + 8, :], stage.ap[:, k0:k0 + 8, :], r=[stage.r], w=[scr_r], key="scr", group=grp)

    p.barrier()
    p.release(m0)
    rec = p.sb([128, TB], F32, "rec")
    gat = p.sb([128, TB], F32, "gat")
    xc = p.sb([128, TB], F32, "xc")
    xcb = p.sb([128, TB], BF16, "xcb")
    Rb = p.sb([128, TB], F32, "Rb")
    Ib = p.sb([128, TB], F32, "Ib")
    Eb = p.sb([128, TB], F32, "Eb")
    H = [p.sb([128, TB], F32, f"H{i}") for i in range(2)]
    ob = p.sb([128, TB], BF16, "ob")
    segs = [(0, CTX), (CTX, TB)]
    outs = []
    for j in range(8):
        p.dma("sp", rec.ap, scr[j], r=[scr_r], w=[rec.r], key="rec")
        p.dma("sp", gat.ap, scr[8 + j], r=[scr_r], w=[gat.r], key="gat")
        cw = lambda k: V.ap[:, LV_CW + j * 4 + k: LV_CW + j * 4 + k + 1]
        p.ts("dve", xc.ap, rec.ap, cw(2), ALU.mult, V.ap[:, LV_CB + j:LV_CB + j + 1], ALU.add, r=[rec.r, V.r], w=[xc.r])
        for (s0, e0) in segs:
            for k, off in ((0, -2), (1, -1), (3, 1)):
                if off < 0:
                    o_sl = slice(s0 - off, e0)
                    i_sl = slice(s0, e0 + off)
                else:
                    o_sl = slice(s0, e0 - off)
                    i_sl = slice(s0 + off, e0)
                p.stt("dve", xc.ap[:, o_sl], rec.ap[:, i_sl], cw(k), xc.ap[:, o_sl], ALU.mult, ALU.add,
                      r=[rec.r, xc.r, V.r], w=[xc.r])
        p.copy("act", xcb.ap, xc.ap, r=[xc.r], w=[xcb.r])
        for d in range(2):
            for which, dstb in ((0, Rb), (1, Ib)):
                gi = (d * 2 + which) * 8 + j
                bcol = V.ap[:, LV_GB + gi: LV_GB + gi + 1]
                for bi, t0 in enumerate(range(0, TB, 512)):
                    n = min(512, TB - t0)
                    bank = p.banks[bi % 4]
                    p.matmul(bank.ap[:, 0:n], gw_sb.ap[:, gi, :], xcb.ap[:, t0:t0 + n], True, True, r=[gw_sb.r, xcb.r], w=[bank.r])
                    p.act(dstb.ap[:, t0:t0 + n], bank.ap[:, 0:n], AF.Sigmoid, r=[bank.r, V.r], w=[dstb.r], bias=bcol)
            li = d * 8 + j
            p.act(Eb.ap, Rb.ap, AF.Exp, r=[Rb.r, c2.r], w=[Eb.r], scale=c2.ap[:, li:li + 1])
            p.act(Rb.ap, Rb.ap, AF.Exp, r=[Rb.r, ca.r], w=[Rb.r], scale=ca.ap[:, li:li + 1])
            p.act(Eb.ap, Eb.ap, AF.Sqrt, r=[Eb.r, one.r], w=[Eb.r], bias=one.ap, scale=-1.0)
            p.tt("dve", Ib.ap, Ib.ap, Eb.ap, ALU.mult, r=[Ib.r, Eb.r], w=[Ib.r])
            p.tt("dve", Ib.ap, Ib.ap, xc.ap, ALU.mult, r=[Ib.r, xc.r], w=[Ib.r])
            Hd = H[d]
            if d == 0:
                p.op("dve", lambda e, Hd=Hd: e.tensor_tensor_scan(out=Hd.ap, data0=Rb.ap, data1=Ib.ap, initial=0.0,
                                                                   op0=ALU.mult, op1=ALU.add), [Rb.r, Ib.r], [Hd.r])
            else:
                p.op("dve", lambda e, Hd=Hd: e.tensor_tensor_scan(out=Hd.ap[:, 0:CTX][:, ::-1], data0=Rb.ap[:, 0:CTX][:, ::-1],
                                                                   data1=Ib.ap[:, 0:CTX][:, ::-1], initial=0.0,
                                                                   op0=ALU.mult, op1=ALU.add), [Rb.r, Ib.r], [Hd.r])
                p.op("dve", lambda e, Hd=Hd: e.tensor_tensor_scan(out=Hd.ap[:, CTX:TB][:, ::-1], data0=Rb.ap[:, CTX:TB][:, ::-1],
                                                                   data1=Ib.ap[:, CTX:TB][:, ::-1], initial=Hd.ap[:, 0:1],
                                                                   op0=ALU.mult, op1=ALU.add), [Rb.r, Ib.r, Hd.r], [Hd.r])
        p.tt("dve", H[0].ap, H[0].ap, H[1].ap, ALU.add, r=[H[0].r, H[1].r], w=[H[0].r])
        p.act(gat.ap, gat.ap, AF.Gelu_apprx_tanh, r=[gat.r], w=[gat.r])
        p.tt("dve", ob.ap, H[0].ap, gat.ap, ALU.mult, r=[H[0].r, gat.r], w=[ob.r])
        outs.append(p.dma("sp", gout[j * 128:(j + 1) * 128, :], ob.ap, r=[ob.r], key="out"))
    p.emit(final_waits=outs[-1:])
    return nc


def lru_vec(gprev, mod, layer, slot, hf, inp):
    ch = slice(hf * 1024, (hf + 1) * 1024)
    cols = [pp(gprev[0]), pp(gprev[1])]
    for cls in range(2):
        for k in range(6):
            cols.append(pp(mod[cls, k]))
    cols.append(pp(inp["norm_mix_g"][layer]))
    cw = inp["lru_conv_w"][slot][:, ch]
    cols.append(np.ascontiguousarray(cw.reshape(4, 8, 128).transpose(2, 1, 0).reshape(128, 32)))
    cols.append(pp(inp["lru_conv_b"][slot][ch]))
    gb = inp["lru_gate_b"][slot][:, :, ch]
    cols.append(np.ascontiguousarray(gb.reshape(2, 2, 8, 128).transpose(3, 0, 1, 2).reshape(128, 32)))
    lam = inp["lru_lambda"][slot][:, ch]
    cols.append(np.ascontiguousarray(lam.reshape(2, 8, 128).transpose(2, 0, 1).reshape(128, 16)))
    v = np.concatenate(cols, axis=1).astype(np.float32)
    assert v.shape == (128, LRU_NV)
    return v


TC = 128 + S // 2
NT = TC // 128
PCORES = 4
TP = TB
NTP = TP // 128
NBP = (2 * TP + 32 * 127 + 127) // 128
NSP = NBP * 128
PV_GPREV, PV_MOD, PV_GFFN, POST_NV = 0, 32, 224, 240
BIG = 1.0e30
RC_N = 36 + NBP


def build_post():
    nc = new_nc()
    p = Prog(nc)
    xa = nc.dram_tensor("xa", [D, TP], F32, kind="ExternalInput").ap()
    yb = nc.dram_tensor("yb", [D, TP], F32, kind="ExternalInput").ap()
    G = nc.dram_tensor("G", [D, TP], BF16, kind="ExternalInput").ap()
    vec = nc.dram_tensor("vec", [128, POST_NV], F32, kind="ExternalInput").ap()
    w_out = nc.dram_tensor("w_out", [D, D], F32, kind="ExternalInput").ap()
    wr = nc.dram_tensor("wr", [D, 36], F32, kind="ExternalInput").ap()
    rc = nc.dram_tensor("rc", [1, RC_N], F32, kind="ExternalInput").ap()
    pcol = nc.dram_tensor("pcol", [128, 1], F32, kind="ExternalInput").ap()
    w1 = nc.dram_tensor("w1", [32 * 128, NCH * 512], F32, kind="ExternalInput").ap()
    w3 = nc.dram_tensor("w3", [32 * 128, NCH * 512], F32, kind="ExternalInput").ap()
    w2 = nc.dram_tensor("w2", [32 * 128, 4 * D], F32, kind="ExternalInput").ap()
    xmid = nc.dram_tensor("xmid", [D, TP], F32, kind="ExternalOutput").ap()
    ymoe = nc.dram_tensor("ymoe", [TP, D], F32, kind="ExternalOutput").ap()
    fd = p.dram("fd", [TP, D], BF16)
    xs = p.dram("xs", [NSP, D], BF16)
    ys = p.dram("ys", [NSP, D], F32)
    fd_r = Res("fd", multi=True)
    xs_r = Res("xs", multi=True)
    ys_r = Res("ys", multi=True)
    regs = {}

    def bnd_reg(e):
        if "b" not in regs:
            regs["b"] = e.to_reg(32 * 128 - 1)
        return regs["b"]

    c = make_consts(p)
    V = p.sb([128, POST_NV], F32, "V")
    p.dma("sp", V.ap, vec, w=[V.r], key="in")
    RC = p.sb([128, RC_N], F32, "RC")
    p.dma("sp", RC.ap, rc.partition_broadcast(128), w=[RC.r], key="in")
    PC = p.sb([128, 1], F32, "PC")
    p.dma("sp", PC.ap, pcol, w=[PC.r], key="in")
    gs = p.sb([128, 2, NCH], F32, "gs")
    for cls in range(2):
        sc = V.ap[:, PV_MOD + cls * 96 + 64: PV_MOD + cls * 96 + 80]
        p.stt("dve", gs.ap[:, cls, :], sc, 1.0, V.ap[:, PV_GFFN:PV_GFFN + 16], ALU.add, ALU.mult, r=[V.r], w=[gs.r])
    U = p.sb([128, 128], BF16, "U")
    uf = p.sb([128, 128], F32, "uf")
    p.memset("pool", uf.ap, 0.0, w=[uf.r])
    p.op("pool", lambda e: e.affine_select(out=uf.ap, in_=uf.ap, pattern=[[-1, 128]], compare_op=ALU.is_ge,
                                           fill=1.0, base=0, channel_multiplier=1), [uf.r], [uf.r])
    p.copy("pool", U.ap, uf.ap, r=[uf.r], w=[U.r])
    desti = p.sb([128, NTP, 2], I32, "desti")
    gall = p.sb([128, NTP, 2], F32, "gall")
    idxw = p.sb([128, NBP], I32, "idxw")
    m0 = p.mark()

    NB = 256
    cum = p.sb([128, 32], F32, "cum")
    p.memset("pool", cum.ap, 0.0, w=[cum.r])
    posall = p.sb([128, NTP, 32], F32, "posall")
    mkall = p.sb([128, NTP, 2, 32], F32, "mkall")
    wo = p.sb([128, NCH, D], BF16, "wo")
    load_w(p, wo, w_out, key="w", nsplit=8)
    wr_sb = p.sb([128, NCH, 36], F32, "wr")
    p.dma("sp", wr_sb.ap, wr.rearrange("(kc p) n -> p kc n", p=128), w=[wr_sb.r], key="in")
    Xb = [p.sb([128, NCH, NB], F32, f"X{i}") for i in range(2)]
    Y = p.sb([128, NCH, NB], F32, "Y")
    Gb = [p.sb([128, NCH, NB], BF16, f"G{i}") for i in range(2)]
    sq = p.sb([128, NCH, NB], BF16, "sq")
    fbf = p.sb([128, NCH, NB], BF16, "fbf")
    rstd = p.sb([128, NB], F32, "rstd")
    ftok = [p.sb([128, D], BF16, f"ftok{i}") for i in range(2)]
    Mt = [p.sb([128, 32], BF16, f"Mt{i}") for i in range(2)]

    def rt(name, shape, dt=F32):
        return [p.sb(shape, dt, f"{name}{i}") for i in range(2)]
    lg = rt("lg", [128, 36]); m4 = rt("m4", [128, 1]); d4 = rt("d4", [128, 4]); s4 = rt("s4", [128, 1])
    oh4 = rt("oh4", [128, 4]); lem = rt("lem", [128, 32]); lem2 = rt("lem2", [128, 32])
    top1 = rt("top1", [128, 1]); top2 = rt("top2", [128, 1]); d12 = rt("d12", [128, 1])
    blocks = [(CTX * 0, CTX)] + [(CTX + NB * i, NB) for i in range((TP - CTX) // NB)]
    xm_outs = []
    ti = 0
    for bi, (c0, N) in enumerate(blocks):
        cls = 0 if bi == 0 else 1
        X = Xb[bi % 2]
        Gt = Gb[bi % 2]
        load_fm(p, "sp", X, xa, c0, N, "xa")
        load_fm(p, "sp", Y, yb, c0, N, "yb")
        load_fm(p, "sp", Gt, G, c0, N, "G")
        for kc in range(NCH):
            p.stt("dve", X.ap[:, kc, 0:N], Y.ap[:, kc, 0:N], V.ap[:, PV_GPREV + cls * 16 + kc: PV_GPREV + cls * 16 + kc + 1],
                  X.ap[:, kc, 0:N], ALU.mult, ALU.add, r=[X.r, Y.r, V.r], w=[X.r])
        g1c = PV_MOD + cls * 96 + 32
        for oc in range(NCH):
            bank = p.banks[oc % 2]
            for kc in range(NCH):
                p.matmul(bank.ap[:, 0:N], wo.ap[:, kc, oc * 128:(oc + 1) * 128], Gt.ap[:, kc, 0:N], kc == 0, kc == NCH - 1,
                         r=[wo.r, Gt.r], w=[bank.r])
            p.stt("dve", X.ap[:, oc, 0:N], bank.ap[:, 0:N], V.ap[:, g1c + oc: g1c + oc + 1], X.ap[:, oc, 0:N], ALU.mult, ALU.add,
                  r=[bank.r, X.r, V.r], w=[X.r])
        xm_outs.append(store_fm(p, "sp", xmid, c0, N, X, "xmid"))
        emit_rstd(p, c, X, N, sq, p.banks[2], rstd)
        sh = (V.ap[:, PV_MOD + cls * 96 + 48: PV_MOD + cls * 96 + 64], V.r)
        emit_mod(p, X, rstd, (gs.ap[:, cls, :], gs.r), sh, N, fbf, out_f32=Y)
        for tt_ in range(N // 128):
            q = ti % 2
            tsl = slice(tt_ * 128, (tt_ + 1) * 128)
            rb = p.banks[3]
            for kc in range(NCH):
                p.matmul(rb.ap[:, 0:36], Y.ap[:, kc, tsl], wr_sb.ap[:, kc, :], kc == 0, kc == NCH - 1, r=[Y.r, wr_sb.r], w=[rb.r])
            L = lg[q]
            mk1 = mkall.ap[:, ti, 0, :]
            mk2 = mkall.ap[:, ti, 1, :]
            p.tt("dve", L.ap, rb.ap[:, 0:36], RC.ap[:, 0:36], ALU.add, r=[rb.r, RC.r], w=[L.r])
            p.op("dve", lambda e, o=m4[q], L=L: e.tensor_reduce(out=o.ap, in_=L.ap[:, 0:4], axis=AX.X, op=ALU.max), [L.r], [m4[q].r])
            p.ts("dve", d4[q].ap, L.ap[:, 0:4], m4[q].ap, ALU.subtract, r=[L.r, m4[q].r], w=[d4[q].r])
            p.act(d4[q].ap, d4[q].ap, AF.Exp, r=[d4[q].r], w=[d4[q].r])
            p.op("dve", lambda e, o=s4[q], i_=d4[q]: e.tensor_reduce(out=o.ap, in_=i_.ap, axis=AX.X, op=ALU.add), [d4[q].r], [s4[q].r])
            p.op("dve", lambda e, o=s4[q]: e.reciprocal(out=o.ap, in_=o.ap), [s4[q].r], [s4[q].r])
            p.ts("dve", oh4[q].ap, L.ap[:, 0:4], m4[q].ap, ALU.is_equal, r=[L.r, m4[q].r], w=[oh4[q].r])
            p.ts("dve", oh4[q].ap, oh4[q].ap, -1.0, ALU.add, BIG, ALU.mult, r=[oh4[q].r], w=[oh4[q].r])
            for g in range(4):
                p.ts("dve", lem[q].ap[:, 8 * g:8 * g + 8], L.ap[:, 4 + 8 * g:12 + 8 * g], oh4[q].ap[:, g:g + 1], ALU.add,
                     r=[L.r, oh4[q].r], w=[lem[q].r])
            p.op("dve", lambda e, o=top1[q], i_=lem[q]: e.tensor_reduce(out=o.ap, in_=i_.ap, axis=AX.X, op=ALU.max), [lem[q].r], [top1[q].r])
            p.ts("dve", mk1, lem[q].ap, top1[q].ap, ALU.is_equal, r=[lem[q].r, top1[q].r], w=[mkall.r])
            p.stt("dve", lem2[q].ap, mk1, -BIG, lem[q].ap, ALU.mult, ALU.add, r=[mkall.r, lem[q].r], w=[lem2[q].r])
            p.op("dve", lambda e, o=top2[q], i_=lem2[q]: e.tensor_reduce(out=o.ap, in_=i_.ap, axis=AX.X, op=ALU.max), [lem2[q].r], [top2[q].r])
            p.ts("dve", mk2, lem2[q].ap, top2[q].ap, ALU.is_equal, r=[lem2[q].r, top2[q].r], w=[mkall.r])
            p.tt("dve", d12[q].ap, top1[q].ap, top2[q].ap, ALU.subtract, r=[top1[q].r, top2[q].r], w=[d12[q].r])
            p.act(d12[q].ap, d12[q].ap, AF.Sigmoid, r=[d12[q].r], w=[d12[q].r])
            p.tt("dve", gall.ap[:, ti, 0:1], d12[q].ap, s4[q].ap, ALU.mult, r=[d12[q].r, s4[q].r], w=[gall.r])
            p.tt("dve", gall.ap[:, ti, 1:2], s4[q].ap, gall.ap[:, ti, 0:1], ALU.subtract, r=[s4[q].r, gall.r], w=[gall.r])
            p.tt("dve", Mt[q].ap, mk1, mk2, ALU.add, r=[mkall.r], w=[Mt[q].r])
            pb = p.banks[4]
            cb = p.banks[5]
            p.matmul(pb.ap[:, 0:32], U.ap, Mt[q].ap, True, True, r=[U.r, Mt[q].r], w=[pb.r])
            p.matmul(cb.ap[:, 0:32], c["ones_bf"].ap, Mt[q].ap, True, True, r=[c["ones_bf"].r, Mt[q].r], w=[cb.r])
            p.tt("dve", posall.ap[:, ti, :], pb.ap[:, 0:32], cum.ap, ALU.add, r=[pb.r, cum.r], w=[posall.r])
            p.tt("dve", cum.ap, cb.ap[:, 0:32], cum.ap, ALU.add, r=[cb.r, cum.r], w=[cum.r])
            F = ftok[q]
            for half in range(2):
                tb = p.banks[6 + half]
                tbv = tb.ap.bitcast(BF16)
                for k8 in range(8):
                    kc = half * 8 + k8
                    p.transpose(tbv[:, k8 * 128:(k8 + 1) * 128], fbf.ap[:, kc, tsl], c["ident_bf"].ap, r=[fbf.r, c["ident_bf"].r], w=[tb.r])
                p.copy("act", F.ap[:, half * 1024:(half + 1) * 1024], tbv[:, 0:1024], r=[tb.r], w=[F.r])
            p.dma("sp", fd[ti * 128:(ti + 1) * 128, :], F.ap, r=[F.r], w=[fd_r], key="fd")
            ti += 1
    assert ti == NTP

    padded = p.sb([128, 32], F32, "padded")
    pend = p.sb([128, 32], F32, "pend")
    pstart = p.sb([128, 32], F32, "pstart")
    onesr = p.sb([128, 32], F32, "onesr")
    be_f = p.sb([128, NBP], F32, "be_f")
    p.memset("pool", onesr.ap, 1.0, w=[onesr.r])
    p.memset("pool", padded.ap, 0.0, w=[padded.r])
    for m_ in range((2 * TP) // 128):
        p.stt("dve", padded.ap, cum.ap, float(128 * m_), padded.ap, ALU.is_gt, ALU.add, r=[cum.r, padded.r], w=[padded.r])
    p.ts("dve", padded.ap, padded.ap, 128.0, ALU.mult, r=[padded.r], w=[padded.r])
    p.op("dve", lambda e: e.tensor_tensor_scan(out=pend.ap, data0=onesr.ap, data1=padded.ap, initial=0.0, op0=ALU.mult, op1=ALU.add),
         [onesr.r, padded.r], [pend.r])
    p.tt("dve", pstart.ap, pend.ap, padded.ap, ALU.subtract, r=[pend.r, padded.r], w=[pstart.r])
    p.memset("pool", be_f.ap, 0.0, w=[be_f.r])
    for e_ in range(32):
        p.stt("dve", be_f.ap, RC.ap[:, 36:36 + NBP], pend.ap[:, e_:e_ + 1], be_f.ap, ALU.is_ge, ALU.add, r=[RC.r, pend.r, be_f.r], w=[be_f.r])
    p.ts("dve", be_f.ap, be_f.ap, 31.0, ALU.min, r=[be_f.r], w=[be_f.r])
    same = p.sb([128, NBP], F32, "same")
    p.memset("pool", same.ap, 0.0, w=[same.r])
    p.tt("dve", same.ap[:, 2:NBP], be_f.ap[:, 2:NBP], be_f.ap[:, 0:NBP - 2], ALU.is_equal, r=[be_f.r, same.r], w=[same.r])
    p.ts("dve", be_f.ap, be_f.ap, 128.0, ALU.mult, PC.ap[:, 0:1], ALU.add, r=[be_f.r, PC.r], w=[be_f.r])
    p.stt("dve", be_f.ap, same.ap, 1.0e6, be_f.ap, ALU.mult, ALU.add, r=[same.r, be_f.r], w=[be_f.r])
    p.copy("dve", idxw.ap, be_f.ap, r=[be_f.r], w=[idxw.r])
    pe_ = rt("pe", [128, 32]); t32 = rt("t32", [128, 32]); dstk = rt("dstk", [128, 1])
    for ti in range(NTP):
        q = ti % 2
        p.tt("dve", pe_[q].ap, posall.ap[:, ti, :], pstart.ap, ALU.add, r=[posall.r, pstart.r], w=[pe_[q].r])
        for k in range(2):
            p.tt("dve", t32[q].ap, mkall.ap[:, ti, k, :], pe_[q].ap, ALU.mult, r=[mkall.r, pe_[q].r], w=[t32[q].r])
            p.op("dve", lambda e, o=dstk[q], i_=t32[q]: e.tensor_reduce(out=o.ap, in_=i_.ap, axis=AX.X, op=ALU.add), [t32[q].r], [dstk[q].r])
            p.copy("dve", desti.ap[:, ti, k:k + 1], dstk[q].ap, r=[dstk[q].r], w=[desti.r])
        F = ftok[q]
        p.dma("sp", F.ap, fd[ti * 128:(ti + 1) * 128, :], r=[fd_r], w=[F.r], key="fdr")
        for k in range(2):
            p.op("pool", lambda e, F=F, ti=ti, k=k: e.indirect_dma_start(
                out=xs[:, :], out_offset=bass.IndirectOffsetOnAxis(ap=desti.ap[:, ti, k:k + 1], axis=0),
                in_=F.ap[:, :], in_offset=None), [F.r, desti.r], [xs_r], dkey=("pool", id(F.r)))

    p.barrier()
    p.release(m0)
    W1 = [p.sb([128, NCH * 512], BF16, f"W1{i}") for i in range(2)]
    W3 = [p.sb([128, NCH * 512], BF16, f"W3{i}") for i in range(2)]
    W2 = [p.sb([128, 4 * D], BF16, f"W2{i}") for i in range(2)]
    XS = [p.sb([128, D], BF16, f"XS{i}") for i in range(2)]
    xsT = p.sb([128, NCH, 128], BF16, "xsT")
    hh = p.sb([128, 512], BF16, "hh")
    hT = p.sb([128, 4, 128], BF16, "hT")
    sil = p.sb([128, 512], F32, "sil")
    ysb = [p.sb([128, D], F32, f"ysb{i}") for i in range(2)]
    for b in range(NBP):
        q = b % 2
        for Wt, wsrc, key in ((W1[q], w1, "w1"), (W3[q], w3, "w3"), (W2[q], w2, "w2")):
            p.op("pool", lambda e, Wt=Wt, wsrc=wsrc, b=b: e.indirect_dma_start(
                out=Wt.ap[:, :], out_offset=None, in_=wsrc[:, :],
                in_offset=bass.IndirectOffsetOnAxis(ap=idxw.ap[:, b:b + 1], axis=0),
                bounds_check=bnd_reg(e), oob_is_err=False), [idxw.r], [Wt.r], dkey=("pool", id(Wt.r)))
        p.dma("sp", XS[q].ap, xs[b * 128:(b + 1) * 128, :], r=[xs_r], w=[XS[q].r], key="xs")
        for half in range(2):
            tb = p.banks[half]
            tbv = tb.ap.bitcast(BF16)
            for k8 in range(8):
                kc = half * 8 + k8
                p.transpose(tbv[:, k8 * 128:(k8 + 1) * 128], XS[q].ap[:, kc * 128:(kc + 1) * 128], c["ident_bf"].ap,
                            r=[XS[q].r, c["ident_bf"].r], w=[tb.r])
            p.copy("act" if half == 0 else "dve", xsT.ap[:, half * 8:(half + 1) * 8, :],
                   tbv[:, 0:1024].rearrange("p (a b) -> p a b", a=8), r=[tb.r], w=[xsT.r])
        b1 = p.banks[2]
        b3 = p.banks[3]
        for kc in range(NCH):
            p.matmul(b1.ap[:, 0:512], xsT.ap[:, kc, :], W1[q].ap[:, kc * 512:(kc + 1) * 512], kc == 0, kc == NCH - 1,
                     r=[W1[q].r, xsT.r], w=[b1.r])
        for kc in range(NCH):
            p.matmul(b3.ap[:, 0:512], xsT.ap[:, kc, :], W3[q].ap[:, kc * 512:(kc + 1) * 512], kc == 0, kc == NCH - 1,
                     r=[W3[q].r, xsT.r], w=[b3.r])
        p.act(sil.ap, b1.ap[:, 0:512], AF.Silu, r=[b1.r], w=[sil.r])
        p.tt("dve", hh.ap, sil.ap, b3.ap[:, 0:512], ALU.mult, r=[sil.r, b3.r], w=[hh.r])
        tb = p.banks[4]
        tbv = tb.ap.bitcast(BF16)
        for hc in range(4):
            p.transpose(tbv[:, hc * 128:(hc + 1) * 128], hh.ap[:, hc * 128:(hc + 1) * 128], c["ident_bf"].ap,
                        r=[hh.r, c["ident_bf"].r], w=[tb.r])
        p.copy("act", hT.ap, tbv[:, 0:512].rearrange("p (a b) -> p a b", a=4), r=[tb.r], w=[hT.r])
        yb_ = ysb[q]
        for db in range(4):
            bank = p.banks[5 + db % 3]
            for hc in range(4):
                p.matmul(bank.ap[:, 0:512], hT.ap[:, hc, :], W2[q].ap[:, hc * D + db * 512: hc * D + (db + 1) * 512],
                         hc == 0, hc == 3, r=[hT.r, W2[q].r], w=[bank.r])
            p.copy("act" if db % 2 == 0 else "dve", yb_.ap[:, db * 512:(db + 1) * 512], bank.ap[:, 0:512], r=[bank.r], w=[yb_.r])
        p.dma("sp", ys[b * 128:(b + 1) * 128, :], yb_.ap, r=[yb_.r], w=[ys_r], key="ys")

    p.barrier()
    p.release(m0)
    R1 = [p.sb([128, D], F32, f"R1{i}") for i in range(2)]
    R2 = [p.sb([128, D], F32, f"R2{i}") for i in range(2)]
    YO = [p.sb([128, D], F32, f"YO{i}") for i in range(2)]
    outs = []
    for ti in range(NTP):
        q = ti % 2
        for k, Rk in ((0, R1[q]), (1, R2[q])):
            p.op("pool", lambda e, Rk=Rk, ti=ti, k=k: e.indirect_dma_start(
                out=Rk.ap[:, :], out_offset=None, in_=ys[:, :],
                in_offset=bass.IndirectOffsetOnAxis(ap=desti.ap[:, ti, k:k + 1], axis=0)),
                [ys_r, desti.r], [Rk.r], dkey=("pool", id(Rk.r)))
        p.ts("dve", YO[q].ap, R1[q].ap, gall.ap[:, ti, 0:1], ALU.mult, r=[R1[q].r, gall.r], w=[YO[q].r])
        p.stt("dve", YO[q].ap, R2[q].ap, gall.ap[:, ti, 1:2], YO[q].ap, ALU.mult, ALU.add, r=[R2[q].r, gall.r, YO[q].r], w=[YO[q].r])
        outs.append(p.dma("sp", ymoe[ti * 128:(ti + 1) * 128, :], YO[q].ap, r=[YO[q].r], key="out"))
    p.emit(final_waits=[xm_outs[-1], outs[-1]])
    return nc


def post_vec(gprev, mod, layer, inp):
    cols = [pp(gprev[0]), pp(gprev[1])]
    for cls in range(2):
        for k in range(6):
            cols.append(pp(mod[cls, k]))
    cols.append(pp(inp["norm_ffn_g"][layer]))
    v = np.concatenate(cols, axis=1).astype(np.float32)
    assert v.shape == (128, POST_NV)
    return v


def post_weights(inp, layer):
    w1 = np.ascontiguousarray(inp["expert_w1"][layer].reshape(32, NCH, 128, 512).transpose(0, 2, 1, 3)).reshape(32 * 128, NCH * 512)
    w3 = np.ascontiguousarray(inp["expert_w3"][layer].reshape(32, NCH, 128, 512).transpose(0, 2, 1, 3)).reshape(32 * 128, NCH * 512)
    w2 = np.ascontiguousarray(inp["expert_w2"][layer].reshape(32, 4, 128, D).transpose(0, 2, 1, 3)).reshape(32 * 128, 4 * D)
    wr = np.ascontiguousarray(np.concatenate([inp["router_group_w"][layer], inp["router_expert_w"][layer]], 1))
    rc = np.concatenate([inp["router_group_b"][layer], inp["router_expert_b"][layer],
                         np.arange(NBP, dtype=np.float32) * 128.0])[None].astype(np.float32)
    pcol = np.arange(128, dtype=np.float32).reshape(128, 1)
    return {"w1": w1, "w3": w3, "w2": w2, "wr": wr, "rc": rc, "pcol": pcol}


AT_NV = 242
AV_GPREV, AV_MOD, AV_GMIX, AV_SUBG, AV_PAD = 0, 32, 224, 240, 241
HD = 64
DIFF_SCALE = HD ** -0.5


def rope_tables():
    t = np.arange(S)
    row = (t // 64).astype(np.float32)
    col = (t % 64).astype(np.float32)
    nf = HD // 4
    inv = (10000.0 ** (-np.arange(nf, dtype=np.float32) / nf)).astype(np.float32)
    ang = np.concatenate([row[:, None] * inv, col[:, None] * inv], axis=-1).astype(np.float32)
    cos = np.cos(ang).astype(np.float32)
    sin = np.sin(ang).astype(np.float32)
    jj = np.arange(128) % 32
    cosT = np.ascontiguousarray(cos[:, jj].T)
    sinT = np.ascontiguousarray(sin[:, jj].T)
    R = np.zeros((128, 128), np.float32)
    for p_ in range(128):
        j = p_ % 64
        if j < 32:
            R[p_ + 32, p_] = -1.0
        else:
            R[p_ - 32, p_] = 1.0
    return cosT, sinT, R


def build_attn(lambda_init, dbg_heads=8, dbg_blocks=None, dbg_skip=(), LOOK=1):
    nc = new_nc()
    p = Prog(nc)
    xa = nc.dram_tensor("xa", [D, TB], F32, kind="ExternalInput").ap()
    yb = nc.dram_tensor("yb", [D, TB], F32, kind="ExternalInput").ap()
    vec = nc.dram_tensor("vec", [128, AT_NV], F32, kind="ExternalInput").ap()
    lamv = nc.dram_tensor("lamv", [1, 256], F32, kind="ExternalInput").ap()
    wq = nc.dram_tensor("wq", [D, 1024], F32, kind="ExternalInput").ap()
    wk = nc.dram_tensor("wk", [D, 1024], F32, kind="ExternalInput").ap()
    wv = nc.dram_tensor("wv", [D, 1024], F32, kind="ExternalInput").ap()
    cosd = nc.dram_tensor("cosT", [128, S], F32, kind="ExternalInput").ap()
    sind = nc.dram_tensor("sinT", [128, S], F32, kind="ExternalInput").ap()
    Rd = nc.dram_tensor("R", [128, 128], F32, kind="ExternalInput").ap()
    gout = nc.dram_tensor("g", [1024, TB], BF16, kind="ExternalOutput").ap()
    qs = p.dram("qs", [8, 128, TB], BF16)
    ks = p.dram("ks", [8, 128, TB], BF16)
    vs = p.dram("vs", [TB, 1024], BF16)
    qs_r, ks_r, vs_r = Res("qs", multi=True), Res("ks", multi=True), Res("vs", multi=True)
    c = make_consts(p)
    V = p.sb([128, AT_NV], F32, "V")
    p.dma("sp", V.ap, vec, w=[V.r], key="in")
    gs = p.sb([128, 2, NCH], F32, "gs")
    for cls in range(2):
        sc = V.ap[:, AV_MOD + cls * 96 + 16: AV_MOD + cls * 96 + 32]
        p.stt("dve", gs.ap[:, cls, :], sc, 1.0, V.ap[:, AV_GMIX:AV_GMIX + 16], ALU.add, ALU.mult, r=[V.r], w=[gs.r])
    LV = p.sb([128, 256], F32, "LV")
    p.dma("sp", LV.ap, lamv.partition_broadcast(128), w=[LV.r], key="in")
    lt = p.sb([128, 2, 64], F32, "lt")
    le = p.sb([128, 2], F32, "le")
    nlam = p.sb([128, 1], F32, "nlam")
    gsub = p.sb([128, 1], F32, "gsub")
    for i in range(2):
        p.tt("dve", lt.ap[:, i, :], LV.ap[:, 128 * i:128 * i + 64], LV.ap[:, 128 * i + 64:128 * i + 128], ALU.mult, r=[LV.r], w=[lt.r])
        p.op("dve", lambda e, i=i: e.tensor_reduce(out=le.ap[:, i:i + 1], in_=lt.ap[:, i, :], axis=AX.X, op=ALU.add), [lt.r], [le.r])
    p.act(le.ap, le.ap, AF.Exp, r=[le.r], w=[le.r])
    p.tt("dve", nlam.ap, le.ap[:, 1:2], le.ap[:, 0:1], ALU.subtract, r=[le.r], w=[nlam.r])
    p.ts("dve", nlam.ap, nlam.ap, -float(lambda_init), ALU.add, r=[nlam.r], w=[nlam.r])
    p.ts("dve", gsub.ap, V.ap[:, AV_SUBG:AV_SUBG + 1], float(1.0 - lambda_init), ALU.mult, r=[V.r], w=[gsub.r])
    Rb = p.sb([128, 128], BF16, "Rb")
    p.dma("pool", Rb.ap, Rd, w=[Rb.r], key="R")
    m0 = p.mark()

    NB = 256
    Wq = p.sb([128, NCH, 1024], BF16, "Wq")
    Wk = p.sb([128, NCH, 1024], BF16, "Wk")
    Wv = p.sb([128, NCH, 1024], BF16, "Wv")
    load_w(p, Wq, wq, key="wq", nsplit=8)
    load_w(p, Wk, wk, key="wk", nsplit=8)
    load_w(p, Wv, wv, key="wv", nsplit=8)
    X = p.sb([128, NCH, NB], F32, "X")
    Y = p.sb([128, NCH, NB], F32, "Y")
    h = p.sb([128, NCH, NB], BF16, "h")
    sq = p.sb([128, NCH, NB], BF16, "sq")
    rstd = p.sb([128, NB], F32, "rstd")
    tmps = [p.sb([128, NB], F32, f"tmp{i}") for i in range(2)]
    qst = p.sb([128, 8, NB], BF16, "qst")
    kst = p.sb([128, 8, NB], BF16, "kst")
    vst = p.sb([128, NB // 128, 1024], BF16, "vst")
    cosb = p.sb([128, NB], F32, "cosb")
    sinb = p.sb([128, NB], F32, "sinb")
    qb16 = [p.sb([128, NB], BF16, f"qb{i}") for i in range(2)]
    t1 = [p.sb([128, NB], F32, f"t1{i}") for i in range(2)]
    t2 = [p.sb([128, NB], F32, f"t2{i}") for i in range(2)]
    it = 0
    for bi in (range(TB // NB) if dbg_blocks is None else dbg_blocks):
        c0 = bi * NB
        cls = 0 if bi == 0 else 1
        load_fm(p, "sp", X, xa, c0, NB, "xa")
        load_fm(p, "sp", Y, yb, c0, NB, "yb")
        if cls == 1:
            p.dma("sp", cosb.ap, cosd[:, c0 - CTX:c0 - CTX + NB], w=[cosb.r], key="cs")
            p.dma("sp", sinb.ap, sind[:, c0 - CTX:c0 - CTX + NB], w=[sinb.r], key="cs")
        for kc in range(NCH):
            p.stt("dve", X.ap[:, kc, :], Y.ap[:, kc, :], V.ap[:, AV_GPREV + cls * 16 + kc: AV_GPREV + cls * 16 + kc + 1],
                  X.ap[:, kc, :], ALU.mult, ALU.add, r=[X.r, Y.r, V.r], w=[X.r])
        emit_rstd(p, c, X, NB, sq, p.banks[0], rstd)
        sh = (V.ap[:, AV_MOD + cls * 96: AV_MOD + cls * 96 + 16], V.r)
        emit_mod(p, X, rstd, (gs.ap[:, cls, :], gs.r), sh, NB, h, tmps=tmps)
        for W, st in ((Wq, qst), (Wk, kst)):
            for hd in range(8):
                bank = p.banks[1 + it % 3]
                q_ = it % 2
                it += 1
                for kc in range(NCH):
                    p.matmul(bank.ap[:, 0:NB], W.ap[:, kc, hd * 128:(hd + 1) * 128], h.ap[:, kc, :], kc == 0, kc == NCH - 1,
                             r=[W.r, h.r], w=[bank.r])
                if cls == 0:
                    p.copy("act", st.ap[:, hd, :], bank.ap[:, 0:NB], r=[bank.r], w=[st.r])
                else:
                    rbk = p.banks[4 + q_]
                    p.copy("dve", qb16[q_].ap, bank.ap[:, 0:NB], r=[bank.r], w=[qb16[q_].r])
                    p.matmul(rbk.ap[:, 0:NB], Rb.ap, qb16[q_].ap, True, True, r=[Rb.r, qb16[q_].r], w=[rbk.r])
                    p.tt("dve", t1[q_].ap, bank.ap[:, 0:NB], cosb.ap, ALU.mult, r=[bank.r, cosb.r], w=[t1[q_].r])
                    p.tt("dve", t2[q_].ap, rbk.ap[:, 0:NB], sinb.ap, ALU.mult, r=[rbk.r, sinb.r], w=[t2[q_].r])
                    p.tt("pool", st.ap[:, hd, :], t1[q_].ap, t2[q_].ap, ALU.add, r=[t1[q_].r, t2[q_].r], w=[st.r])
        for tt_ in range(NB // 128):
            for nb in range(2):
                bank = p.banks[6 + nb]
                for kc in range(NCH):
                    p.matmul(bank.ap[:, 0:512], h.ap[:, kc, tt_ * 128:(tt_ + 1) * 128], Wv.ap[:, kc, nb * 512:(nb + 1) * 512],
                             kc == 0, kc == NCH - 1, r=[Wv.r, h.r], w=[bank.r])
                p.copy("act" if nb == 0 else "dve", vst.ap[:, tt_, nb * 512:(nb + 1) * 512], bank.ap[:, 0:512], r=[bank.r], w=[vst.r])
        p.dma("sp", qs[:, :, c0:c0 + NB].rearrange("h p t -> p h t"), qst.ap, r=[qst.r], w=[qs_r], key="qs")
        p.dma("sp", ks[:, :, c0:c0 + NB].rearrange("h p t -> p h t"), kst.ap, r=[kst.r], w=[ks_r], key="ks")
        p.dma("sp", vs[c0:c0 + NB, :].rearrange("(t p) v -> p t v", p=128), vst.ap, r=[vst.r], w=[vs_r], key="vs")

    p.barrier()
    p.release(m0)
    NKT = TB // 128
    qT = [p.sb([128, TB], BF16, f"qT{i}") for i in range(2)]
    kT = [p.sb([128, TB], BF16, f"kT{i}") for i in range(2)]
    Vt = [p.sb([128, NKT, 128], BF16, f"Vt{i}") for i in range(2)]
    Pt = [p.sb([128, 512], BF16, f"Pt{i}") for i in range(4)]
    dacc = [p.sb([128, 512], F32, f"dacc{i}") for i in range(2)]
    ones_f = p.sb([128, 128], F32, "ones_f")
    p.memset("pool", ones_f.ap, 1.0, w=[ones_f.r])
    ost = [p.sb([128, TB], BF16, f"ost{i}") for i in range(2)]
    r0 = p.sb([128, 512], F32, "r0")
    o0 = p.sb([128, 512], F32, "o0")
    o1 = p.sb([128, 512], F32, "o1")
    sqo = p.sb([128, 512], BF16, "sqo")
    rs = p.sb([128, 512], F32, "rs")
    qblocks = [(0, CTX, CTX // 128)] + [(CTX + 512 * i, 512, NKT) for i in range(S // 512)]
    outs = []
    pi = 0
    for hd in range(dbg_heads):
        q_ = hd % 2
        p.dma("sp", qT[q_].ap, qs[hd], r=[qs_r], w=[qT[q_].r], key="lq")
        p.dma("sp", kT[q_].ap, ks[hd], r=[ks_r], w=[kT[q_].r], key="lk")
        vsrc = vs[:, hd * 128:(hd + 1) * 128].rearrange("(kt p) v -> p kt v", p=128)
        half = NKT // 2
        grp = p.newgroup()
        p.dma("sp", Vt[q_].ap[:, 0:half, :], vsrc[:, 0:half, :], r=[vs_r], w=[Vt[q_].r], key="lv", group=grp)
        p.dma("sp", Vt[q_].ap[:, half:NKT, :], vsrc[:, half:NKT, :], r=[vs_r], w=[Vt[q_].r], key="lv", group=grp)
        O = ost[q_]
        for (q0, N, nkt) in qblocks:
            Ob = [p.banks[0], p.banks[1]]
            Db = [p.banks[2], p.banks[7]]
            slots = {}
            for i in range(nkt + LOOK):
                if i < nkt:
                    kt = i
                    cur = []
                    for cc in range(2):
                        sbk = p.banks[3 + pi % 4]
                        P_ = Pt[pi % 4]
                        pi += 1
                        cur.append((sbk, P_))
                        p.matmul(sbk.ap[:, 0:N], kT[q_].ap[cc * 64:(cc + 1) * 64, kt * 128:(kt + 1) * 128],
                                 qT[q_].ap[cc * 64:(cc + 1) * 64, q0:q0 + N], True, True, r=[kT[q_].r, qT[q_].r], w=[sbk.r])
                    for cc in range(2):
                        sbk, P_ = cur[cc]
                        p.act(P_.ap[:, 0:N], sbk.ap[:, 0:N], AF.Exp, r=[sbk.r], w=[P_.r], scale=float(DIFF_SCALE))
                    slots[i] = cur
                j = i - LOOK
                if j >= 0:
                    kt = j
                    cur = slots.pop(j)
                    for cc in range(2):
                        P_ = cur[cc][1]
                        p.matmul(Ob[cc].ap[:, 0:N], Vt[q_].ap[:, kt, :], P_.ap[:, 0:N], kt == 0, kt == nkt - 1, r=[Vt[q_].r, P_.r], w=[Ob[cc].r])
                        if kt == 0:
                            p.copy("dve", dacc[cc].ap[:, 0:N], P_.ap[:, 0:N], r=[P_.r], w=[dacc[cc].r])
                        else:
                            p.tt("dve", dacc[cc].ap[:, 0:N], dacc[cc].ap[:, 0:N], P_.ap[:, 0:N], ALU.add, r=[dacc[cc].r, P_.r], w=[dacc[cc].r])
            for cc in range(2):
                p.matmul(Db[cc].ap[:, 0:N], ones_f.ap, dacc[cc].ap[:, 0:N], True, True, r=[ones_f.r, dacc[cc].r], w=[Db[cc].r])
            p.op("dve", lambda e, N=N, Db=Db: e.reciprocal(out=r0.ap[:, 0:N], in_=Db[0].ap[:, 0:N]), [Db[0].r], [r0.r])
            p.tt("dve", o0.ap[:, 0:N], Ob[0].ap[:, 0:N], r0.ap[:, 0:N], ALU.mult, r=[Ob[0].r, r0.r], w=[o0.r])
            p.op("dve", lambda e, N=N, Db=Db: e.reciprocal(out=r0.ap[:, 0:N], in_=Db[1].ap[:, 0:N]), [Db[1].r], [r0.r])
            p.tt("dve", o1.ap[:, 0:N], Ob[1].ap[:, 0:N], r0.ap[:, 0:N], ALU.mult, r=[Ob[1].r, r0.r], w=[o1.r])
            p.stt("dve", o0.ap[:, 0:N], o1.ap[:, 0:N], nlam.ap[:, 0:1], o0.ap[:, 0:N], ALU.mult, ALU.add, r=[o1.r, nlam.r, o0.r], w=[o0.r])
            p.act(sqo.ap[:, 0:N], o0.ap[:, 0:N], AF.Square, r=[o0.r], w=[sqo.r])
            nbk = p.banks[2]
            p.matmul(nbk.ap[:, 0:N], c["ones_bf"].ap, sqo.ap[:, 0:N], True, True, r=[c["ones_bf"].r, sqo.r], w=[nbk.r])
            p.act(rs.ap[:, 0:N], nbk.ap[:, 0:N], AF.Sqrt, r=[nbk.r, c["eps"].r], w=[rs.r], bias=c["eps"].ap, scale=1.0 / 128.0)
            p.op("dve", lambda e, N=N: e.reciprocal(out=rs.ap[:, 0:N], in_=rs.ap[:, 0:N]), [rs.r], [rs.r])
            p.stt("dve", O.ap[:, q0:q0 + N], o0.ap[:, 0:N], gsub.ap[:, 0:1], rs.ap[:, 0:N], ALU.mult, ALU.mult, r=[o0.r, gsub.r, rs.r], w=[O.r])
        outs.append(p.dma("sp", gout[hd * 128:(hd + 1) * 128, :], O.ap, r=[O.r], key="out"))
    p.emit(final_waits=outs[-1:] if outs else [])
    return nc


def attn_vec(gprev, mod, layer, slot, inp):
    cols = [pp(gprev[0]), pp(gprev[1])]
    for cls in range(2):
        for k in range(6):
            cols.append(pp(mod[cls, k]))
    cols.append(pp(inp["norm_mix_g"][layer]))
    cols.append(np.asarray(inp["diff_subln_g"][slot]).reshape(128, 1))
    cols.append(np.zeros((128, 1), np.float32))
    v = np.concatenate(cols, axis=1).astype(np.float32)
    assert v.shape == (128, AT_NV)
    return v


GM_NV = 240 + 16
GV_GPREV, GV_MOD, GV_GMIX, GV_BS = 0, 32, 224, 240


def build_gmlp():
    nc = new_nc()
    p = Prog(nc)
    xa = nc.dram_tensor("xa", [D, TC], F32, kind="ExternalInput").ap()
    yb = nc.dram_tensor("yb", [D, TC], F32, kind="ExternalInput").ap()
    vec = nc.dram_tensor("vec", [128, GM_NV], F32, kind="ExternalInput").ap()
    w_uv = nc.dram_tensor("w_uv", [D, 2 * D], F32, kind="ExternalInput").ap()
    rows = nc.dram_tensor("rows", [3, 2 * D], F32, kind="ExternalInput").ap()
    wsT = nc.dram_tensor("wsT", [16, 128, 128], F32, kind="ExternalInput").ap()
    zout = nc.dram_tensor("g", [D, TC], BF16, kind="ExternalOutput").ap()
    c = make_consts(p)
    V = p.sb([128, GM_NV], F32, "V")
    p.dma("sp", V.ap, vec, w=[V.r], key="in")
    gs = p.sb([128, 2, NCH], F32, "gs")
    for cls in range(2):
        sc = V.ap[:, GV_MOD + cls * 96 + 16: GV_MOD + cls * 96 + 32]
        p.stt("dve", gs.ap[:, cls, :], sc, 1.0, V.ap[:, GV_GMIX:GV_GMIX + 16], ALU.add, ALU.mult, r=[V.r], w=[gs.r])
    buvb = p.sb([1, 2 * D], BF16, "buvb")
    p.dma("pool", buvb.ap, rows[0:1, :], w=[buvb.r], key="rows")
    lng = p.sb([128, D], F32, "lng")
    lnb = p.sb([128, D], F32, "lnb")
    p.dma("sp", lng.ap, rows[1:2, 0:D].partition_broadcast(128), w=[lng.r], key="in")
    p.dma("sp", lnb.ap, rows[2:3, 0:D].partition_broadcast(128), w=[lnb.r], key="in")
    ws = p.sb([128, 16, 128], BF16, "ws")
    p.dma("pool", ws.ap, wsT.rearrange("g q p -> q g p"), w=[ws.r], key="ws")
    Wuv = p.sb([128, NCH, 2 * D], BF16, "Wuv")
    load_w(p, Wuv, w_uv, key="w", nsplit=16)
    X = p.sb([128, NCH, 128], F32, "X")
    Y = p.sb([128, NCH, 128], F32, "Y")
    h = p.sb([128, NCH, 128], BF16, "h")
    sq = p.sb([128, NCH, 128], BF16, "sq")
    rstd = p.sb([128, 128], F32, "rstd")
    tmps = [p.sb([128, 128], F32, f"tmp{i}") for i in range(2)]
    u = p.sb([128, D], BF16, "u")
    v = p.sb([128, D], F32, "v")
    vln = p.sb([128, D], BF16, "vln")
    z = p.sb([128, D], BF16, "z")
    zst = sq
    st1 = p.sb([128, 1], F32, "st1")
    st2 = p.sb([128, 1], F32, "st2")
    outs = []
    for ti in range(NT):
        c0 = ti * 128
        cls = 0 if ti == 0 else 1
        load_fm(p, "sp", X, xa, c0, 128, "xa")
        load_fm(p, "sp", Y, yb, c0, 128, "yb")
        for kc in range(NCH):
            p.stt("dve", X.ap[:, kc, :], Y.ap[:, kc, :], V.ap[:, GV_GPREV + cls * 16 + kc: GV_GPREV + cls * 16 + kc + 1],
                  X.ap[:, kc, :], ALU.mult, ALU.add, r=[X.r, Y.r, V.r], w=[X.r])
        emit_rstd(p, c, X, 128, sq, p.banks[0], rstd)
        sh = (V.ap[:, GV_MOD + cls * 96: GV_MOD + cls * 96 + 16], V.r)
        emit_mod(p, X, rstd, (gs.ap[:, cls, :], gs.r), sh, 128, h, tmps=tmps)
        for cb in range(8):
            bank = p.banks[1 + cb % 3]
            for kc in range(NCH):
                p.matmul(bank.ap[:, 0:512], h.ap[:, kc, :], Wuv.ap[:, kc, cb * 512:(cb + 1) * 512], kc == 0, False,
                         r=[h.r, Wuv.r], w=[bank.r])
            p.matmul(bank.ap[:, 0:512], c["ones_bf"].ap[0:1, :], buvb.ap[0:1, cb * 512:(cb + 1) * 512], False, True,
                     r=[c["ones_bf"].r, buvb.r], w=[bank.r])
            if cb < 4:
                p.act(u.ap[:, cb * 512:(cb + 1) * 512], bank.ap[:, 0:512], AF.Gelu_apprx_tanh, r=[bank.r], w=[u.r])
            else:
                p.act(v.ap[:, (cb - 4) * 512:(cb - 3) * 512], bank.ap[:, 0:512], AF.Gelu_apprx_tanh, r=[bank.r], w=[v.r])
        p.op("dve", lambda e: e.tensor_reduce(out=st1.ap, in_=v.ap, axis=AX.X, op=ALU.add), [v.r], [st1.r])
        p.ts("dve", st1.ap, st1.ap, 1.0 / D, ALU.mult, r=[st1.r], w=[st1.r])
        p.ts("dve", v.ap, v.ap, st1.ap[:, 0:1], ALU.subtract, r=[v.r, st1.r], w=[v.r])
        p.tt("dve", z.ap, v.ap, v.ap, ALU.mult, r=[v.r], w=[z.r])
        p.op("dve", lambda e: e.tensor_reduce(out=st2.ap, in_=z.ap, axis=AX.X, op=ALU.add), [z.r], [st2.r])
        p.act(st2.ap, st2.ap, AF.Sqrt, r=[st2.r, c["eps"].r], w=[st2.r], bias=c["eps"].ap, scale=1.0 / D)
        p.op("dve", lambda e: e.reciprocal(out=st2.ap, in_=st2.ap), [st2.r], [st2.r])
        p.stt("dve", v.ap, v.ap, st2.ap[:, 0:1], lng.ap, ALU.mult, ALU.mult, r=[v.r, st2.r, lng.r], w=[v.r])
        p.tt("dve", vln.ap, v.ap, lnb.ap, ALU.add, r=[v.r, lnb.r], w=[vln.r])
        for g in range(16):
            bank = p.banks[4 + (g // 4) % 2]
            j = g % 4
            p.matmul(bank.ap[:, j * 128:(j + 1) * 128], ws.ap[:, g, :], vln.ap[:, g * 128:(g + 1) * 128], True, True,
                     r=[ws.r, vln.r], w=[bank.r])
            p.stt("dve", z.ap[:, g * 128:(g + 1) * 128], bank.ap[:, j * 128:(j + 1) * 128], V.ap[:, GV_BS + g: GV_BS + g + 1],
                  u.ap[:, g * 128:(g + 1) * 128], ALU.add, ALU.mult, r=[bank.r, V.r, u.r], w=[z.r])
        for half in range(2):
            tb = p.banks[6 + half]
            tbv = tb.ap.bitcast(BF16)
            for k8 in range(8):
                kc = half * 8 + k8
                p.transpose(tbv[:, k8 * 128:(k8 + 1) * 128], z.ap[:, kc * 128:(kc + 1) * 128], c["ident_bf"].ap,
                            r=[z.r, c["ident_bf"].r], w=[tb.r])
            p.copy("act", zst.ap[:, half * 8:(half + 1) * 8, :], tbv[:, 0:1024].rearrange("p (a b) -> p a b", a=8), r=[tb.r], w=[zst.r])
        outs.append(store_fm(p, "sp", zout, c0, 128, zst, "out"))
    p.emit(final_waits=outs[-1:])
    return nc


def gmlp_vec(gprev, mod, layer, slot, inp):
    cols = [pp(gprev[0]), pp(gprev[1])]
    for cls in range(2):
        for k in range(6):
            cols.append(pp(mod[cls, k]))
    cols.append(pp(inp["norm_mix_g"][layer]))
    cols.append(np.ascontiguousarray(np.asarray(inp["gmlp_b_s"][slot]).T))
    v = np.concatenate(cols, axis=1).astype(np.float32)
    assert v.shape == (128, GM_NV)
    return v


def gmlp_rows(slot, inp):
    r = np.zeros((3, 2 * D), np.float32)
    r[0] = inp["gmlp_b_uv"][slot]
    r[1, :D] = inp["gmlp_ln_g"][slot]
    r[2, :D] = inp["gmlp_ln_b"][slot]
    return r


TF = S // 2


def build_final():
    nc = new_nc()
    p = Prog(nc)
    xa = nc.dram_tensor("xa", [D, TF], F32, kind="ExternalInput").ap()
    yb = nc.dram_tensor("yb", [D, TF], F32, kind="ExternalInput").ap()
    vec = nc.dram_tensor("vec", [128, 48], F32, kind="ExternalInput").ap()
    out = nc.dram_tensor("out", [D, TF], F32, kind="ExternalOutput").ap()
    c = make_consts(p)
    V = p.sb([128, 48], F32, "V")
    p.dma("sp", V.ap, vec, w=[V.r], key="in")
    NB = 256
    Xb = [p.sb([128, NCH, NB], F32, f"X{i}") for i in range(2)]
    Yb = [p.sb([128, NCH, NB], F32, f"Y{i}") for i in range(2)]
    sq = p.sb([128, NCH, NB], BF16, "sq")
    rstd = p.sb([128, NB], F32, "rstd")
    outs = []
    for bi in range(TF // NB):
        c0 = bi * NB
        X = Xb[bi % 2]
        Y = Yb[bi % 2]
        load_fm(p, "sp", X, xa, c0, NB, "xa")
        load_fm(p, "sp", Y, yb, c0, NB, "yb")
        for kc in range(NCH):
            p.stt("dve", X.ap[:, kc, :], Y.ap[:, kc, :], V.ap[:, kc:kc + 1], X.ap[:, kc, :], ALU.mult, ALU.add,
                  r=[X.r, Y.r, V.r], w=[X.r])
        emit_rstd(p, c, X, NB, sq, p.banks[bi % 2], rstd)
        emit_mod(p, X, rstd, (V.ap[:, 16:32], V.r), (V.ap[:, 32:48], V.r), NB, None, out_f32=Y)
        outs.append(store_fm(p, "sp", out, c0, NB, Y, "out"))
    p.emit(final_waits=outs[-1:])
    return nc


_PROGS = {}


def _prog(name, fn):
    if name not in _PROGS:
        _PROGS[name] = fn()
    return _PROGS[name]


def _run(nc, maps):
    res = run_bass_kernel_spmd(nc, maps, core_ids=list(range(len(maps))))
    return res.results


def _core_cols(hf):
    return np.r_[hf * 128:(hf + 1) * 128, CTX + hf * (S // 2): CTX + (hf + 1) * (S // 2)]


def kernel(**inputs):
    inp = {k: np.asarray(v) for k, v in inputs.items()}
    f32 = np.float32
    c5 = np.concatenate([inp["c"], inp["c_ctx"][None]], 0).astype(f32)
    cT = np.ascontiguousarray(c5.T.reshape(NCH, 128, 5).transpose(1, 0, 2))
    maps = [{"cT": cT,
             "w": np.ascontiguousarray(inp["ada_w"][:, :, j * ADA_COLS:(j + 1) * ADA_COLS]),
             "b": np.ascontiguousarray(inp["ada_b"][:, None, j * ADA_COLS:(j + 1) * ADA_COLS])} for j in range(8)]
    r = _run(_prog("ada", build_ada), maps)
    mod = np.concatenate([x["mod"] for x in r], axis=2)

    def modb(layer, b):
        return np.stack([mod[layer, 4].reshape(6, D), mod[layer, b].reshape(6, D)], 0)

    xa = [np.ascontiguousarray(np.concatenate([inp["ctx"][b], inp["x"][b]], 0).T.astype(f32)) for b in range(B)]
    yb = [np.zeros((D, TB), f32) for _ in range(B)]
    gprev = [np.zeros((2, D), f32) for _ in range(B)]
    cosT, sinT, Rm = rope_tables()
    for layer in range(DEPTH):
        kind, slot = layer % 3, layer // 3
        maps = []
        for core in range(8):
            b, hf = core // 2, core % 2
            m = modb(layer, b)
            if kind == 0:
                w = inp["lru_w_in"][slot]
                maps.append({"xa": xa[b], "yb": yb[b], "vec": lru_vec(gprev[b], m, layer, slot, hf, inp),
                             "w_in": np.ascontiguousarray(np.concatenate([w[:, hf * 1024:(hf + 1) * 1024],
                                                                          w[:, D + hf * 1024:D + (hf + 1) * 1024]], 1)),
                             "gw": np.ascontiguousarray(inp["lru_gate_w"][slot][:, :, hf * 8:(hf + 1) * 8])})
            elif kind == 1:
                wqkv = inp["diff_w_qkv"][slot]
                sl = slice(hf * 1024, (hf + 1) * 1024)
                maps.append({"xa": xa[b], "yb": yb[b], "vec": attn_vec(gprev[b], m, layer, slot, inp),
                             "lamv": np.ascontiguousarray(inp["diff_lambda"][slot].reshape(1, 256)),
                             "wq": np.ascontiguousarray(wqkv[:, 0:D][:, sl]),
                             "wk": np.ascontiguousarray(wqkv[:, D:2 * D][:, sl]),
                             "wv": np.ascontiguousarray(wqkv[:, 2 * D:3 * D][:, sl]),
                             "cosT": cosT, "sinT": sinT, "R": Rm})
            else:
                cols = _core_cols(hf)
                maps.append({"xa": np.ascontiguousarray(xa[b][:, cols]), "yb": np.ascontiguousarray(yb[b][:, cols]),
                             "vec": gmlp_vec(gprev[b], m, layer, slot, inp), "w_uv": inp["gmlp_w_uv"][slot],
                             "rows": gmlp_rows(slot, inp),
                             "wsT": np.ascontiguousarray(inp["gmlp_w_s"][slot].transpose(0, 2, 1))})
        if kind == 0:
            r = _run(_prog("lru", build_lru), maps)
            G = [np.concatenate([r[2 * b]["g"], r[2 * b + 1]["g"]], 0) for b in range(B)]
            w_out = inp["lru_w_out"][slot]
        elif kind == 1:
            li = 0.8 - 0.6 * math.exp(-0.3 * layer)
            r = _run(_prog("attn", lambda: build_attn(li)), maps)
            G = [np.concatenate([r[2 * b]["g"], r[2 * b + 1]["g"]], 0) for b in range(B)]
            w_out = inp["diff_w_out"][slot]
        else:
            r = _run(_prog("gmlp", build_gmlp), maps)
            G = []
            for b in range(B):
                g = np.empty((D, TB), dtype=r[0]["g"].dtype)
                for hf in range(2):
                    g[:, _core_cols(hf)] = r[2 * b + hf]["g"]
                G.append(g)
            w_out = inp["gmlp_w_out"][slot]
        del maps
        pw = post_weights(inp, layer)
        maps = [{"xa": xa[b], "yb": yb[b], "G": np.ascontiguousarray(G[b]),
                 "vec": post_vec(gprev[b], modb(layer, b), layer, inp), "w_out": w_out, **pw} for b in range(B)]
        r = _run(_prog("post", build_post), maps)
        del maps, pw
        for b in range(B):
            xa[b] = np.ascontiguousarray(r[b]["xmid"])
            yb[b] = np.ascontiguousarray(r[b]["ymoe"].T)
            m = modb(layer, b)
            gprev[b] = np.stack([m[0, 5], m[1, 5]], 0)
    maps = []
    for core in range(8):
        b, hf = core // 2, core % 2
        sl = slice(CTX + hf * TF, CTX + (hf + 1) * TF)
        vec = np.concatenate([pp(gprev[b][1]), pp(inp["norm_final_g"]), np.zeros((128, 16), f32)], 1).astype(f32)
        maps.append({"xa": np.ascontiguousarray(xa[b][:, sl]), "yb": np.ascontiguousarray(yb[b][:, sl]), "vec": vec})
    r = _run(_prog("final", build_final), maps)
    out = np.empty((B, S, D), f32)
    for core in range(8):
        b, hf = core // 2, core % 2
        out[b, hf * TF:(hf + 1) * TF, :] = r[core]["out"].T
    return out
```
